# Optimizing a Trainium2 kernel written in Bass

```python
import math
import jax
import jax.numpy as jnp
from jax import lax
import numpy as np

D_MODEL = 2048
BATCH = 4
SEQ = 4096
DEPTH = 4

N_MIXERS = 2
N_RET_LAYERS = (DEPTH + 1) // 2
N_GDN_LAYERS = DEPTH // 2
N_MEM = 256
MIX_WIDTH = 2 * D_MODEL
TOK_WIDTH = 3 * MIX_WIDTH // 4
MEM_WIDTH = MIX_WIDTH - TOK_WIDTH
QK_HEAD_DIM = 128
N_QK_HEADS = TOK_WIDTH // 256
QK_WIDTH = N_QK_HEADS * QK_HEAD_DIM
RET_V_HEAD_DIM = TOK_WIDTH // N_QK_HEADS
GDN_V_HEAD_DIM = 128
N_GDN_V_HEADS = TOK_WIDTH // GDN_V_HEAD_DIM
GDN_GQA = N_GDN_V_HEADS // N_QK_HEADS
MEM_HEADS = 4
MEM_HEAD_DIM = MEM_WIDTH // MEM_HEADS
CONV_WIDTH = 4
CHUNK = 64
ROPE_BASE = 10000.0
CONV_CH = 2 * QK_WIDTH + TOK_WIDTH
RET_COLS = CONV_CH + MEM_WIDTH + MIX_WIDTH
GDN_COLS = RET_COLS + 2 * N_GDN_V_HEADS
DEEPNORM_ALPHA = (2.0 * DEPTH) ** 0.25
DEEPNORM_BETA = (8.0 * DEPTH) ** -0.25
LN_EPS = 1e-5
NORM_EPS = 1e-6

kernel_name = 'hybrid_retention_gdn_memory_deepnorm'


def layer_norm(x, g, b):
    xf = x.astype(jnp.float32)
    mu = jnp.mean(xf, -1, keepdims=True)
    var = jnp.mean(jnp.square(xf - mu), -1, keepdims=True)
    return ((xf - mu) * lax.rsqrt(var + LN_EPS) * g.astype(jnp.float32) + b.astype(jnp.float32)).astype(x.dtype)


def rotary(t, positions):
    half = t.shape[-1] // 2
    inv_freq = ROPE_BASE ** (-jnp.arange(half, dtype=jnp.float32) / half)
    ang = positions.astype(jnp.float32)[..., None] * inv_freq
    cos = jnp.cos(ang)[:, :, None, :]
    sin = jnp.sin(ang)[:, :, None, :]
    t1, t2 = t[..., :half], t[..., half:]
    return jnp.concatenate([t1 * cos - t2 * sin, t1 * sin + t2 * cos], -1)


def retention(q, k, v):
    B, S, H, dk = q.shape
    dv = v.shape[-1]
    n = S // CHUNK
    log_gamma = jnp.log1p(-jnp.exp2(-5.0 - jnp.arange(H, dtype=jnp.float32)))
    k = k * dk ** -0.5
    qc = q.reshape(B, n, CHUNK, H, dk)
    kc = k.reshape(B, n, CHUNK, H, dk)
    vc = v.reshape(B, n, CHUNK, H, dv)
    idx = jnp.arange(CHUNK, dtype=jnp.float32)
    rel = idx[:, None] - idx[None, :]
    intra_decay = jnp.where(rel[None] >= 0, jnp.exp(log_gamma[:, None, None] * jnp.maximum(rel, 0.0)[None]), 0.0)
    scores = jnp.einsum('bnihd,bnjhd->bnhij', qc, kc) * intra_decay
    inner = jnp.einsum('bnhij,bnjhv->bnihv', scores, vc)
    q_decay = jnp.exp((idx[:, None] + 1.0) * log_gamma[None, :])
    k_decay = jnp.exp((CHUNK - 1.0 - idx)[:, None] * log_gamma[None, :])
    chunk_decay = jnp.exp(CHUNK * log_gamma)

    def step(state, inp):
        q_i, k_i, v_i = inp
        cross = jnp.einsum('bihd,bhdv->bihv', q_i, state) * q_decay[None, :, :, None]
        state = state * chunk_decay[None, :, None, None] + jnp.einsum('bjhd,bjhv->bhdv', k_i * k_decay[None, :, :, None], v_i)
        return state, cross

    state0 = jnp.zeros((B, H, dk, dv), jnp.float32)
    xs = (jnp.moveaxis(qc, 1, 0), jnp.moveaxis(kc, 1, 0), jnp.moveaxis(vc, 1, 0))
    _, cross = lax.scan(step, state0, xs)
    return (inner + jnp.moveaxis(cross, 0, 1)).reshape(B, S, H, dv)


def _to_chunks(t):
    B, S, H = t.shape[:3]
    t = t.reshape((B, S // CHUNK, CHUNK, H) + t.shape[3:])
    return jnp.moveaxis(t, 3, 1)


def gated_delta_rule(q, k, v, g, beta):
    B, S, H, dk = q.shape
    dv = v.shape[-1]
    qc = _to_chunks(q * dk ** -0.5)
    kc = _to_chunks(k)
    vc = _to_chunks(v)
    bc = _to_chunks(beta)
    g_cum = jnp.cumsum(_to_chunks(g), -1)
    causal = jnp.tril(jnp.ones((CHUNK, CHUNK), bool))
    strict = jnp.tril(jnp.ones((CHUNK, CHUNK), bool), -1)
    diff = g_cum[..., :, None] - g_cum[..., None, :]
    decay = jnp.where(causal, jnp.exp(jnp.where(causal, diff, 0.0)), 0.0)
    k_beta = kc * bc[..., None]
    v_beta = vc * bc[..., None]
    lower = jnp.where(strict, jnp.einsum('bhnid,bhnjd->bhnij', k_beta, kc) * decay, 0.0)
    a_mat = lower + jnp.eye(CHUNK, dtype=jnp.float32)
    u = lax.linalg.triangular_solve(a_mat, v_beta, left_side=True, lower=True, unit_diagonal=True)
    w = lax.linalg.triangular_solve(a_mat, k_beta * jnp.exp(g_cum)[..., None], left_side=True, lower=True, unit_diagonal=True)
    qk = jnp.where(causal, jnp.einsum('bhnid,bhnjd->bhnij', qc, kc) * decay, 0.0)
    q_g = qc * jnp.exp(g_cum)[..., None]
    k_tail = kc * jnp.exp(g_cum[..., -1:] - g_cum)[..., None]
    g_last = jnp.exp(g_cum[..., -1])

    def step(state, inp):
        u_i, w_i, qk_i, q_i, k_i, gl_i = inp
        v_new = u_i - jnp.einsum('bhcd,bhdv->bhcv', w_i, state)
        out = jnp.einsum('bhcd,bhdv->bhcv', q_i, state) + jnp.einsum('bhij,bhjv->bhiv', qk_i, v_new)
        state = state * gl_i[..., None, None] + jnp.einsum('bhcd,bhcv->bhdv', k_i, v_new)
        return state, out

    state0 = jnp.zeros((B, H, dk, dv), jnp.float32)
    xs = tuple(jnp.moveaxis(t, 2, 0) for t in (u, w, qk, q_g, k_tail, g_last))
    _, out = lax.scan(step, state0, xs)
    return out.transpose(1, 0, 3, 2, 4).reshape(B, S, H, dv)


def causal_conv_silu(t, w):
    S = t.shape[1]
    tp = jnp.pad(t, ((0, 0), (CONV_WIDTH - 1, 0), (0, 0)))
    y = tp[:, 0:S, :] * w[0]
    for j in range(1, CONV_WIDTH):
        y = y + tp[:, j:j + S, :] * w[j]
    return jax.nn.silu(y)


def retention_branch(x, positions, w_in, norm_g):
    B, S, _ = x.shape
    h = x @ w_in
    q = h[..., :QK_WIDTH].reshape(B, S, N_QK_HEADS, QK_HEAD_DIM).astype(jnp.float32)
    k = h[..., QK_WIDTH:2 * QK_WIDTH].reshape(B, S, N_QK_HEADS, QK_HEAD_DIM).astype(jnp.float32)
    v = h[..., 2 * QK_WIDTH:CONV_CH].reshape(B, S, N_QK_HEADS, RET_V_HEAD_DIM).astype(jnp.float32)
    mq = h[..., CONV_CH:CONV_CH + MEM_WIDTH]
    z = h[..., CONV_CH + MEM_WIDTH:CONV_CH + MEM_WIDTH + MIX_WIDTH]
    o = retention(rotary(q, positions), rotary(k, positions), v)
    mu = jnp.mean(o, -1, keepdims=True)
    var = jnp.mean(jnp.square(o - mu), -1, keepdims=True)
    o = ((o - mu) * lax.rsqrt(var + NORM_EPS)).reshape(B, S, TOK_WIDTH) * norm_g.astype(jnp.float32)
    return o.astype(x.dtype), mq, z


def gdn_branch(x, w_in, conv_w, a_log, dt_bias, norm_g):
    B, S, _ = x.shape
    h = x @ w_in
    qkv = causal_conv_silu(h[..., :CONV_CH], conv_w).astype(jnp.float32)
    mq = h[..., CONV_CH:CONV_CH + MEM_WIDTH]
    z = h[..., CONV_CH + MEM_WIDTH:CONV_CH + MEM_WIDTH + MIX_WIDTH]
    a = h[..., RET_COLS:RET_COLS + N_GDN_V_HEADS].astype(jnp.float32)
    b = h[..., RET_COLS + N_GDN_V_HEADS:].astype(jnp.float32)
    q = qkv[..., :QK_WIDTH].reshape(B, S, N_QK_HEADS, QK_HEAD_DIM)
    k = qkv[..., QK_WIDTH:2 * QK_WIDTH].reshape(B, S, N_QK_HEADS, QK_HEAD_DIM)
    v = qkv[..., 2 * QK_WIDTH:].reshape(B, S, N_GDN_V_HEADS, GDN_V_HEAD_DIM)
    q = q * lax.rsqrt(jnp.sum(jnp.square(q), -1, keepdims=True) + NORM_EPS)
    k = k * lax.rsqrt(jnp.sum(jnp.square(k), -1, keepdims=True) + NORM_EPS)
    q = jnp.repeat(q, GDN_GQA, axis=2)
    k = jnp.repeat(k, GDN_GQA, axis=2)
    beta = jax.nn.sigmoid(b)
    g = -jnp.exp(a_log.astype(jnp.float32)) * jax.nn.softplus(a + dt_bias.astype(jnp.float32))
    o = gated_delta_rule(q, k, v, g, beta)
    o = o * lax.rsqrt(jnp.mean(jnp.square(o), -1, keepdims=True) + NORM_EPS) * norm_g.astype(jnp.float32)
    return o.reshape(B, S, TOK_WIDTH).astype(x.dtype), mq, z


def memory_attention(mq, mem, w_kv):
    B, S, _ = mq.shape
    kv = mem @ w_kv
    mk = kv[..., :MEM_WIDTH].reshape(B, N_MEM, MEM_HEADS, MEM_HEAD_DIM)
    mv = kv[..., MEM_WIDTH:].reshape(B, N_MEM, MEM_HEADS, MEM_HEAD_DIM)
    q = mq.reshape(B, S, MEM_HEADS, MEM_HEAD_DIM)
    s = jnp.einsum('bshd,bmhd->bhsm', q, mk).astype(jnp.float32) * MEM_HEAD_DIM ** -0.5
    p = jax.nn.softmax(s, -1).astype(mv.dtype)
    return jnp.einsum('bhsm,bmhd->bshd', p, mv).reshape(B, S, MEM_WIDTH)


def setup_inputs(seed: int = 0) -> dict:
    key = jax.random.key(seed)
    ks = jax.random.split(key, 14)
    f32 = jnp.float32
    x = jax.random.normal(ks[0], (BATCH, SEQ, D_MODEL), f32)
    mem = jax.random.normal(ks[1], (BATCH, N_MEM, D_MODEL), f32)
    positions = jnp.arange(SEQ, dtype=jnp.int32)[None, :] + jax.random.randint(ks[2], (BATCH, 1), 0, 1024, dtype=jnp.int32)
    w_in_ret = jax.random.normal(ks[3], (N_RET_LAYERS, D_MODEL, RET_COLS), f32) * D_MODEL ** -0.5
    ret_norm_g = 1.0 + 0.02 * jax.random.normal(ks[4], (N_RET_LAYERS, TOK_WIDTH), f32)
    w_in_gdn = jax.random.normal(ks[5], (N_GDN_LAYERS, D_MODEL, GDN_COLS), f32) * D_MODEL ** -0.5
    conv_w = jax.random.normal(ks[6], (N_GDN_LAYERS, CONV_WIDTH, CONV_CH), f32) * CONV_WIDTH ** -0.5
    a_log = jnp.log(jax.random.uniform(ks[7], (N_GDN_LAYERS, N_GDN_V_HEADS), f32, 1.0, 16.0))
    dt = jnp.exp(jax.random.uniform(ks[8], (N_GDN_LAYERS, N_GDN_V_HEADS), f32, math.log(1e-3), math.log(1e-1)))
    dt_bias = dt + jnp.log(-jnp.expm1(-dt))
    gdn_norm_g = 1.0 + 0.02 * jax.random.normal(ks[9], (N_GDN_LAYERS, GDN_V_HEAD_DIM), f32)
    w_mem_kv = jax.random.normal(ks[10], (DEPTH, D_MODEL, 2 * MEM_WIDTH), f32) * D_MODEL ** -0.5
    w_out = jax.random.normal(ks[11], (DEPTH, MIX_WIDTH, D_MODEL), f32) * (MIX_WIDTH ** -0.5 * DEEPNORM_BETA)
    ln_g = 1.0 + 0.02 * jax.random.normal(ks[12], (DEPTH, D_MODEL), f32)
    ln_b = 0.02 * jax.random.normal(ks[13], (DEPTH, D_MODEL), f32)
    return {'x': x, 'mem': mem, 'positions': positions, 'w_in_ret': w_in_ret, 'ret_norm_g': ret_norm_g,
            'w_in_gdn': w_in_gdn, 'conv_w': conv_w, 'a_log': a_log, 'dt_bias': dt_bias, 'gdn_norm_g': gdn_norm_g,
            'w_mem_kv': w_mem_kv, 'w_out': w_out, 'ln_g': ln_g, 'ln_b': ln_b}


def reference(x, mem, positions, w_in_ret, ret_norm_g, w_in_gdn, conv_w, a_log, dt_bias, gdn_norm_g,
              w_mem_kv, w_out, ln_g, ln_b):
    for i in range(DEPTH):
        j = i // N_MIXERS
        if i % N_MIXERS == 0:
            tok, mq, z = retention_branch(x, positions, w_in_ret[j], ret_norm_g[j])
        else:
            tok, mq, z = gdn_branch(x, w_in_gdn[j], conv_w[j], a_log[j], dt_bias[j], gdn_norm_g[j])
        mem_out = memory_attention(mq, mem, w_mem_kv[i])
        branch = jnp.concatenate([tok, mem_out.astype(tok.dtype)], -1) * jax.nn.silu(z)
        y = branch @ w_out[i]
        x = layer_norm(DEEPNORM_ALPHA * x + y, ln_g[i], ln_b[i])
    return x
```

```python
import contextlib
import os
import numpy as np
GSTOP = float(os.environ.get('GSTOP', '9'))
import concourse.bass as bass
import concourse.mybir as mybir
from concourse.bass_utils import run_bass_kernel_spmd

F32 = mybir.dt.float32
BF16 = mybir.dt.bfloat16
I32 = mybir.dt.int32
AF = mybir.ActivationFunctionType
ALU = mybir.AluOpType
AX = mybir.AxisListType

D = 2048
KC = 16
NMEM = 256
DEPTH = 4
ALPHA = (2.0 * DEPTH) ** 0.25
LN_EPS = 1e-5
NORM_EPS = 1e-6
GW = 772
TWO_PI = float(2 * np.pi)
C_ID, C_UT, C_SM, C_NEGM, C_NEGMT, C_ONES, C_QS, C_KS, C_INVF, C_END = 0, 128, 256, 384, 512, 640, 768, 780, 792, 856


class Buf:
    __slots__ = ("name", "w", "r", "dsem", "excl")

    def __init__(self, name, excl=False):
        self.name = name
        self.w = None
        self.r = []
        self.dsem = None
        self.excl = excl


class Ctx:
    def __init__(self, nc, es):
        self.nc = nc
        self.es = es
        self.eng = {"pe": nc.tensor, "act": nc.scalar, "dve": nc.vector, "pool": nc.gpsimd, "sp": nc.sync}
        self.sem = {}
        self.cnt = {}
        for k in self.eng:
            self.sem[k] = es.enter_context(nc.semaphore("s_" + k))
            self.cnt[k] = 0
        self.waited = {k: {} for k in self.eng}
        self.semobj = {k: self.sem[k] for k in self.eng}
        self.latest = {k: 0 for k in self.eng}
        self.ndsem = 0
        self.free_dsems = []
        self.in_phase = False
        self.phase_keys = []

    def _wait(self, e, ev):
        if ev is None:
            return
        key, val = ev
        if key == e:
            if e == "pe":
                return
            if self.cnt[e] - val >= 2:
                return
        if key not in self.eng:
            val = max(val, self.latest.get(key, val))
        if self.waited[e].get(key, 0) >= val:
            return
        self.waited[e][key] = val
        self.eng[e].wait_ge(self.semobj[key], val)

    def deps(self, e, reads, writes):
        for b in reads:
            self._wait(e, b.w)
            if b.excl:
                for ev in b.r:
                    if ev[0] != e:
                        self._wait(e, ev)
        for b in writes:
            self._wait(e, b.w)
            for ev in b.r:
                self._wait(e, ev)

    def record(self, ev, reads, writes):
        for b in writes:
            b.w = ev
            b.r = []
        for b in reads:
            b.r = [x for x in b.r if x[0] != ev[0]] + [ev]

    def op(self, e, fn, reads=(), writes=(), inc=True):
        self.deps(e, reads, writes)
        ins = fn()
        if inc:
            self.cnt[e] += 1
            ins.then_inc(self.sem[e], 1)
            self.latest[e] = self.cnt[e]
            ev = (e, self.cnt[e])
        else:
            ev = (e, self.cnt[e] + 1)
        self.record(ev, reads, writes)
        return ins

    def dsem_of(self, buf):
        if buf.dsem is None:
            if self.free_dsems:
                key = self.free_dsems.pop()
            else:
                self.ndsem += 1
                s = self.es.enter_context(self.nc.semaphore("d%d" % self.ndsem))
                key = "d%d" % self.ndsem
                self.semobj[key] = s
                self.latest[key] = 0
            buf.dsem = key
            if self.in_phase:
                self.phase_keys.append(key)
        return buf.dsem

    def begin_phase(self):
        self.in_phase = True
        self.phase_keys = []

    def end_phase(self):
        self.barrier()
        self.free_dsems.extend(self.phase_keys)
        self.phase_keys = []
        self.in_phase = False

    def dma(self, q, out, in_, reads, writes, sembuf, transpose=False):
        self.deps(q, reads, writes)
        key = self.dsem_of(sembuf)
        if transpose:
            ins = self.eng[q].dma_start_transpose(out=out, in_=in_)
        else:
            ins = self.eng[q].dma_start(out=out, in_=in_)
        ins.then_inc(self.semobj[key], 16)
        self.latest[key] += 16
        self.record((key, self.latest[key]), reads, writes)
        return ins

    def barrier(self, engines=("pe", "act", "dve", "pool", "sp")):
        for e in engines:
            for key, val in self.latest.items():
                if val <= 0 or key == e:
                    continue
                if self.waited[e].get(key, 0) >= val:
                    continue
                self.waited[e][key] = val
                self.eng[e].wait_ge(self.semobj[key], val)


class Reg:
    __slots__ = ("ap", "buf")

    def __init__(self, ap, buf):
        self.ap = ap
        self.buf = buf


def build(SEGT, NSEG, kinds, dbg=False):
    NB = SEGT // 128
    T = SEGT * NSEG
    NBT = T // 128
    NL = len(kinds)
    nc = bass.Bass("TRN2", target_bir_lowering=False)
    dt = nc.dram_tensor
    x_d = dt("x", [T, D], F32, kind="ExternalInput").ap()
    mem_d = dt("mem", [NMEM, D], F32, kind="ExternalInput").ap()
    pos_d = dt("pos", [128, NBT], I32, kind="ExternalInput").ap()
    cst_d = dt("consts", [128, C_END], F32, kind="ExternalInput").ap()
    wg_d, wo_d, wkv_d, lng_d, lnb_d, p1_d, p2_d, p3_d, p4_d = [], [], [], [], [], [], [], [], []
    for l in range(NL):
        wg_d.append(dt("wg%d" % l, [16, 128, KC, GW], F32, kind="ExternalInput").ap())
        wo_d.append(dt("wo%d" % l, [8, 128, 32, 256], F32, kind="ExternalInput").ap())
        wkv_d.append(dt("wkv%d" % l, [4, 128, KC, 512], F32, kind="ExternalInput").ap())
        lng_d.append(dt("lng%d" % l, [D], F32, kind="ExternalInput").ap())
        lnb_d.append(dt("lnb%d" % l, [D], F32, kind="ExternalInput").ap())
        if kinds[l] == "ret":
            p1_d.append(dt("rng%d" % l, [3072], F32, kind="ExternalInput").ap())
            p2_d.append(None); p3_d.append(None); p4_d.append(None)
        else:
            p1_d.append(dt("gng%d" % l, [128], F32, kind="ExternalInput").ap())
            p2_d.append(dt("cw%d" % l, [128, 192], F32, kind="ExternalInput").ap())
            p3_d.append(dt("alog%d" % l, [24], F32, kind="ExternalInput").ap())
            p4_d.append(dt("dtb%d" % l, [24], F32, kind="ExternalInput").ap())
    out_d = dt("out", [T, D], F32, kind="ExternalOutput").ap()
    xres_d = dt("xres", [T, D], F32).ap()
    xb_d = dt("xb", [T, D], BF16).ap()
    br_d = dt("br", [T, 4096], BF16).ap()
    memb_d = dt("memb", [NMEM, D], BF16).ap()
    st_d = [dt("st%d" % l, [24, 128, 256], F32).ap() for l in range(NL)]
    cst8_d = [dt("cvst%d" % l, [12, 128, 12], F32).ap() for l in range(NL)]
    if dbg:
        dbg_d = dt("dbg_br", [T, 4096], F32, kind="ExternalOutput").ap()

    with contextlib.ExitStack() as es:
        c = Ctx(nc, es)

        _uid = [0]

        def uname(name):
            _uid[0] += 1
            return "%s_u%d" % (name, _uid[0])

        def sbt(stack, name, shape, dtype):
            t = stack.enter_context(nc.sbuf_tensor(uname(name), shape, dtype))
            return t, Buf(name)

        def act(out, in_, func, reads, writes, **kw):
            return c.op("act", lambda: nc.scalar.activation(out=out, in_=in_, func=func, **kw), reads, writes)

        def tt(e, out, in0, in1, op, reads, writes):
            eng = c.eng[e]
            return c.op(e, lambda: eng.tensor_tensor(out=out, in0=in0, in1=in1, op=op), reads, writes)

        def ts(e, out, in0, s1, s2, op0, op1, reads, writes):
            eng = c.eng[e]
            if op1 is None:
                return c.op(e, lambda: eng.tensor_scalar(out=out, in0=in0, scalar1=s1, scalar2=None, op0=op0), reads, writes)
            return c.op(e, lambda: eng.tensor_scalar(out=out, in0=in0, scalar1=s1, scalar2=s2, op0=op0, op1=op1), reads, writes)

        def stt(e, out, in0, scalar, in1, op0, op1, reads, writes):
            eng = c.eng[e]
            return c.op(e, lambda: eng.scalar_tensor_tensor(out=out, in0=in0, scalar=scalar, in1=in1, op0=op0, op1=op1), reads, writes)

        def cp(e, out, in_, reads, writes):
            eng = c.eng[e]
            return c.op(e, lambda: eng.tensor_copy(out=out, in_=in_), reads, writes)

        def mm(out, pairs, reads, writes):
            n = len(pairs)
            for i, (l_, r_) in enumerate(pairs):
                last = i == n - 1
                c.op("pe", lambda: nc.tensor.matmul(out, l_, r_, start=(i == 0), stop=last),
                     reads if (last or i == 0) else (), writes if (last or i == 0) else (), inc=last)

        def trp(out, in_, ident, reads, writes):
            return c.op("pe", lambda: nc.tensor.transpose(out, in_, ident), reads, writes)

        cst, cstb = sbt(es, "cst", [128, C_END], F32)
        c.dma("sp", cst[:], cst_d, [], [cstb], cstb)
        idbf, idbfb = sbt(es, "idbf", [128, 128], BF16)
        cp("dve", idbf[:], cst[:, C_ID:C_ID + 128], [cstb], [idbfb])
        ID32 = cst[:, C_ID:C_ID + 128]
        UT32 = cst[:, C_UT:C_UT + 128]
        SM32 = cst[:, C_SM:C_SM + 128]
        ONES32 = cst[:, C_ONES:C_ONES + 128]
        onebf, onebfb = sbt(es, "onebf", [128, 128], BF16)
        cp("dve", onebf[:], ONES32, [cstb], [onebfb])
        negmbf, negmbfb = sbt(es, "negmbf", [128, 256], BF16)
        cp("dve", negmbf[:], cst[:, C_NEGM:C_NEGM + 256], [cstb], [negmbfb])

        dummy = Buf("dummy")
        for r0 in range(0, T, 512):
            c.dma("pool", xb_d[r0:r0 + 512, :], x_d[r0:r0 + 512, :], [], [], dummy)
        c.dma("pool", memb_d, mem_d, [], [], dummy)
        c.barrier(["sp", "pool"])
        memT, memTb = sbt(es, "memT", [128, KC, NMEM], BF16)
        for kc in range(KC):
            c.dma("sp", memT[:, kc, :], memb_d[:, kc * 128:(kc + 1) * 128], [], [memTb], memTb, transpose=True)
        mkT, mkTb = sbt(es, "mkT", [128, 8, NMEM], BF16)
        mv, mvb = sbt(es, "mv", [128, 2, 1024], BF16)

        has_ret = "ret" in kinds
        rot_d = dt("rot_d", [3, 128, NBT, 64], F32).ap()
        if has_ret:
            c.begin_phase()
            with contextlib.ExitStack() as ps:
                cosT, cosb = sbt(ps, "cosT0", [128, NBT, 64], F32)
                sinT, sinb = sbt(ps, "sinT0", [128, NBT, 64], F32)
                nsinT, nsinb = sbt(ps, "nsinT0", [128, NBT, 64], F32)
                pi_, pib = sbt(ps, "posi", [128, NBT], I32)
                pf_, pfb = sbt(ps, "posf", [128, NBT], F32)
                ang, angb = sbt(ps, "ang", [128, NBT, 64], F32)
                nf, nfb = sbt(ps, "nf", [128, NBT, 64], F32)
                ni, nib = sbt(ps, "ni", [128, NBT, 64], I32)
                c.dma("sp", pi_[:], pos_d, [], [pib], pib)
                cp("dve", pf_[:], pi_[:], [pib], [pfb])
                invf = cst[:, C_INVF:C_INVF + 64]
                for b in range(NBT):
                    ts("dve", ang[:, b, :], invf, pf_[:, b:b + 1], None, ALU.mult, None, [cstb, pfb], [angb])

                def reduce_and_sin(dst, dstb, shift):
                    ts("dve", nf[:], ang[:], shift, 1.0 / TWO_PI, ALU.add, ALU.mult, [angb], [nfb])
                    cp("dve", ni[:], nf[:], [nfb], [nib])
                    cp("dve", nf[:], ni[:], [nib], [nfb])
                    stt("dve", nf[:], nf[:], -TWO_PI, ang[:], ALU.mult, ALU.add, [nfb, angb], [nfb])
                    if shift != 0.0:
                        ts("dve", nf[:], nf[:], shift, None, ALU.add, None, [nfb], [nfb])
                    ni_f = ni[:].bitcast(F32)
                    ts("dve", ni_f, nf[:], float(np.pi), -TWO_PI, ALU.is_gt, ALU.mult, [nfb], [nib])
                    tt("dve", nf[:], nf[:], ni_f, ALU.add, [nfb, nib], [nfb])
                    ts("dve", ni_f, nf[:], -float(np.pi), TWO_PI, ALU.is_lt, ALU.mult, [nfb], [nib])
                    tt("dve", nf[:], nf[:], ni_f, ALU.add, [nfb, nib], [nfb])
                    act(dst[:], nf[:], AF.Sin, [nfb], [dstb])

                reduce_and_sin(sinT, sinb, 0.0)
                reduce_and_sin(cosT, cosb, float(np.pi / 2))
                ts("dve", nsinT[:], sinT[:], -1.0, None, ALU.mult, None, [sinb], [nsinb])
                c.dma("sp", rot_d[0], cosT[:], [cosb], [], cosb)
                c.dma("sp", rot_d[1], sinT[:], [sinb], [], sinb)
                c.dma("sp", rot_d[2], nsinT[:], [nsinb], [], nsinb)
                c.end_phase()
        c.barrier()

        def wtiles(stack, name, shape, dtype, n=2):
            return [Reg(*_mk(stack, "%s%d" % (name, i), shape, dtype)) for i in range(n)]

        def _mk(stack, name, shape, dtype):
            t, b = sbt(stack, name, shape, dtype)
            return t[:], b

        def load_bcast(stack, name, src, n, q="sp"):
            t, b = sbt(stack, name, [128, n], F32)
            c.dma(q, t[:], src.partition_broadcast(128), [], [b], b)
            return t, b

        def mem_kv(l):
            c.begin_phase()
            with contextlib.ExitStack() as ps:
                wk = wtiles(ps, "wkv", [128, KC, 512], BF16, 2)
                pk = [Reg(ps.enter_context(nc.psum_tensor(uname("pkv"), [128, 512], F32))[:], Buf("pkv%d" % i, excl=True)) for i in range(2)]
                it = 0
                for ch in range(4):
                    w = wk[ch % 2]
                    for q4 in range(4):
                        c.dma("pool", w.ap[:, q4 * 4:(q4 + 1) * 4, :], wkv_d[l][ch][:, q4 * 4:(q4 + 1) * 4, :], [], [w.buf], w.buf)
                    if ch < 2:
                        for ct in range(4):
                            p = pk[it % 2]; it += 1
                            mm(p.ap[:, 0:NMEM], [(w.ap[:, kc, ct * 128:(ct + 1) * 128], memT[:, kc, :]) for kc in range(KC)],
                               [w.buf, memTb], [p.buf])
                            act(mkT[:, ch * 4 + ct, :], p.ap[:, 0:NMEM], AF.Copy, [p.buf], [mkTb])
                    else:
                        for mc in range(2):
                            p = pk[it % 2]; it += 1
                            mm(p.ap[:, :], [(memT[:, kc, mc * 128:(mc + 1) * 128], w.ap[:, kc, :]) for kc in range(KC)],
                               [w.buf, memTb], [p.buf])
                            act(mv[:, mc, (ch - 2) * 512:(ch - 1) * 512], p.ap[:, :], AF.Copy, [p.buf], [mvb])
                c.end_phase()

        def phase_a(l, s):
            kind = kinds[l]
            tok0 = s * SEGT
            c.begin_phase()
            with contextlib.ExitStack() as ps:
                xT, xTb = sbt(ps, "xT", [128, KC, SEGT], BF16)
                for kc in range(KC):
                    for r0 in range(0, SEGT, 512):
                        r1 = min(SEGT, r0 + 512)
                        c.dma("sp", xT[:, kc, r0:r1], xb_d[tok0 + r0:tok0 + r1, kc * 128:(kc + 1) * 128], [], [xTb], xTb, transpose=True)
                Wt = wtiles(ps, "W", [128, KC, GW], BF16, 2)
                PS = [ps.enter_context(nc.psum_tensor(uname("ps"), [128, 512], F32)) for i in range(7)]
                PSB = ps.enter_context(nc.psum_tensor(uname("psb"), [128, 1024], BF16))
                _regs = {}
                _bankbuf = {}

                def R(bank, c0, c1):
                    k = (bank, c0, c1)
                    if k not in _regs:
                        if bank not in _bankbuf:
                            _bankbuf[bank] = Buf("psbank_%s" % bank, excl=True)
                        if bank == "b":
                            _regs[k] = Reg(PSB[:, c0:c1], _bankbuf[bank])
                        else:
                            _regs[k] = Reg(PS[bank][:, c0:c1], _bankbuf[bank])
                    return _regs[k]

                def load_w(g, ncols):
                    w = Wt[g % 2]
                    for q4 in range(4):
                        c.dma("pool", w.ap[:, q4 * 4:(q4 + 1) * 4, 0:ncols], wg_d[l][g][:, q4 * 4:(q4 + 1) * 4, 0:ncols], [], [w.buf], w.buf)
                    return w

                def store_branch(t, blk, col0, ncol):
                    r0 = tok0 + blk * 128
                    c.dma("sp", br_d[r0:r0 + 128, col0:col0 + ncol], t.ap, [t.buf], [], t.buf)

                gcols = [768] * 12 + [512] * 4 if kind == "ret" else [GW] * 12 + [512] * 4
                load_w(0, gcols[0])

                st4 = wtiles(ps, "st4", [128, 16], F32, 2)
                e_t = wtiles(ps, "e_t", [128, 256], F32, 2)
                zs_t = wtiles(ps, "zs_t", [128, 256], F32, 2)
                osb = wtiles(ps, "osb", [128, 256], F32, 2)
                junk = wtiles(ps, "junk", [128, 256], F32, 2)
                brt = wtiles(ps, "brt", [128, 256], BF16, 3)

                def gate_from_z(zreg, it, width=256):
                    e = e_t[it % 2]; zs = zs_t[it % 2]
                    act(e.ap[:, 0:width], zreg.ap, AF.Exp, [zreg.buf], [e.buf], scale=-1.0)
                    ts("dve", e.ap[:, 0:width], e.ap[:, 0:width], 1.0, None, ALU.add, None, [e.buf], [e.buf])
                    c.op("dve", lambda: nc.vector.reciprocal(out=e.ap[:, 0:width], in_=e.ap[:, 0:width]), [e.buf], [e.buf])
                    tt("dve", zs.ap[:, 0:width], zreg.ap, e.ap[:, 0:width], ALU.mult, [zreg.buf, e.buf], [zs.buf])
                    return zs

                def rstd_from(stt_, col_in, col_out, scale, eps):
                    act(stt_.ap[:, col_out:col_out + 1], stt_.ap[:, col_in:col_in + 1], AF.Ln, [stt_.buf, epsTb], [stt_.buf], scale=scale, bias=epsT[:, eps:eps + 1])
                    act(stt_.ap[:, col_out:col_out + 1], stt_.ap[:, col_out:col_out + 1], AF.Exp, [stt_.buf], [stt_.buf], scale=-0.5)

                epsT, epsTb = sbt(ps, "epsT", [128, 2], F32)
                c.op("dve", lambda: nc.vector.memset(epsT[:, 0:1], NORM_EPS), [], [epsTb])
                c.op("dve", lambda: nc.vector.memset(epsT[:, 1:2], LN_EPS), [], [epsTb])

                git = 0
                if kind == "ret":
                    gam, gamb = load_bcast(ps, "rng", p1_d[l], 3072)
                    b0 = tok0 // 128
                    cosT, cosb = sbt(ps, "cosT", [128, NB, 64], F32)
                    sinT, sinb = sbt(ps, "sinT", [128, NB, 64], F32)
                    nsinT, nsinb = sbt(ps, "nsinT", [128, NB, 64], F32)
                    c.dma("sp", cosT[:], rot_d[0][:, b0:b0 + NB, :], [], [cosb], cosb)
                    c.dma("sp", sinT[:], rot_d[1][:, b0:b0 + NB, :], [], [sinb], sinb)
                    c.dma("sp", nsinT[:], rot_d[2][:, b0:b0 + NB, :], [], [nsinb], nsinb)
                    qks = wtiles(ps, "qks", [128, 256], F32, 2)
                    vbf = wtiles(ps, "vbf", [128, 256], BF16, 2)
                    rA = wtiles(ps, "rA", [128, 256], F32, 2)
                    rB = wtiles(ps, "rB", [128, 256], F32, 2)
                    rot = wtiles(ps, "rot", [128, 256], BF16, 2)
                    qkT = wtiles(ps, "qkT", [128, 256], BF16, 2)
                    PT = wtiles(ps, "PT", [128, 128], BF16, 2)
                    Y, Yb = sbt(ps, "Y", [128, 256], F32)
                    Sbf = wtiles(ps, "Sbf", [128, 256], BF16, 2)
                    for h in range(12):
                        w = Wt[h % 2]
                        load_w(h + 1, gcols[h + 1])
                        g128 = float((1.0 - 2.0 ** (-5 - h)) ** 128)
                        have_state = s > 0
                        if have_state:
                            c.dma("sp", Y[:], st_d[l][h], [], [Yb], Yb)
                            sb0 = Sbf[0]
                            act(sb0.ap, Y[:], AF.Copy, [Yb], [sb0.buf], scale=g128)
                        for blk in range(NB):
                            it = git; git += 1
                            tok = slice(blk * 128, (blk + 1) * 128)
                            gb = blk
                            PA = R(it % 2, 0, 512)
                            PZ = R(2 + it % 2, 0, 256)
                            mm(PA.ap, [(xT[:, kc, tok], w.ap[:, kc, 0:512]) for kc in range(KC)], [xTb, w.buf], [PA.buf])
                            mm(PZ.ap, [(xT[:, kc, tok], w.ap[:, kc, 512:768]) for kc in range(KC)], [xTb, w.buf], [PZ.buf])
                            qk = qks[it % 2]; v = vbf[it % 2]
                            act(qk.ap[:, 0:128], PA.ap[:, 0:128], AF.Copy, [PA.buf, cstb], [qk.buf], scale=cst[:, C_QS + h:C_QS + h + 1])
                            act(qk.ap[:, 128:256], PA.ap[:, 128:256], AF.Copy, [PA.buf, cstb], [qk.buf], scale=cst[:, C_KS + h:C_KS + h + 1])
                            act(v.ap, PA.ap[:, 256:512], AF.Copy, [PA.buf], [v.buf])
                            a_ = rA[it % 2]; b_ = rB[it % 2]; ro = rot[it % 2]
                            qk4 = qk.ap.rearrange("p (a b d) -> p a b d", a=2, b=2)
                            a4 = a_.ap.rearrange("p (a b d) -> p a b d", a=2, b=2)
                            b4 = b_.ap.rearrange("p (a b d) -> p a b d", a=2, b=2)
                            cosb4 = cosT[:, gb, :].unsqueeze(1).unsqueeze(1).to_broadcast([128, 2, 2, 64])
                            sinb3 = sinT[:, gb, :].unsqueeze(1).to_broadcast([128, 2, 64])
                            nsinb3 = nsinT[:, gb, :].unsqueeze(1).to_broadcast([128, 2, 64])
                            tt("dve", a4, qk4, cosb4, ALU.mult, [qk.buf, cosb], [a_.buf])
                            tt("pool", b4[:, :, 0, :], qk4[:, :, 1, :], nsinb3, ALU.mult, [qk.buf, nsinb], [b_.buf])
                            tt("pool", b4[:, :, 1, :], qk4[:, :, 0, :], sinb3, ALU.mult, [qk.buf, sinb], [b_.buf])
                            tt("dve", ro.ap, a_.ap, b_.ap, ALU.add, [a_.buf, b_.buf], [ro.buf])
                            TR = R("b", 0, 256)
                            trp(TR.ap[:, 0:128], ro.ap[:, 0:128], idbf[:], [ro.buf, idbfb], [TR.buf])
                            trp(TR.ap[:, 128:256], ro.ap[:, 128:256], idbf[:], [ro.buf, idbfb], [TR.buf])
                            qt = qkT[it % 2]
                            act(qt.ap, TR.ap, AF.Copy, [TR.buf], [qt.buf])
                            SC = R(4, 0, 128)
                            mm(SC.ap, [(qt.ap[:, 128:256], qt.ap[:, 0:128])], [qt.buf], [SC.buf])
                            pt = PT[it % 2]
                            tt("dve", pt.ap, SC.ap, UT32, ALU.mult, [SC.buf, cstb], [pt.buf])
                            O = R(5, 0, 256)
                            sprev = Sbf[blk % 2]
                            if have_state:
                                mm(O.ap, [(pt.ap, v.ap), (qt.ap[:, 0:128], sprev.ap)], [pt.buf, v.buf, qt.buf, sprev.buf], [O.buf])
                            else:
                                mm(O.ap, [(pt.ap, v.ap)], [pt.buf, v.buf], [O.buf])
                            KV = R(6, 0, 256)
                            mm(KV.ap, [(ro.ap[:, 128:256], v.ap)], [ro.buf, v.buf], [KV.buf])
                            if have_state:
                                stt("dve", Y[:], Y[:], g128, KV.ap, ALU.mult, ALU.add, [Yb, KV.buf], [Yb])
                            else:
                                cp("dve", Y[:], KV.ap, [KV.buf], [Yb])
                            have_state = True
                            snext = Sbf[(blk + 1) % 2]
                            act(snext.ap, Y[:], AF.Copy, [Yb], [snext.buf], scale=g128)
                            st = st4[it % 2]; o_ = osb[it % 2]; jk = junk[it % 2]
                            act(o_.ap, O.ap, AF.Copy, [O.buf], [o_.buf, st.buf], accum_out=st.ap[:, 0:1])
                            act(jk.ap, O.ap, AF.Square, [O.buf], [jk.buf, st.buf], accum_out=st.ap[:, 1:2])
                            ts("dve", st.ap[:, 2:3], st.ap[:, 0:1], 1.0 / 256, None, ALU.mult, None, [st.buf], [st.buf])
                            tt("dve", st.ap[:, 3:4], st.ap[:, 2:3], st.ap[:, 2:3], ALU.mult, [st.buf], [st.buf])
                            stt("dve", st.ap[:, 4:5], st.ap[:, 1:2], 1.0 / 256, st.ap[:, 3:4], ALU.mult, ALU.subtract, [st.buf], [st.buf])
                            rstd_from(st, 4, 5, 1.0, 0)
                            ts("dve", o_.ap, o_.ap, st.ap[:, 2:3], st.ap[:, 5:6], ALU.subtract, ALU.mult, [o_.buf, st.buf], [o_.buf])
                            zs = gate_from_z(PZ, it)
                            tt("pool", o_.ap, o_.ap, gam[:, h * 256:(h + 1) * 256], ALU.mult, [o_.buf, gamb], [o_.buf])
                            bt = brt[it % 3]
                            tt("dve", bt.ap, o_.ap, zs.ap, ALU.mult, [o_.buf, zs.buf], [bt.buf])
                            store_branch(bt, blk, h * 256, 256)
                        c.dma("sp", st_d[l][h], Y[:], [Yb], [], Yb)
                else:
                    gdn_heads(ps, l, s, tok0, xT, xTb, Wt, load_w, gcols, R, store_branch, gate_from_z, rstd_from,
                              st4, e_t, zs_t, osb, junk, brt)
                    git = 1000
                mqT = wtiles(ps, "mqT", [128, 2, 128], BF16, 2)
                pbf = wtiles(ps, "pbf", [128, 256], BF16, 2)
                pTt = wtiles(ps, "pTt", [128, 256], BF16, 2)
                for m in range(4):
                    g = 12 + m
                    w = Wt[g % 2]
                    if g + 1 < 16:
                        load_w(g + 1, gcols[g + 1])
                    for blk in range(NB):
                        it = git; git += 1
                        tok = slice(blk * 128, (blk + 1) * 128)
                        PQ = R(it % 2, 0, 256)
                        PZ = R(2 + it % 2, 0, 256)
                        for hf in range(2):
                            mm(PQ.ap[:, hf * 128:(hf + 1) * 128], [(w.ap[:, kc, hf * 128:(hf + 1) * 128], xT[:, kc, tok]) for kc in range(KC)],
                               [xTb, w.buf], [PQ.buf])
                        mm(PZ.ap, [(xT[:, kc, tok], w.ap[:, kc, 256:512]) for kc in range(KC)], [xTb, w.buf], [PZ.buf])
                        mq = mqT[it % 2]
                        act(mq.ap.rearrange("p a b -> p (a b)"), PQ.ap, AF.Copy, [PQ.buf], [mq.buf])
                        SC = R(4, 0, 256)
                        mm(SC.ap, [(mq.ap[:, hf, :], mkT[:, m * 2 + hf, :]) for hf in range(2)], [mq.buf, mkTb], [SC.buf])
                        st = st4[it % 2]
                        c.op("dve", lambda: nc.vector.reduce_max(out=st.ap[:, 0:1], in_=SC.ap, axis=AX.X), [SC.buf], [st.buf])
                        ts("dve", st.ap[:, 1:2], st.ap[:, 0:1], -1.0 / 16, None, ALU.mult, None, [st.buf], [st.buf])
                        pb_ = pbf[it % 2]
                        act(pb_.ap, SC.ap, AF.Exp, [SC.buf, st.buf], [pb_.buf, st.buf], scale=1.0 / 16, bias=st.ap[:, 1:2], accum_out=st.ap[:, 2:3])
                        TR = R("b", 0, 256)
                        trp(TR.ap[:, 0:128], pb_.ap[:, 0:128], idbf[:], [pb_.buf, idbfb], [TR.buf])
                        trp(TR.ap[:, 128:256], pb_.ap[:, 128:256], idbf[:], [pb_.buf, idbfb], [TR.buf])
                        pT_ = pTt[it % 2]
                        cp("dve", pT_.ap, TR.ap, [TR.buf], [pT_.buf])
                        O = R(5, 0, 256)
                        mm(O.ap, [(pT_.ap[:, mc * 128:(mc + 1) * 128], mv[:, mc, m * 256:(m + 1) * 256]) for mc in range(2)],
                           [pT_.buf, mvb], [O.buf])
                        c.op("dve", lambda: nc.vector.reciprocal(out=st.ap[:, 3:4], in_=st.ap[:, 2:3]), [st.buf], [st.buf])
                        zs = gate_from_z(PZ, it)
                        bt = brt[it % 3]
                        stt("dve", bt.ap, O.ap, st.ap[:, 3:4], zs.ap, ALU.mult, ALU.mult, [O.buf, st.buf, zs.buf], [bt.buf])
                        store_branch(bt, blk, 3072 + m * 256, 256)
                c.end_phase()

        def gdn_heads(ps, l, s, tok0, xT, xTb, Wt, load_w, gcols, R, store_branch, gate_from_z, rstd_from,
                      st4, e_t, zs_t, osb, junk, brt):
            TT = min(512, SEGT)
            NBK = TT // 128
            cwt, cwtb = sbt(ps, "cwt", [128, 192], F32)
            c.dma("sp", cwt[:], p2_d[l], [], [cwtb], cwtb)
            negA, negAb = load_bcast(ps, "negA", p3_d[l], 24)
            act(negA[:], negA[:], AF.Exp, [negAb], [negAb])
            ts("dve", negA[:], negA[:], -1.0, None, ALU.mult, None, [negAb], [negAb])
            dtb, dtbb = load_bcast(ps, "dtb", p4_d[l], 24)
            gg, ggb = sbt(ps, "gg", [128, 256], F32)
            c.dma("sp", gg[:, 0:128], p1_d[l].partition_broadcast(128), [], [ggb], ggb)
            c.dma("sp", gg[:, 128:256], p1_d[l].partition_broadcast(128), [], [ggb], ggb)
            epsG, epsGb = sbt(ps, "epsG", [128, 1], F32)
            c.op("dve", lambda: nc.vector.memset(epsG[:], NORM_EPS), [], [epsGb])
            hraw, hrawb = sbt(ps, "hraw", [128, 4, 3 + TT], F32)
            acc, accb = sbt(ps, "acc", [128, 4, TT], F32)
            etmp, etmpb = sbt(ps, "etmp", [128, TT], F32)
            cvo, cvob = sbt(ps, "cvo", [128, 4, TT], BF16)
            qsq, qsqb = sbt(ps, "qsq", [128, 2, TT], BF16)
            S32, S32b = sbt(ps, "S32", [128, 2, 128], F32)
            Sbf = wtiles(ps, "gSbf", [128, 2, 128], BF16, 2)
            scal = wtiles(ps, "scal", [128, 32], F32, 2)
            gt_ = wtiles(ps, "gt", [128, 2], F32, 2)
            Gbt = wtiles(ps, "Gbt", [128, 128], F32, 2)
            ngbt = wtiles(ps, "ngbt", [128, 128], F32, 2)
            gbtt = wtiles(ps, "gbtt", [128, 128], F32, 2)
            Dm = wtiles(ps, "Dm", [128, 2, 128], F32, 2)
            DTm = wtiles(ps, "DTm", [128, 2, 128], F32, 2)
            DSm = wtiles(ps, "DSm", [128, 2, 128], F32, 2)
            khat_ = wtiles(ps, "khat", [128, 128], BF16, 2)
            khT_ = wtiles(ps, "khT", [128, 128], BF16, 2)
            khT2_ = wtiles(ps, "khT2", [128, 128], BF16, 2)
            vb_ = wtiles(ps, "vb", [128, 2, 128], F32, 2)
            ktl_ = wtiles(ps, "ktl", [128, 2, 128], BF16, 2)
            NLt = wtiles(ps, "NLt", [128, 2, 128], F32, 2)
            NP = wtiles(ps, "NP", [128, 2, 256], F32, 2)
            QKD = wtiles(ps, "QKD", [128, 2, 128], BF16, 2)
            TTt = wtiles(ps, "TTt", [128, 2, 128], BF16, 2)
            r_ = wtiles(ps, "r_", [128, 2, 128], BF16, 2)
            qSs = wtiles(ps, "qSs", [128, 2, 128], F32, 2)
            vn_ = wtiles(ps, "vn", [128, 2, 128], BF16, 2)
            ones_row = cst[0:1, C_ONES:C_ONES + 128]
            git = 0
            for g in range(12):
                w = Wt[g % 2]
                load_w(g + 1, gcols[g + 1])
                if s > 0:
                    for h in range(2):
                        c.dma("sp", S32[:, h, :], st_d[l][2 * g + h][:, 0:128], [], [S32b], S32b)
                    c.dma("sp", hraw[:, :, 0:3], cst8_d[l][g].rearrange("p (a b) -> p a b", a=4), [], [hrawb], hrawb)
                else:
                    c.op("dve", lambda: nc.vector.memset(S32[:], 0.0), [], [S32b])
                    c.op("dve", lambda: nc.vector.memset(hraw[:, :, 0:3], 0.0), [], [hrawb])
                cp("dve", Sbf[0].ap, S32[:], [S32b], [Sbf[0].buf])
                sidx = 0
                for tt_i in range(SEGT // TT):
                    t0 = tt_i * TT
                    for ct in range(4):
                        PJ = R(1, 0, TT)
                        mm(PJ.ap, [(w.ap[:, kc, ct * 128:(ct + 1) * 128], xT[:, kc, t0:t0 + TT]) for kc in range(KC)], [xTb, w.buf], [PJ.buf])
                        act(hraw[:, ct, 3:3 + TT], PJ.ap, AF.Copy, [PJ.buf], [hrawb])
                        cb = g * 16 + ct * 4
                        ts("dve", acc[:, ct, :], hraw[:, ct, 0:TT], cwt[:, cb:cb + 1], None, ALU.mult, None, [hrawb, cwtb], [accb])
                        for jj in range(1, 4):
                            stt("dve", acc[:, ct, :], hraw[:, ct, jj:jj + TT], cwt[:, cb + jj:cb + jj + 1], acc[:, ct, :], ALU.mult, ALU.add,
                                [hrawb, cwtb, accb], [accb])
                        act(etmp[:], acc[:, ct, :], AF.Exp, [accb], [etmpb], scale=-1.0)
                        ts("dve", etmp[:], etmp[:], 1.0, None, ALU.add, None, [etmpb], [etmpb])
                        c.op("dve", lambda: nc.vector.reciprocal(out=etmp[:], in_=etmp[:]), [etmpb], [etmpb])
                        tt("dve", cvo[:, ct, :], acc[:, ct, :], etmp[:], ALU.mult, [accb, etmpb], [cvob])
                        if ct < 2:
                            act(qsq[:, ct, :], cvo[:, ct, :], AF.Square, [cvob], [qsqb])
                    cp("dve", hraw[:, :, 0:3], hraw[:, :, TT:TT + 3], [hrawb], [hrawb])
                    for bk in range(NBK):
                        it = git; git += 1
                        blk = tt_i * NBK + bk
                        tb = slice(bk * 128, (bk + 1) * 128)
                        tokb = slice(t0 + bk * 128, t0 + (bk + 1) * 128)
                        if GSTOP < 1:
                            bt = brt[it % 3]
                            c.op("dve", lambda: nc.vector.memset(bt.ap, 0.0), [], [bt.buf])
                            store_branch(bt, blk, g * 256, 256)
                            continue
                        PZ = R(2, 0, 260)
                        mm(PZ.ap, [(xT[:, kc, tokb], w.ap[:, kc, 512:772]) for kc in range(KC)], [xTb, w.buf], [PZ.buf])
                        TRK = R("b", 0, 384)
                        for i3 in range(3):
                            trp(TRK.ap[:, i3 * 128:(i3 + 1) * 128], cvo[:, 1 + i3, tb], idbf[:], [cvob, idbfb], [TRK.buf])
                        SM = R(2, 264, 272)
                        mm(SM.ap[:, 0:1], [(qsq[:, 0, tb], onebf[:, 0:1])], [qsqb, onebfb], [SM.buf])
                        mm(SM.ap[:, 1:2], [(qsq[:, 1, tb], onebf[:, 0:1])], [qsqb, onebfb], [SM.buf])
                        sc = scal[it % 2]; gt = gt_[it % 2]
                        act(sc.ap[:, 0:2], SM.ap[:, 0:2], AF.Ln, [SM.buf, epsGb], [sc.buf], bias=epsG[:, 0:1])
                        act(sc.ap[:, 0:2], sc.ap[:, 0:2], AF.Exp, [sc.buf], [sc.buf], scale=-0.5)
                        tt("dve", sc.ap[:, 2:4], PZ.ap[:, 256:258], dtb[:, 2 * g:2 * g + 2], ALU.add, [PZ.buf, dtbb], [sc.buf])
                        act(sc.ap[:, 2:4], sc.ap[:, 2:4], AF.Exp, [sc.buf], [sc.buf])
                        act(sc.ap[:, 2:4], sc.ap[:, 2:4], AF.Ln, [sc.buf], [sc.buf], bias=1.0)
                        tt("dve", gt.ap, sc.ap[:, 2:4], negA[:, 2 * g:2 * g + 2], ALU.mult, [sc.buf, negAb], [gt.buf])
                        act(sc.ap[:, 4:6], PZ.ap[:, 258:260], AF.Exp, [PZ.buf], [sc.buf], scale=-1.0)
                        ts("dve", sc.ap[:, 4:6], sc.ap[:, 4:6], 1.0, None, ALU.add, None, [sc.buf], [sc.buf])
                        c.op("dve", lambda: nc.vector.reciprocal(out=sc.ap[:, 4:6], in_=sc.ap[:, 4:6]), [sc.buf], [sc.buf])
                        if GSTOP < 2:
                            bt = brt[it % 3]
                            c.op("dve", lambda: nc.vector.memset(bt.ap, 0.0), [], [bt.buf])
                            store_branch(bt, blk, g * 256, 256)
                            continue
                        mm(SM.ap[:, 2:4], [(UT32, gt.ap)], [cstb, gt.buf], [SM.buf])
                        mm(SM.ap[:, 4:6], [(ONES32, gt.ap)], [cstb, gt.buf], [SM.buf])
                        act(sc.ap[:, 20:24], SM.ap[:, 2:6], AF.Copy, [SM.buf], [sc.buf])
                        ts("dve", sc.ap[:, 24:26], sc.ap[:, 20:22], -1.0, None, ALU.mult, None, [sc.buf], [sc.buf])
                        D_ = Dm[it % 2]; DT_ = DTm[it % 2]; DS_ = DSm[it % 2]
                        for h in range(2):
                            Gb = Gbt[(2 * it + h) % 2]; ngb = ngbt[(2 * it + h) % 2]; gbt = gbtt[(2 * it + h) % 2]
                            ts("dve", Gb.ap, ONES32, gt.ap[:, h:h + 1], None, ALU.mult, None, [cstb, gt.buf], [Gb.buf])
                            GCB = R(4, h * 128, (h + 1) * 128)
                            mm(GCB.ap, [(Gb.ap, UT32)], [Gb.buf, cstb], [GCB.buf])
                            stt("dve", ngb.ap, GCB.ap, -1.0, cst[:, C_NEGM:C_NEGM + 128], ALU.mult, ALU.add, [GCB.buf, cstb], [ngb.buf])
                            tt("dve", gbt.ap, GCB.ap, cst[:, C_NEGMT:C_NEGMT + 128], ALU.add, [GCB.buf, cstb], [gbt.buf])
                            act(D_.ap[:, h, :], ngb.ap, AF.Exp, [ngb.buf, sc.buf], [D_.buf], bias=sc.ap[:, 20 + h:21 + h])
                            act(DT_.ap[:, h, :], gbt.ap, AF.Exp, [gbt.buf, sc.buf], [DT_.buf], bias=sc.ap[:, 24 + h:25 + h])
                            tt("pool", DS_.ap[:, h, :], D_.ap[:, h, :], SM32, ALU.mult, [D_.buf, cstb], [DS_.buf])
                        if GSTOP < 3:
                            bt = brt[it % 3]
                            c.op("dve", lambda: nc.vector.memset(bt.ap, 0.0), [], [bt.buf])
                            store_branch(bt, blk, g * 256, 256)
                            continue
                        kh = khat_[it % 2]; khT = khT_[it % 2]
                        act(kh.ap, TRK.ap[:, 0:128], AF.Copy, [TRK.buf, sc.buf], [kh.buf], scale=sc.ap[:, 1:2])
                        TK2 = R("b", 384, 512)
                        trp(TK2.ap, kh.ap, idbf[:], [kh.buf, idbfb], [TK2.buf])
                        act(khT.ap, TK2.ap, AF.Copy, [TK2.buf], [khT.buf])
                        khT2 = khT2_[it % 2]
                        cp("dve", khT2.ap, TK2.ap, [TK2.buf], [khT2.buf])
                        vb = vb_[it % 2]; ktl = ktl_[it % 2]
                        for h in range(2):
                            act(vb.ap[:, h, :], TRK.ap[:, 128 + h * 128:256 + h * 128], AF.Copy, [TRK.buf, sc.buf], [vb.buf], scale=sc.ap[:, 4 + h:5 + h])
                        tt("dve", sc.ap[:, 6:8], sc.ap[:, 22:24], sc.ap[:, 20:22], ALU.subtract, [sc.buf], [sc.buf])
                        act(sc.ap[:, 6:8], sc.ap[:, 6:8], AF.Exp, [sc.buf], [sc.buf])
                        act(sc.ap[:, 8:12], sc.ap[:, 20:24], AF.Exp, [sc.buf], [sc.buf])
                        stt("dve", sc.ap[:, 12:14], sc.ap[:, 4:6], -1.0, sc.ap[:, 8:10], ALU.mult, ALU.mult, [sc.buf], [sc.buf])
                        ts("dve", sc.ap[:, 14:15], sc.ap[:, 0:1], 128.0 ** -0.5, None, ALU.mult, None, [sc.buf], [sc.buf])
                        ts("dve", sc.ap[:, 16:18], sc.ap[:, 8:10], sc.ap[:, 14:15], None, ALU.mult, None, [sc.buf], [sc.buf])
                        ts("dve", sc.ap[:, 18:20], sc.ap[:, 4:6], -1.0, None, ALU.mult, None, [sc.buf], [sc.buf])
                        for h in range(2):
                            ts("dve", ktl.ap[:, h, :], kh.ap, sc.ap[:, 6 + h:7 + h], None, ALU.mult, None, [kh.buf, sc.buf], [ktl.buf])
                        if GSTOP < 3.5:
                            bt = brt[it % 3]
                            c.op("dve", lambda: nc.vector.memset(bt.ap, 0.0), [], [bt.buf])
                            store_branch(bt, blk, g * 256, 256)
                            continue
                        KK = R(5, 0, 128); QKT = R(5, 128, 256)
                        mm(KK.ap, [(khT.ap, khT2.ap)], [khT.buf, khT2.buf], [KK.buf])
                        mm(QKT.ap, [(khT.ap, cvo[:, 0, tb])], [khT.buf, cvob], [QKT.buf])
                        NL = NLt[it % 2]; np_ = NP[it % 2]; qkd = QKD[it % 2]; TTm = TTt[it % 2]
                        for h in range(2):
                            stt("dve", NL.ap[:, h, :], KK.ap, sc.ap[:, 18 + h:19 + h], DS_.ap[:, h, :], ALU.mult, ALU.mult, [KK.buf, sc.buf, DS_.buf], [NL.buf])
                            tt("dve", qkd.ap[:, h, :], QKT.ap, DT_.ap[:, h, :], ALU.mult, [QKT.buf, DT_.buf], [qkd.buf])
                        if GSTOP < 4:
                            bt = brt[it % 3]
                            c.op("dve", lambda: nc.vector.memset(bt.ap, 0.0), [], [bt.buf])
                            store_branch(bt, blk, g * 256, 256)
                            continue
                        TRF = [R(5, 256 + h * 128, 384 + h * 128) for h in range(2)]
                        for h in range(2):
                            trp(TRF[h].ap, NL.ap[:, h, :], ID32, [NL.buf, cstb], [TRF[h].buf])
                            act(np_.ap[:, h, 0:128], TRF[h].ap, AF.Copy, [TRF[h].buf], [np_.buf])
                            tt("dve", np_.ap[:, h, 128:256], TRF[h].ap, ID32, ALU.add, [TRF[h].buf, cstb], [np_.buf])
                        for h in range(2):
                            DB = R(6, h * 256, h * 256 + 256)
                            mm(DB.ap[:, 0:128], [(NL.ap[:, h, :], np_.ap[:, h, 0:128])], [NL.buf, np_.buf], [DB.buf])
                            act(np_.ap[:, h, 0:128], DB.ap[:, 0:128], AF.Copy, [DB.buf], [np_.buf])
                            trp(TRF[h].ap, np_.ap[:, h, 0:128], ID32, [np_.buf, cstb], [TRF[h].buf])
                            act(NL.ap[:, h, :], TRF[h].ap, AF.Copy, [TRF[h].buf], [NL.buf])
                        for lev in range(5):
                            for h in range(2):
                                DB = R(6, h * 256, h * 256 + 256)
                                mm(DB.ap, [(NL.ap[:, h, :], np_.ap[:, h, :])], [NL.buf, np_.buf], [DB.buf])
                                act(np_.ap[:, h, 0:128], DB.ap[:, 0:128], AF.Copy, [DB.buf], [np_.buf])
                                tt("dve", np_.ap[:, h, 128:256], np_.ap[:, h, 128:256], DB.ap[:, 128:256], ALU.add, [DB.buf, np_.buf], [np_.buf])
                                trp(TRF[h].ap, np_.ap[:, h, 0:128], ID32, [np_.buf, cstb], [TRF[h].buf])
                                act(NL.ap[:, h, :], TRF[h].ap, AF.Copy, [TRF[h].buf], [NL.buf])
                        for h in range(2):
                            DB = R(6, h * 256, h * 256 + 256)
                            mm(DB.ap[:, 0:128], [(NL.ap[:, h, :], np_.ap[:, h, 128:256])], [NL.buf, np_.buf], [DB.buf])
                            tt("dve", TTm.ap[:, h, :], np_.ap[:, h, 128:256], DB.ap[:, 0:128], ALU.add, [DB.buf, np_.buf], [TTm.buf])
                        if GSTOP < 5:
                            bt = brt[it % 3]
                            c.op("dve", lambda: nc.vector.memset(bt.ap, 0.0), [], [bt.buf])
                            store_branch(bt, blk, g * 256, 256)
                            continue
                        sprev = Sbf[sidx % 2]; snext = Sbf[(sidx + 1) % 2]; sidx += 1
                        rr = r_[it % 2]; qs_ = qSs[it % 2]; vn = vn_[it % 2]; o_ = osb[it % 2]; st = st4[it % 2]; jk = junk[it % 2]
                        for h in range(2):
                            kS = R(0, h * 128, (h + 1) * 128)
                            qS = R(3, 256 + h * 128, 384 + h * 128)
                            mm(kS.ap, [(khT.ap, sprev.ap[:, h, :])], [khT.buf, sprev.buf], [kS.buf])
                            mm(qS.ap, [(cvo[:, 0, tb], sprev.ap[:, h, :])], [cvob, sprev.buf], [qS.buf])
                            stt("dve", rr.ap[:, h, :], kS.ap, sc.ap[:, 12 + h:13 + h], vb.ap[:, h, :], ALU.mult, ALU.add, [kS.buf, sc.buf, vb.buf], [rr.buf])
                            act(qs_.ap[:, h, :], qS.ap, AF.Copy, [qS.buf, sc.buf], [qs_.buf], scale=sc.ap[:, 16 + h:17 + h])
                            VN = R(0, 256 + h * 128, 384 + h * 128)
                            mm(VN.ap, [(TTm.ap[:, h, :], rr.ap[:, h, :])], [TTm.buf, rr.buf], [VN.buf])
                            act(vn.ap[:, h, :], VN.ap, AF.Copy, [VN.buf], [vn.buf])
                            O2 = R(4, h * 128, (h + 1) * 128)
                            KVr = R(4, 256 + h * 128, 256 + (h + 1) * 128)
                            mm(O2.ap, [(qkd.ap[:, h, :], vn.ap[:, h, :])], [qkd.buf, vn.buf], [O2.buf])
                            mm(KVr.ap, [(ktl.ap[:, h, :], vn.ap[:, h, :])], [ktl.buf, vn.buf], [KVr.buf])
                            stt("dve", o_.ap[:, h * 128:(h + 1) * 128], O2.ap, sc.ap[:, 14:15], qs_.ap[:, h, :], ALU.mult, ALU.add,
                                [O2.buf, sc.buf, qs_.buf], [o_.buf])
                            stt("dve", S32[:, h, :], S32[:, h, :], sc.ap[:, 10 + h:11 + h], KVr.ap, ALU.mult, ALU.add, [S32b, sc.buf, KVr.buf], [S32b])
                            act(snext.ap[:, h, :], S32[:, h, :], AF.Copy, [S32b], [snext.buf])
                            act(jk.ap[:, h * 128:(h + 1) * 128], o_.ap[:, h * 128:(h + 1) * 128], AF.Square, [o_.buf], [jk.buf, st.buf], accum_out=st.ap[:, h:h + 1])
                        act(st.ap[:, 2:4], st.ap[:, 0:2], AF.Ln, [st.buf, epsGb], [st.buf], scale=1.0 / 128, bias=epsG[:, 0:1])
                        act(st.ap[:, 2:4], st.ap[:, 2:4], AF.Exp, [st.buf], [st.buf], scale=-0.5)
                        zs = gate_from_z(Reg(PZ.ap[:, 0:256], PZ.buf), it)
                        tt("pool", zs.ap, zs.ap, gg[:], ALU.mult, [zs.buf, ggb], [zs.buf])
                        bt = brt[it % 3]
                        for h in range(2):
                            stt("dve", bt.ap[:, h * 128:(h + 1) * 128], o_.ap[:, h * 128:(h + 1) * 128], st.ap[:, 2 + h:3 + h], zs.ap[:, h * 128:(h + 1) * 128],
                                ALU.mult, ALU.mult, [o_.buf, st.buf, zs.buf], [bt.buf])
                        store_branch(bt, blk, g * 256, 256)
                for h in range(2):
                    c.dma("sp", st_d[l][2 * g + h][:, 0:128], S32[:, h, :], [S32b], [], S32b)
                c.dma("sp", cst8_d[l][g].rearrange("p (a b) -> p a b", a=4), hraw[:, :, 0:3], [hrawb], [], hrawb)

        def phase_b(l, s, last):
            tok0 = s * SEGT
            TT = min(512, SEGT)
            NBK = TT // 128
            c.begin_phase()
            with contextlib.ExitStack() as ps:
                brT = wtiles(ps, "brT", [128, 32, TT], BF16, 2)
                wo = wtiles(ps, "wo", [128, 32, 256], BF16, 2)
                xr = wtiles(ps, "xr", [128, D], F32, NBK)
                y = xr
                jk, jkb = sbt(ps, "jkB", [128, D], F32)
                lng, lngb = load_bcast(ps, "lng", lng_d[l], D)
                lnb, lnbb = load_bcast(ps, "lnb", lnb_d[l], D)
                st = wtiles(ps, "stB", [128, 8], F32, 2)
                epsT, epsTb = sbt(ps, "epsB", [128, 1], F32)
                c.op("dve", lambda: nc.vector.memset(epsT[:], LN_EPS), [], [epsTb])
                PS = [Reg(ps.enter_context(nc.psum_tensor(uname("pB"), [128, 512], F32))[:], Buf("pB%d" % i, excl=True)) for i in range(8)]
                src = x_d if l == 0 else xres_d
                dst = out_d if last else xres_d
                pit = 0
                wit = 0
                for tt_i in range(SEGT // TT):
                    t0 = tok0 + tt_i * TT
                    bT = brT[tt_i % 2]
                    for fc in range(32):
                        c.dma("sp", bT.ap[:, fc, :], br_d[t0:t0 + TT, fc * 128:(fc + 1) * 128], [], [bT.buf], bT.buf, transpose=True)
                    for bk in range(NBK):
                        c.dma("sp", xr[bk].ap, src[t0 + bk * 128:t0 + (bk + 1) * 128, :], [], [xr[bk].buf], xr[bk].buf)
                    for n in range(8):
                        w = wo[wit % 2]; wit += 1
                        for q4 in range(4):
                            c.dma("pool", w.ap[:, q4 * 8:(q4 + 1) * 8, :], wo_d[l][n][:, q4 * 8:(q4 + 1) * 8, :], [], [w.buf], w.buf)
                        for bk in range(NBK):
                            p = PS[pit % 8]; pit += 1
                            mm(p.ap[:, 0:256], [(bT.ap[:, fc, bk * 128:(bk + 1) * 128], w.ap[:, fc, :]) for fc in range(32)],
                               [bT.buf, w.buf], [p.buf])
                            stt("dve", y[bk].ap[:, n * 256:(n + 1) * 256], xr[bk].ap[:, n * 256:(n + 1) * 256], ALPHA, p.ap[:, 0:256],
                                ALU.mult, ALU.add, [xr[bk].buf, p.buf], [y[bk].buf])
                    for bk in range(NBK):
                        s_ = st[bk % 2]; yy = y[bk]
                        act(jk[:], yy.ap, AF.Copy, [yy.buf], [jkb, s_.buf], accum_out=s_.ap[:, 0:1])
                        act(jk[:], yy.ap, AF.Square, [yy.buf], [jkb, s_.buf], accum_out=s_.ap[:, 1:2])
                        ts("dve", s_.ap[:, 2:3], s_.ap[:, 0:1], 1.0 / D, None, ALU.mult, None, [s_.buf], [s_.buf])
                        tt("dve", s_.ap[:, 3:4], s_.ap[:, 2:3], s_.ap[:, 2:3], ALU.mult, [s_.buf], [s_.buf])
                        stt("dve", s_.ap[:, 4:5], s_.ap[:, 1:2], 1.0 / D, s_.ap[:, 3:4], ALU.mult, ALU.subtract, [s_.buf], [s_.buf])
                        act(s_.ap[:, 5:6], s_.ap[:, 4:5], AF.Ln, [s_.buf, epsTb], [s_.buf], bias=epsT[:, 0:1])
                        act(s_.ap[:, 5:6], s_.ap[:, 5:6], AF.Exp, [s_.buf], [s_.buf], scale=-0.5)
                        ts("dve", yy.ap, yy.ap, s_.ap[:, 2:3], s_.ap[:, 5:6], ALU.subtract, ALU.mult, [yy.buf, s_.buf], [yy.buf])
                        tt("pool", yy.ap, yy.ap, lng[:], ALU.mult, [yy.buf, lngb], [yy.buf])
                        tt("pool", yy.ap, yy.ap, lnb[:], ALU.add, [yy.buf, lnbb], [yy.buf])
                        r0 = t0 + bk * 128
                        c.dma("sp", dst[r0:r0 + 128, :], yy.ap, [yy.buf], [], yy.buf)
                        if not last:
                            c.dma("pool", xb_d[r0:r0 + 128, :], yy.ap, [yy.buf], [], yy.buf)
                c.end_phase()

        for l in range(NL):
            mem_kv(l)
            for s in range(NSEG):
                phase_a(l, s)
                phase_b(l, s, l == NL - 1)
        if dbg:
            pass
        c.barrier()
    return nc


def _consts():
    cst = np.zeros((128, C_END), np.float32)
    i = np.arange(128)
    cst[:, C_ID:C_ID + 128] = np.eye(128, dtype=np.float32)
    cst[:, C_UT:C_UT + 128] = (i[None, :] >= i[:, None]).astype(np.float32)
    cst[:, C_SM:C_SM + 128] = (i[:, None] > i[None, :]).astype(np.float32)
    cst[:, C_NEGM:C_NEGM + 128] = np.where(i[None, :] > i[:, None], -30000.0, 0.0)
    cst[:, C_NEGMT:C_NEGMT + 128] = np.where(i[:, None] > i[None, :], -30000.0, 0.0)
    cst[:, C_ONES:C_ONES + 128] = 1.0
    for h in range(12):
        gam = 1.0 - 2.0 ** (-5.0 - h)
        cst[:, C_QS + h] = gam ** (i + 1.0)
        cst[:, C_KS + h] = gam ** (-(i + 1.0)) * 128.0 ** -0.5
    half = 64
    invf = (10000.0 ** (-np.arange(half, dtype=np.float32) / half)).astype(np.float32)
    cst[:, C_INVF:C_INVF + 64] = invf[None, :]
    return cst


def _kc_layout(w, ncols_pad=None):
    n = w.shape[1]
    a = np.ascontiguousarray(w.reshape(KC, 128, n).transpose(1, 0, 2))
    if ncols_pad is not None and ncols_pad != n:
        o = np.zeros((128, KC, ncols_pad), np.float32)
        o[:, :, :n] = a
        return o
    return a


def prep_weights(inp, kinds):
    ws = {"consts": _consts()}
    for l, kind in enumerate(kinds):
        j = l // 2
        wg = np.zeros((16, 128, KC, GW), np.float32)
        if kind == "ret":
            w = np.asarray(inp["w_in_ret"][j])
            for h in range(12):
                cols = np.concatenate([np.arange(h * 128, (h + 1) * 128), 1536 + np.arange(h * 128, (h + 1) * 128),
                                       3072 + np.arange(h * 256, (h + 1) * 256), 7168 + np.arange(h * 256, (h + 1) * 256)])
                wg[h] = _kc_layout(w[:, cols], GW)
            ws["rng%d" % l] = np.ascontiguousarray(inp["ret_norm_g"][j], dtype=np.float32)
        else:
            w = np.asarray(inp["w_in_gdn"][j])
            cwl = np.zeros((128, 192), np.float32)
            cw = np.asarray(inp["conv_w"][j])
            for g in range(12):
                chans = [g * 128, 1536 + g * 128, 3072 + (2 * g) * 128, 3072 + (2 * g + 1) * 128]
                cols = np.concatenate([np.arange(ch, ch + 128) for ch in chans] + [7168 + np.arange(g * 256, (g + 1) * 256),
                                      np.array([11264 + 2 * g, 11264 + 2 * g + 1, 11288 + 2 * g, 11288 + 2 * g + 1])])
                wg[g] = _kc_layout(w[:, cols], GW)
                for ct, ch in enumerate(chans):
                    for jj in range(4):
                        cwl[:, g * 16 + ct * 4 + jj] = cw[jj, ch:ch + 128]
            ws["cw%d" % l] = cwl
            ws["gng%d" % l] = np.ascontiguousarray(inp["gdn_norm_g"][j], dtype=np.float32)
            ws["alog%d" % l] = np.ascontiguousarray(inp["a_log"][j], dtype=np.float32)
            ws["dtb%d" % l] = np.ascontiguousarray(inp["dt_bias"][j], dtype=np.float32)
        for m in range(4):
            cols = np.concatenate([6144 + np.arange(m * 256, (m + 1) * 256), 7168 + 3072 + np.arange(m * 256, (m + 1) * 256)])
            wg[12 + m] = _kc_layout(w[:, cols], GW)
        ws["wg%d" % l] = wg
        wo = np.asarray(inp["w_out"][l])
        ws["wo%d" % l] = np.ascontiguousarray(wo.reshape(32, 128, 8, 256).transpose(2, 1, 0, 3))
        wkv = np.asarray(inp["w_mem_kv"][l])
        ws["wkv%d" % l] = np.ascontiguousarray(wkv.reshape(KC, 128, 4, 512).transpose(2, 1, 0, 3))
        ws["lng%d" % l] = np.ascontiguousarray(inp["ln_g"][l], dtype=np.float32)
        ws["lnb%d" % l] = np.ascontiguousarray(inp["ln_b"][l], dtype=np.float32)
    return ws


def core_map(ws, x_b, mem_b, pos_b):
    T = x_b.shape[0]
    m = dict(ws)
    m["x"] = np.ascontiguousarray(x_b, dtype=np.float32)
    m["mem"] = np.ascontiguousarray(mem_b, dtype=np.float32)
    m["pos"] = np.ascontiguousarray(np.asarray(pos_b, dtype=np.int32).reshape(T // 128, 128).T)
    return m


KINDS = ["ret", "gdn", "ret", "gdn"]


def kernel(x, mem, positions, w_in_ret, ret_norm_g, w_in_gdn, conv_w, a_log, dt_bias, gdn_norm_g,
           w_mem_kv, w_out, ln_g, ln_b):
    inp = dict(w_in_ret=w_in_ret, ret_norm_g=ret_norm_g, w_in_gdn=w_in_gdn, conv_w=conv_w, a_log=a_log, dt_bias=dt_bias,
               gdn_norm_g=gdn_norm_g, w_mem_kv=w_mem_kv, w_out=w_out, ln_g=ln_g, ln_b=ln_b)
    x = np.asarray(x); mem = np.asarray(mem); positions = np.asarray(positions)
    B, S, _ = x.shape
    ws = prep_weights(inp, KINDS)
    nc = build(2048, S // 2048, KINDS)
    in_maps = [core_map(ws, x[b % B], mem[b % B], positions[b % B]) for b in range(8)]
    res = run_bass_kernel_spmd(nc, in_maps, core_ids=list(range(8)))
    return np.stack([res.results[b]["out"] for b in range(B)], axis=0).astype(np.float32)
```

```python
import contextlib
import os
import numpy as np
GSTOP = float(os.environ.get('GSTOP', '9'))
import concourse.bass as bass
import concourse.mybir as mybir
from concourse.bass_utils import run_bass_kernel_spmd

F32 = mybir.dt.float32
BF16 = mybir.dt.bfloat16
I32 = mybir.dt.int32
AF = mybir.ActivationFunctionType
ALU = mybir.AluOpType
AX = mybir.AxisListType

D = 2048
KC = 16
NMEM = 256
DEPTH = 4
ALPHA = (2.0 * DEPTH) ** 0.25
LN_EPS = 1e-5
NORM_EPS = 1e-6
GW = 772
TWO_PI = float(2 * np.pi)
C_ID, C_UT, C_SM, C_NEGM, C_NEGMT, C_ONES, C_QS, C_KS, C_INVF, C_END = 0, 128, 256, 384, 512, 640, 768, 780, 792, 856


class Buf:
    __slots__ = ("name", "w", "r", "dsem", "excl")

    def __init__(self, name, excl=False):
        self.name = name
        self.w = None
        self.r = []
        self.dsem = None
        self.excl = excl


class Ctx:
    def __init__(self, nc, es):
        self.nc = nc
        self.es = es
        self.eng = {"pe": nc.tensor, "act": nc.scalar, "dve": nc.vector, "pool": nc.gpsimd, "sp": nc.sync}
        self.sem = {}
        self.cnt = {}
        for k in self.eng:
            self.sem[k] = es.enter_context(nc.semaphore("s_" + k))
            self.cnt[k] = 0
        self.waited = {k: {} for k in self.eng}
        self.semobj = {k: self.sem[k] for k in self.eng}
        self.latest = {k: 0 for k in self.eng}
        self.ndsem = 0
        self.free_dsems = []
        self.in_phase = False
        self.phase_keys = []

    def _wait(self, e, ev):
        if ev is None:
            return
        key, val = ev
        if key == e:
            if e == "pe":
                return
            if self.cnt[e] - val >= 2:
                return
        if key not in self.eng:
            val = max(val, self.latest.get(key, val))
        if self.waited[e].get(key, 0) >= val:
            return
        self.waited[e][key] = val
        self.eng[e].wait_ge(self.semobj[key], val)

    def deps(self, e, reads, writes):
        for b in reads:
            self._wait(e, b.w)
            if b.excl:
                for ev in b.r:
                    if ev[0] != e:
                        self._wait(e, ev)
        for b in writes:
            self._wait(e, b.w)
            for ev in b.r:
                self._wait(e, ev)

    def record(self, ev, reads, writes):
        for b in writes:
            b.w = ev
            b.r = []
        for b in reads:
            b.r = [x for x in b.r if x[0] != ev[0]] + [ev]

    def op(self, e, fn, reads=(), writes=(), inc=True):
        self.deps(e, reads, writes)
        ins = fn()
        if inc:
            self.cnt[e] += 1
            ins.then_inc(self.sem[e], 1)
            self.latest[e] = self.cnt[e]
            ev = (e, self.cnt[e])
        else:
            ev = (e, self.cnt[e] + 1)
        self.record(ev, reads, writes)
        return ins

    def dsem_of(self, buf):
        if buf.dsem is None:
            if self.free_dsems:
                key = self.free_dsems.pop()
            else:
                self.ndsem += 1
                s = self.es.enter_context(self.nc.semaphore("d%d" % self.ndsem))
                key = "d%d" % self.ndsem
                self.semobj[key] = s
                self.latest[key] = 0
            buf.dsem = key
            if self.in_phase:
                self.phase_keys.append(key)
        return buf.dsem

    def begin_phase(self):
        self.in_phase = True
        self.phase_keys = []

    def end_phase(self):
        self.barrier()
        self.free_dsems.extend(self.phase_keys)
        self.phase_keys = []
        self.in_phase = False

    def dma(self, q, out, in_, reads, writes, sembuf, transpose=False):
        self.deps(q, reads, writes)
        key = self.dsem_of(sembuf)
        if transpose:
            ins = self.eng[q].dma_start_transpose(out=out, in_=in_)
        else:
            ins = self.eng[q].dma_start(out=out, in_=in_)
        ins.then_inc(self.semobj[key], 16)
        self.latest[key] += 16
        self.record((key, self.latest[key]), reads, writes)
        return ins

    def barrier(self, engines=("pe", "act", "dve", "pool", "sp")):
        for e in engines:
            for key, val in self.latest.items():
                if val <= 0 or key == e:
                    continue
                if self.waited[e].get(key, 0) >= val:
                    continue
                self.waited[e][key] = val
                self.eng[e].wait_ge(self.semobj[key], val)


class Reg:
    __slots__ = ("ap", "buf")

    def __init__(self, ap, buf):
        self.ap = ap
        self.buf = buf


def build(SEGT, NSEG, kinds, dbg=False):
    NB = SEGT // 128
    T = SEGT * NSEG
    NBT = T // 128
    NL = len(kinds)
    nc = bass.Bass("TRN2", target_bir_lowering=False)
    dt = nc.dram_tensor
    x_d = dt("x", [T, D], F32, kind="ExternalInput").ap()
    mem_d = dt("mem", [NMEM, D], F32, kind="ExternalInput").ap()
    pos_d = dt("pos", [128, NBT], I32, kind="ExternalInput").ap()
    cst_d = dt("consts", [128, C_END], F32, kind="ExternalInput").ap()
    wg_d, wo_d, wkv_d, lng_d, lnb_d, p1_d, p2_d, p3_d, p4_d = [], [], [], [], [], [], [], [], []
    for l in range(NL):
        wg_d.append(dt("wg%d" % l, [16, 128, KC, GW], F32, kind="ExternalInput").ap())
        wo_d.append(dt("wo%d" % l, [8, 128, 32, 256], F32, kind="ExternalInput").ap())
        wkv_d.append(dt("wkv%d" % l, [4, 128, KC, 512], F32, kind="ExternalInput").ap())
        lng_d.append(dt("lng%d" % l, [D], F32, kind="ExternalInput").ap())
        lnb_d.append(dt("lnb%d" % l, [D], F32, kind="ExternalInput").ap())
        if kinds[l] == "ret":
            p1_d.append(dt("rng%d" % l, [3072], F32, kind="ExternalInput").ap())
            p2_d.append(None); p3_d.append(None); p4_d.append(None)
        else:
            p1_d.append(dt("gng%d" % l, [128], F32, kind="ExternalInput").ap())
            p2_d.append(dt("cw%d" % l, [128, 192], F32, kind="ExternalInput").ap())
            p3_d.append(dt("alog%d" % l, [24], F32, kind="ExternalInput").ap())
            p4_d.append(dt("dtb%d" % l, [24], F32, kind="ExternalInput").ap())
    out_d = dt("out", [T, D], F32, kind="ExternalOutput").ap()
    xres_d = dt("xres", [T, D], F32).ap()
    xb_d = dt("xb", [T, D], BF16).ap()
    br_d = dt("br", [T, 4096], BF16).ap()
    memb_d = dt("memb", [NMEM, D], BF16).ap()
    st_d = [dt("st%d" % l, [24, 128, 256], F32).ap() for l in range(NL)]
    cst8_d = [dt("cvst%d" % l, [12, 128, 12], F32).ap() for l in range(NL)]
    if dbg:
        dbg_d = dt("dbg_br", [T, 4096], F32, kind="ExternalOutput").ap()

    with contextlib.ExitStack() as es:
        c = Ctx(nc, es)

        _uid = [0]

        def uname(name):
            _uid[0] += 1
            return "%s_u%d" % (name, _uid[0])

        def sbt(stack, name, shape, dtype):
            t = stack.enter_context(nc.sbuf_tensor(uname(name), shape, dtype))
            return t, Buf(name)

        def act(out, in_, func, reads, writes, **kw):
            return c.op("act", lambda: nc.scalar.activation(out=out, in_=in_, func=func, **kw), reads, writes)

        def tt(e, out, in0, in1, op, reads, writes):
            eng = c.eng[e]
            return c.op(e, lambda: eng.tensor_tensor(out=out, in0=in0, in1=in1, op=op), reads, writes)

        def ts(e, out, in0, s1, s2, op0, op1, reads, writes):
            eng = c.eng[e]
            if op1 is None:
                return c.op(e, lambda: eng.tensor_scalar(out=out, in0=in0, scalar1=s1, scalar2=None, op0=op0), reads, writes)
            return c.op(e, lambda: eng.tensor_scalar(out=out, in0=in0, scalar1=s1, scalar2=s2, op0=op0, op1=op1), reads, writes)

        def stt(e, out, in0, scalar, in1, op0, op1, reads, writes):
            eng = c.eng[e]
            return c.op(e, lambda: eng.scalar_tensor_tensor(out=out, in0=in0, scalar=scalar, in1=in1, op0=op0, op1=op1), reads, writes)

        def cp(e, out, in_, reads, writes):
            eng = c.eng[e]
            return c.op(e, lambda: eng.tensor_copy(out=out, in_=in_), reads, writes)

        def mm(out, pairs, reads, writes):
            n = len(pairs)
            for i, (l_, r_) in enumerate(pairs):
                last = i == n - 1
                c.op("pe", lambda: nc.tensor.matmul(out, l_, r_, start=(i == 0), stop=last),
                     reads if (last or i == 0) else (), writes if (last or i == 0) else (), inc=last)

        def trp(out, in_, ident, reads, writes):
            return c.op("pe", lambda: nc.tensor.transpose(out, in_, ident), reads, writes)

        cst, cstb = sbt(es, "cst", [128, C_END], F32)
        c.dma("sp", cst[:], cst_d, [], [cstb], cstb)
        idbf, idbfb = sbt(es, "idbf", [128, 128], BF16)
        cp("dve", idbf[:], cst[:, C_ID:C_ID + 128], [cstb], [idbfb])
        ID32 = cst[:, C_ID:C_ID + 128]
        UT32 = cst[:, C_UT:C_UT + 128]
        SM32 = cst[:, C_SM:C_SM + 128]
        ONES32 = cst[:, C_ONES:C_ONES + 128]
        onebf, onebfb = sbt(es, "onebf", [128, 128], BF16)
        cp("dve", onebf[:], ONES32, [cstb], [onebfb])
        negmbf, negmbfb = sbt(es, "negmbf", [128, 256], BF16)
        cp("dve", negmbf[:], cst[:, C_NEGM:C_NEGM + 256], [cstb], [negmbfb])

        dummy = Buf("dummy")
        for r0 in range(0, T, 512):
            c.dma("pool", xb_d[r0:r0 + 512, :], x_d[r0:r0 + 512, :], [], [], dummy)
        c.dma("pool", memb_d, mem_d, [], [], dummy)
        c.barrier(["sp", "pool"])
        memT, memTb = sbt(es, "memT", [128, KC, NMEM], BF16)
        for kc in range(KC):
            c.dma("sp", memT[:, kc, :], memb_d[:, kc * 128:(kc + 1) * 128], [], [memTb], memTb, transpose=True)
        mkT, mkTb = sbt(es, "mkT", [128, 8, NMEM], BF16)
        mv, mvb = sbt(es, "mv", [128, 2, 1024], BF16)

        has_ret = "ret" in kinds
        rot_d = dt("rot_d", [3, 128, NBT, 64], F32).ap()
        if has_ret:
            c.begin_phase()
            with contextlib.ExitStack() as ps:
                cosT, cosb = sbt(ps, "cosT0", [128, NBT, 64], F32)
                sinT, sinb = sbt(ps, "sinT0", [128, NBT, 64], F32)
                nsinT, nsinb = sbt(ps, "nsinT0", [128, NBT, 64], F32)
                pi_, pib = sbt(ps, "posi", [128, NBT], I32)
                pf_, pfb = sbt(ps, "posf", [128, NBT], F32)
                ang, angb = sbt(ps, "ang", [128, NBT, 64], F32)
                nf, nfb = sbt(ps, "nf", [128, NBT, 64], F32)
                ni, nib = sbt(ps, "ni", [128, NBT, 64], I32)
                c.dma("sp", pi_[:], pos_d, [], [pib], pib)
                cp("dve", pf_[:], pi_[:], [pib], [pfb])
                invf = cst[:, C_INVF:C_INVF + 64]
                for b in range(NBT):
                    ts("dve", ang[:, b, :], invf, pf_[:, b:b + 1], None, ALU.mult, None, [cstb, pfb], [angb])

                def reduce_and_sin(dst, dstb, shift):
                    ts("dve", nf[:], ang[:], shift, 1.0 / TWO_PI, ALU.add, ALU.mult, [angb], [nfb])
                    cp("dve", ni[:], nf[:], [nfb], [nib])
                    cp("dve", nf[:], ni[:], [nib], [nfb])
                    stt("dve", nf[:], nf[:], -TWO_PI, ang[:], ALU.mult, ALU.add, [nfb, angb], [nfb])
                    if shift != 0.0:
                        ts("dve", nf[:], nf[:], shift, None, ALU.add, None, [nfb], [nfb])
                    ni_f = ni[:].bitcast(F32)
                    ts("dve", ni_f, nf[:], float(np.pi), -TWO_PI, ALU.is_gt, ALU.mult, [nfb], [nib])
                    tt("dve", nf[:], nf[:], ni_f, ALU.add, [nfb, nib], [nfb])
                    ts("dve", ni_f, nf[:], -float(np.pi), TWO_PI, ALU.is_lt, ALU.mult, [nfb], [nib])
                    tt("dve", nf[:], nf[:], ni_f, ALU.add, [nfb, nib], [nfb])
                    act(dst[:], nf[:], AF.Sin, [nfb], [dstb])

                reduce_and_sin(sinT, sinb, 0.0)
                reduce_and_sin(cosT, cosb, float(np.pi / 2))
                ts("dve", nsinT[:], sinT[:], -1.0, None, ALU.mult, None, [sinb], [nsinb])
                c.dma("sp", rot_d[0], cosT[:], [cosb], [], cosb)
                c.dma("sp", rot_d[1], sinT[:], [sinb], [], sinb)
                c.dma("sp", rot_d[2], nsinT[:], [nsinb], [], nsinb)
                c.end_phase()
        c.barrier()

        def wtiles(stack, name, shape, dtype, n=2):
            return [Reg(*_mk(stack, "%s%d" % (name, i), shape, dtype)) for i in range(n)]

        def _mk(stack, name, shape, dtype):
            t, b = sbt(stack, name, shape, dtype)
            return t[:], b

        def load_bcast(stack, name, src, n, q="sp"):
            t, b = sbt(stack, name, [128, n], F32)
            c.dma(q, t[:], src.partition_broadcast(128), [], [b], b)
            return t, b

        def mem_kv(l):
            c.begin_phase()
            with contextlib.ExitStack() as ps:
                wk = wtiles(ps, "wkv", [128, KC, 512], BF16, 2)
                pk = [Reg(ps.enter_context(nc.psum_tensor(uname("pkv"), [128, 512], F32))[:], Buf("pkv%d" % i, excl=True)) for i in range(2)]
                it = 0
                for ch in range(4):
                    w = wk[ch % 2]
                    for q4 in range(4):
                        c.dma("pool", w.ap[:, q4 * 4:(q4 + 1) * 4, :], wkv_d[l][ch][:, q4 * 4:(q4 + 1) * 4, :], [], [w.buf], w.buf)
                    if ch < 2:
                        for ct in range(4):
                            p = pk[it % 2]; it += 1
                            mm(p.ap[:, 0:NMEM], [(w.ap[:, kc, ct * 128:(ct + 1) * 128], memT[:, kc, :]) for kc in range(KC)],
                               [w.buf, memTb], [p.buf])
                            act(mkT[:, ch * 4 + ct, :], p.ap[:, 0:NMEM], AF.Copy, [p.buf], [mkTb])
                    else:
                        for mc in range(2):
                            p = pk[it % 2]; it += 1
                            mm(p.ap[:, :], [(memT[:, kc, mc * 128:(mc + 1) * 128], w.ap[:, kc, :]) for kc in range(KC)],
                               [w.buf, memTb], [p.buf])
                            act(mv[:, mc, (ch - 2) * 512:(ch - 1) * 512], p.ap[:, :], AF.Copy, [p.buf], [mvb])
                c.end_phase()

        def phase_a(l, s):
            kind = kinds[l]
            tok0 = s * SEGT
            c.begin_phase()
            with contextlib.ExitStack() as ps:
                xT, xTb = sbt(ps, "xT", [128, KC, SEGT], BF16)
                for kc in range(KC):
                    for r0 in range(0, SEGT, 512):
                        r1 = min(SEGT, r0 + 512)
                        c.dma("sp", xT[:, kc, r0:r1], xb_d[tok0 + r0:tok0 + r1, kc * 128:(kc + 1) * 128], [], [xTb], xTb, transpose=True)
                Wt = wtiles(ps, "W", [128, KC, GW], BF16, 2)
                PS = [ps.enter_context(nc.psum_tensor(uname("ps"), [128, 512], F32)) for i in range(7)]
                PSB = ps.enter_context(nc.psum_tensor(uname("psb"), [128, 1024], BF16))
                _regs = {}
                _bankbuf = {}

                def R(bank, c0, c1):
                    k = (bank, c0, c1)
                    if k not in _regs:
                        if bank not in _bankbuf:
                            _bankbuf[bank] = Buf("psbank_%s" % bank, excl=True)
                        if bank == "b":
                            _regs[k] = Reg(PSB[:, c0:c1], _bankbuf[bank])
                        else:
                            _regs[k] = Reg(PS[bank][:, c0:c1], _bankbuf[bank])
                    return _regs[k]

                def load_w(g, ncols):
                    w = Wt[g % 2]
                    for q4 in range(4):
                        c.dma("pool", w.ap[:, q4 * 4:(q4 + 1) * 4, 0:ncols], wg_d[l][g][:, q4 * 4:(q4 + 1) * 4, 0:ncols], [], [w.buf], w.buf)
                    return w

                def store_branch(t, blk, col0, ncol):
                    r0 = tok0 + blk * 128
                    c.dma("sp", br_d[r0:r0 + 128, col0:col0 + ncol], t.ap, [t.buf], [], t.buf)

                gcols = [768] * 12 + [512] * 4 if kind == "ret" else [GW] * 12 + [512] * 4
                load_w(0, gcols[0])

                st4 = wtiles(ps, "st4", [128, 16], F32, 2)
                e_t = wtiles(ps, "e_t", [128, 256], F32, 2)
                zs_t = wtiles(ps, "zs_t", [128, 256], F32, 2)
                osb = wtiles(ps, "osb", [128, 256], F32, 2)
                junk = wtiles(ps, "junk", [128, 256], BF16, 2)
                brt = wtiles(ps, "brt", [128, 256], BF16, 3)

                def gate_from_z(zreg, it, width=256):
                    e = e_t[it % 2]; zs = zs_t[it % 2]
                    act(e.ap[:, 0:width], zreg.ap, AF.Exp, [zreg.buf], [e.buf], scale=-1.0)
                    ts("dve", e.ap[:, 0:width], e.ap[:, 0:width], 1.0, None, ALU.add, None, [e.buf], [e.buf])
                    c.op("dve", lambda: nc.vector.reciprocal(out=e.ap[:, 0:width], in_=e.ap[:, 0:width]), [e.buf], [e.buf])
                    tt("dve", zs.ap[:, 0:width], zreg.ap, e.ap[:, 0:width], ALU.mult, [zreg.buf, e.buf], [zs.buf])
                    return zs

                def rstd_from(stt_, col_in, col_out, scale, eps):
                    act(stt_.ap[:, col_out:col_out + 1], stt_.ap[:, col_in:col_in + 1], AF.Ln, [stt_.buf, epsTb], [stt_.buf], scale=scale, bias=epsT[:, eps:eps + 1])
                    act(stt_.ap[:, col_out:col_out + 1], stt_.ap[:, col_out:col_out + 1], AF.Exp, [stt_.buf], [stt_.buf], scale=-0.5)

                epsT, epsTb = sbt(ps, "epsT", [128, 2], F32)
                c.op("dve", lambda: nc.vector.memset(epsT[:, 0:1], NORM_EPS), [], [epsTb])
                c.op("dve", lambda: nc.vector.memset(epsT[:, 1:2], LN_EPS), [], [epsTb])

                git = 0
                if kind == "ret":
                    gam, gamb = load_bcast(ps, "rng", p1_d[l], 3072)
                    b0 = tok0 // 128
                    cosT, cosb = sbt(ps, "cosT", [128, NB, 64], F32)
                    sinT, sinb = sbt(ps, "sinT", [128, NB, 64], F32)
                    nsinT, nsinb = sbt(ps, "nsinT", [128, NB, 64], F32)
                    c.dma("sp", cosT[:], rot_d[0][:, b0:b0 + NB, :], [], [cosb], cosb)
                    c.dma("sp", sinT[:], rot_d[1][:, b0:b0 + NB, :], [], [sinb], sinb)
                    c.dma("sp", nsinT[:], rot_d[2][:, b0:b0 + NB, :], [], [nsinb], nsinb)
                    qks = wtiles(ps, "qks", [128, 256], F32, 2)
                    vbf = wtiles(ps, "vbf", [128, 256], BF16, 2)
                    rA = wtiles(ps, "rA", [128, 256], F32, 2)
                    rB = wtiles(ps, "rB", [128, 256], F32, 2)
                    rot = wtiles(ps, "rot", [128, 256], BF16, 2)
                    qkT = wtiles(ps, "qkT", [128, 256], BF16, 2)
                    PT = wtiles(ps, "PT", [128, 128], BF16, 2)
                    Y, Yb = sbt(ps, "Y", [128, 256], F32)
                    Sbf = wtiles(ps, "Sbf", [128, 256], BF16, 2)
                    for h in range(12):
                        w = Wt[h % 2]
                        load_w(h + 1, gcols[h + 1])
                        g128 = float((1.0 - 2.0 ** (-5 - h)) ** 128)
                        have_state = s > 0
                        if have_state:
                            c.dma("sp", Y[:], st_d[l][h], [], [Yb], Yb)
                            sb0 = Sbf[0]
                            act(sb0.ap, Y[:], AF.Copy, [Yb], [sb0.buf], scale=g128)
                        git0 = git; git += NB
                        hs = [have_state]

                        def a_proj(blk):
                            it = git0 + blk
                            tok = slice(blk * 128, (blk + 1) * 128)
                            gb = blk
                            PA = R(it % 2, 0, 512)
                            PZ = R(2 + it % 2, 0, 256)
                            mm(PA.ap, [(xT[:, kc, tok], w.ap[:, kc, 0:512]) for kc in range(KC)], [xTb, w.buf], [PA.buf])
                            mm(PZ.ap, [(xT[:, kc, tok], w.ap[:, kc, 512:768]) for kc in range(KC)], [xTb, w.buf], [PZ.buf])

                        def a_rest(blk):
                            it = git0 + blk
                            tok = slice(blk * 128, (blk + 1) * 128)
                            gb = blk
                            PA = R(it % 2, 0, 512)
                            PZ = R(2 + it % 2, 0, 256)
                            qk = qks[it % 2]; v = vbf[it % 2]
                            act(qk.ap[:, 0:128], PA.ap[:, 0:128], AF.Copy, [PA.buf, cstb], [qk.buf], scale=cst[:, C_QS + h:C_QS + h + 1])
                            act(qk.ap[:, 128:256], PA.ap[:, 128:256], AF.Copy, [PA.buf, cstb], [qk.buf], scale=cst[:, C_KS + h:C_KS + h + 1])
                            act(v.ap, PA.ap[:, 256:512], AF.Copy, [PA.buf], [v.buf])
                            a_ = rA[it % 2]; b_ = rB[it % 2]; ro = rot[it % 2]
                            qk4 = qk.ap.rearrange("p (a b d) -> p a b d", a=2, b=2)
                            a4 = a_.ap.rearrange("p (a b d) -> p a b d", a=2, b=2)
                            b4 = b_.ap.rearrange("p (a b d) -> p a b d", a=2, b=2)
                            cosb4 = cosT[:, gb, :].unsqueeze(1).unsqueeze(1).to_broadcast([128, 2, 2, 64])
                            sinb3 = sinT[:, gb, :].unsqueeze(1).to_broadcast([128, 2, 64])
                            nsinb3 = nsinT[:, gb, :].unsqueeze(1).to_broadcast([128, 2, 64])
                            tt("dve", a4, qk4, cosb4, ALU.mult, [qk.buf, cosb], [a_.buf])
                            tt("pool", b4[:, :, 0, :], qk4[:, :, 1, :], nsinb3, ALU.mult, [qk.buf, nsinb], [b_.buf])
                            tt("pool", b4[:, :, 1, :], qk4[:, :, 0, :], sinb3, ALU.mult, [qk.buf, sinb], [b_.buf])
                            tt("dve", ro.ap, a_.ap, b_.ap, ALU.add, [a_.buf, b_.buf], [ro.buf])
                            TR = R("b", 0, 256)
                            trp(TR.ap[:, 0:128], ro.ap[:, 0:128], idbf[:], [ro.buf, idbfb], [TR.buf])
                            trp(TR.ap[:, 128:256], ro.ap[:, 128:256], idbf[:], [ro.buf, idbfb], [TR.buf])
                            qt = qkT[it % 2]
                            act(qt.ap, TR.ap, AF.Copy, [TR.buf], [qt.buf])
                            SC = R(4, 0, 128)
                            mm(SC.ap, [(qt.ap[:, 128:256], qt.ap[:, 0:128])], [qt.buf], [SC.buf])
                            pt = PT[it % 2]
                            tt("dve", pt.ap, SC.ap, UT32, ALU.mult, [SC.buf, cstb], [pt.buf])

                        def b_part(blk):
                            it = git0 + blk
                            tok = slice(blk * 128, (blk + 1) * 128)
                            gb = blk
                            PA = R(it % 2, 0, 512)
                            PZ = R(2 + it % 2, 0, 256)
                            qk = qks[it % 2]; v = vbf[it % 2]
                            a_ = rA[it % 2]; b_ = rB[it % 2]; ro = rot[it % 2]
                            qt = qkT[it % 2]; pt = PT[it % 2]
                            O = R(5, 0, 256)
                            sprev = Sbf[blk % 2]
                            if hs[0]:
                                mm(O.ap, [(pt.ap, v.ap), (qt.ap[:, 0:128], sprev.ap)], [pt.buf, v.buf, qt.buf, sprev.buf], [O.buf])
                            else:
                                mm(O.ap, [(pt.ap, v.ap)], [pt.buf, v.buf], [O.buf])
                            KV = R(6, 0, 256)
                            mm(KV.ap, [(ro.ap[:, 128:256], v.ap)], [ro.buf, v.buf], [KV.buf])
                            if hs[0]:
                                stt("dve", Y[:], Y[:], g128, KV.ap, ALU.mult, ALU.add, [Yb, KV.buf], [Yb])
                            else:
                                cp("dve", Y[:], KV.ap, [KV.buf], [Yb])
                            hs[0] = True
                            snext = Sbf[(blk + 1) % 2]
                            act(snext.ap, Y[:], AF.Copy, [Yb], [snext.buf], scale=g128)
                            st = st4[it % 2]; o_ = osb[it % 2]; jk = junk[it % 2]
                            act(o_.ap, O.ap, AF.Copy, [O.buf], [o_.buf, st.buf], accum_out=st.ap[:, 0:1])
                            act(jk.ap, O.ap, AF.Square, [O.buf], [jk.buf, st.buf], accum_out=st.ap[:, 1:2])
                            ts("dve", st.ap[:, 2:3], st.ap[:, 0:1], 1.0 / 256, None, ALU.mult, None, [st.buf], [st.buf])
                            tt("dve", st.ap[:, 3:4], st.ap[:, 2:3], st.ap[:, 2:3], ALU.mult, [st.buf], [st.buf])
                            stt("dve", st.ap[:, 4:5], st.ap[:, 1:2], 1.0 / 256, st.ap[:, 3:4], ALU.mult, ALU.subtract, [st.buf], [st.buf])
                            rstd_from(st, 4, 5, 1.0, 0)
                            ts("dve", o_.ap, o_.ap, st.ap[:, 2:3], st.ap[:, 5:6], ALU.subtract, ALU.mult, [o_.buf, st.buf], [o_.buf])
                            zs = gate_from_z(PZ, it)
                            tt("pool", o_.ap, o_.ap, gam[:, h * 256:(h + 1) * 256], ALU.mult, [o_.buf, gamb], [o_.buf])
                            bt = brt[it % 3]
                            tt("dve", bt.ap, o_.ap, zs.ap, ALU.mult, [o_.buf, zs.buf], [bt.buf])
                            store_branch(bt, blk, h * 256, 256)

                        a_proj(0)
                        a_rest(0)
                        for blk in range(NB):
                            if blk + 1 < NB:
                                a_proj(blk + 1)
                            b_part(blk)
                            if blk + 1 < NB:
                                a_rest(blk + 1)
                        c.dma("sp", st_d[l][h], Y[:], [Yb], [], Yb)
                else:
                    gdn_heads(ps, l, s, tok0, xT, xTb, Wt, load_w, gcols, R, store_branch, gate_from_z, rstd_from,
                              st4, e_t, zs_t, osb, junk, brt)
                    git = 1000
                mqT = wtiles(ps, "mqT", [128, 2, 128], BF16, 2)
                pbf = wtiles(ps, "pbf", [128, 256], BF16, 2)
                pTt = wtiles(ps, "pTt", [128, 256], BF16, 2)
                for m in range(4):
                    g = 12 + m
                    w = Wt[g % 2]
                    if g + 1 < 16:
                        load_w(g + 1, gcols[g + 1])
                    for blk in range(NB):
                        it = git; git += 1
                        tok = slice(blk * 128, (blk + 1) * 128)
                        PQ = R(it % 2, 0, 256)
                        PZ = R(2 + it % 2, 0, 256)
                        for hf in range(2):
                            mm(PQ.ap[:, hf * 128:(hf + 1) * 128], [(w.ap[:, kc, hf * 128:(hf + 1) * 128], xT[:, kc, tok]) for kc in range(KC)],
                               [xTb, w.buf], [PQ.buf])
                        mm(PZ.ap, [(xT[:, kc, tok], w.ap[:, kc, 256:512]) for kc in range(KC)], [xTb, w.buf], [PZ.buf])
                        mq = mqT[it % 2]
                        act(mq.ap.rearrange("p a b -> p (a b)"), PQ.ap, AF.Copy, [PQ.buf], [mq.buf])
                        SC = R(4, 0, 256)
                        mm(SC.ap, [(mq.ap[:, hf, :], mkT[:, m * 2 + hf, :]) for hf in range(2)], [mq.buf, mkTb], [SC.buf])
                        st = st4[it % 2]
                        c.op("dve", lambda: nc.vector.reduce_max(out=st.ap[:, 0:1], in_=SC.ap, axis=AX.X), [SC.buf], [st.buf])
                        ts("dve", st.ap[:, 1:2], st.ap[:, 0:1], -1.0 / 16, None, ALU.mult, None, [st.buf], [st.buf])
                        pb_ = pbf[it % 2]
                        act(pb_.ap, SC.ap, AF.Exp, [SC.buf, st.buf], [pb_.buf, st.buf], scale=1.0 / 16, bias=st.ap[:, 1:2], accum_out=st.ap[:, 2:3])
                        TR = R("b", 0, 256)
                        trp(TR.ap[:, 0:128], pb_.ap[:, 0:128], idbf[:], [pb_.buf, idbfb], [TR.buf])
                        trp(TR.ap[:, 128:256], pb_.ap[:, 128:256], idbf[:], [pb_.buf, idbfb], [TR.buf])
                        pT_ = pTt[it % 2]
                        cp("dve", pT_.ap, TR.ap, [TR.buf], [pT_.buf])
                        O = R(5, 0, 256)
                        mm(O.ap, [(pT_.ap[:, mc * 128:(mc + 1) * 128], mv[:, mc, m * 256:(m + 1) * 256]) for mc in range(2)],
                           [pT_.buf, mvb], [O.buf])
                        c.op("dve", lambda: nc.vector.reciprocal(out=st.ap[:, 3:4], in_=st.ap[:, 2:3]), [st.buf], [st.buf])
                        zs = gate_from_z(PZ, it)
                        bt = brt[it % 3]
                        stt("dve", bt.ap, O.ap, st.ap[:, 3:4], zs.ap, ALU.mult, ALU.mult, [O.buf, st.buf, zs.buf], [bt.buf])
                        store_branch(bt, blk, 3072 + m * 256, 256)
                c.end_phase()

        def gdn_heads(ps, l, s, tok0, xT, xTb, Wt, load_w, gcols, R, store_branch, gate_from_z, rstd_from,
                      st4, e_t, zs_t, osb, junk, brt):
            TT = min(512, SEGT)
            NBK = TT // 128
            cwt, cwtb = sbt(ps, "cwt", [128, 192], F32)
            c.dma("sp", cwt[:], p2_d[l], [], [cwtb], cwtb)
            negA, negAb = load_bcast(ps, "negA", p3_d[l], 24)
            act(negA[:], negA[:], AF.Exp, [negAb], [negAb])
            ts("dve", negA[:], negA[:], -1.0, None, ALU.mult, None, [negAb], [negAb])
            dtb, dtbb = load_bcast(ps, "dtb", p4_d[l], 24)
            gg, ggb = sbt(ps, "gg", [128, 256], F32)
            c.dma("sp", gg[:, 0:128], p1_d[l].partition_broadcast(128), [], [ggb], ggb)
            c.dma("sp", gg[:, 128:256], p1_d[l].partition_broadcast(128), [], [ggb], ggb)
            epsG, epsGb = sbt(ps, "epsG", [128, 1], F32)
            c.op("dve", lambda: nc.vector.memset(epsG[:], NORM_EPS), [], [epsGb])
            hraw, hrawb = sbt(ps, "hraw", [128, 4, 3 + TT], F32)
            acc, accb = sbt(ps, "acc", [128, 2, TT], F32)
            etmp, etmpb = sbt(ps, "etmp", [128, TT], F32)
            cvo, cvob = sbt(ps, "cvo", [128, 4, TT], BF16)
            qsq, qsqb = sbt(ps, "qsq", [128, 2, TT], BF16)
            S32, S32b = sbt(ps, "S32", [128, 2, 128], F32)
            Sbf = wtiles(ps, "gSbf", [128, 2, 128], BF16, 2)
            scal = wtiles(ps, "scal", [128, 32], F32, 4)
            gt_ = wtiles(ps, "gt", [128, 2], F32, 4)
            ez_ = wtiles(ps, "ez", [128, 256], F32, 2)
            zsb_ = wtiles(ps, "zsb", [128, 256], F32, 4)
            Gbt = wtiles(ps, "Gbt", [128, 128], F32, 2)
            ngbt = wtiles(ps, "ngbt", [128, 128], F32, 2)
            gbtt = wtiles(ps, "gbtt", [128, 128], F32, 2)
            Dm = wtiles(ps, "Dm", [128, 2, 128], F32, 2)
            DTm = wtiles(ps, "DTm", [128, 2, 128], F32, 2)
            DSm = wtiles(ps, "DSm", [128, 2, 128], F32, 2)
            khat_ = wtiles(ps, "khat", [128, 128], BF16, 2)
            khT_ = wtiles(ps, "khT", [128, 128], BF16, 4)
            khT2_ = wtiles(ps, "khT2", [128, 128], BF16, 2)
            vb_ = wtiles(ps, "vb", [128, 2, 128], F32, 4)
            ktl_ = wtiles(ps, "ktl", [128, 2, 128], BF16, 4)
            NLt = wtiles(ps, "NLt", [128, 2, 128], F32, 2)
            NP = wtiles(ps, "NP", [128, 2, 256], F32, 2)
            QKD = wtiles(ps, "QKD", [128, 2, 128], BF16, 4)
            TTt = wtiles(ps, "TTt", [128, 2, 128], BF16, 4)
            r_ = wtiles(ps, "r_", [128, 2, 128], BF16, 2)
            qSs = wtiles(ps, "qSs", [128, 2, 128], F32, 2)
            vn_ = wtiles(ps, "vn", [128, 2, 128], BF16, 2)
            ones_row = cst[0:1, C_ONES:C_ONES + 128]
            git = 0
            for g in range(12):
                w = Wt[g % 2]
                load_w(g + 1, gcols[g + 1])
                if s > 0:
                    for h in range(2):
                        c.dma("sp", S32[:, h, :], st_d[l][2 * g + h][:, 0:128], [], [S32b], S32b)
                    c.dma("sp", hraw[:, :, 0:3], cst8_d[l][g].rearrange("p (a b) -> p a b", a=4), [], [hrawb], hrawb)
                else:
                    c.op("dve", lambda: nc.vector.memset(S32[:], 0.0), [], [S32b])
                    c.op("dve", lambda: nc.vector.memset(hraw[:, :, 0:3], 0.0), [], [hrawb])
                cp("dve", Sbf[0].ap, S32[:], [S32b], [Sbf[0].buf])
                sidx = 0
                for tt_i in range(SEGT // TT):
                    t0 = tt_i * TT
                    for ct in range(4):
                        PJ = R(1, 0, TT)
                        mm(PJ.ap, [(w.ap[:, kc, ct * 128:(ct + 1) * 128], xT[:, kc, t0:t0 + TT]) for kc in range(KC)], [xTb, w.buf], [PJ.buf])
                        act(hraw[:, ct, 3:3 + TT], PJ.ap, AF.Copy, [PJ.buf], [hrawb])
                        cb = g * 16 + ct * 4
                        ts("dve", acc[:, ct % 2, :], hraw[:, ct, 0:TT], cwt[:, cb:cb + 1], None, ALU.mult, None, [hrawb, cwtb], [accb])
                        for jj in range(1, 4):
                            stt("dve", acc[:, ct % 2, :], hraw[:, ct, jj:jj + TT], cwt[:, cb + jj:cb + jj + 1], acc[:, ct % 2, :], ALU.mult, ALU.add,
                                [hrawb, cwtb, accb], [accb])
                        act(etmp[:], acc[:, ct % 2, :], AF.Exp, [accb], [etmpb], scale=-1.0)
                        ts("dve", etmp[:], etmp[:], 1.0, None, ALU.add, None, [etmpb], [etmpb])
                        c.op("dve", lambda: nc.vector.reciprocal(out=etmp[:], in_=etmp[:]), [etmpb], [etmpb])
                        tt("dve", cvo[:, ct, :], acc[:, ct % 2, :], etmp[:], ALU.mult, [accb, etmpb], [cvob])
                        if ct < 2:
                            act(qsq[:, ct, :], cvo[:, ct, :], AF.Square, [cvob], [qsqb])
                    cp("dve", hraw[:, :, 0:3], hraw[:, :, TT:TT + 3], [hrawb], [hrawb])
                    def pre(bk, tt_i=tt_i, t0=t0):
                        blk = tt_i * NBK + bk
                        tb = slice(bk * 128, (bk + 1) * 128)
                        tokb = slice(t0 + bk * 128, t0 + (bk + 1) * 128)
                        par = bk % 2
                        PZ = R(par, 0, 260)
                        mm(PZ.ap, [(xT[:, kc, tokb], w.ap[:, kc, 512:772]) for kc in range(KC)], [xTb, w.buf], [PZ.buf])
                        TRK = R("b", par * 512, par * 512 + 384)
                        for i3 in range(3):
                            trp(TRK.ap[:, i3 * 128:(i3 + 1) * 128], cvo[:, 1 + i3, tb], idbf[:], [cvob, idbfb], [TRK.buf])
                        SM = R(par, 264, 272)
                        mm(SM.ap[:, 0:1], [(qsq[:, 0, tb], onebf[:, 0:1])], [qsqb, onebfb], [SM.buf])
                        mm(SM.ap[:, 1:2], [(qsq[:, 1, tb], onebf[:, 0:1])], [qsqb, onebfb], [SM.buf])
                        yield
                        sc = scal[bk]; gt = gt_[bk]
                        act(sc.ap[:, 0:2], SM.ap[:, 0:2], AF.Ln, [SM.buf, epsGb], [sc.buf], bias=epsG[:, 0:1])
                        act(sc.ap[:, 0:2], sc.ap[:, 0:2], AF.Exp, [sc.buf], [sc.buf], scale=-0.5)
                        tt("dve", sc.ap[:, 2:4], PZ.ap[:, 256:258], dtb[:, 2 * g:2 * g + 2], ALU.add, [PZ.buf, dtbb], [sc.buf])
                        act(sc.ap[:, 2:4], sc.ap[:, 2:4], AF.Exp, [sc.buf], [sc.buf])
                        act(sc.ap[:, 2:4], sc.ap[:, 2:4], AF.Ln, [sc.buf], [sc.buf], bias=1.0)
                        tt("dve", gt.ap, sc.ap[:, 2:4], negA[:, 2 * g:2 * g + 2], ALU.mult, [sc.buf, negAb], [gt.buf])
                        act(sc.ap[:, 4:6], PZ.ap[:, 258:260], AF.Exp, [PZ.buf], [sc.buf], scale=-1.0)
                        ts("dve", sc.ap[:, 4:6], sc.ap[:, 4:6], 1.0, None, ALU.add, None, [sc.buf], [sc.buf])
                        c.op("dve", lambda: nc.vector.reciprocal(out=sc.ap[:, 4:6], in_=sc.ap[:, 4:6]), [sc.buf], [sc.buf])
                        e = ez_[bk % 2]; zs = zsb_[bk]
                        act(e.ap, PZ.ap[:, 0:256], AF.Exp, [PZ.buf], [e.buf], scale=-1.0)
                        ts("dve", e.ap, e.ap, 1.0, None, ALU.add, None, [e.buf], [e.buf])
                        c.op("dve", lambda: nc.vector.reciprocal(out=e.ap, in_=e.ap), [e.buf], [e.buf])
                        tt("dve", zs.ap, PZ.ap[:, 0:256], e.ap, ALU.mult, [PZ.buf, e.buf], [zs.buf])
                        tt("pool", zs.ap, zs.ap, gg[:], ALU.mult, [zs.buf, ggb], [zs.buf])
                        yield
                        mm(SM.ap[:, 2:4], [(UT32, gt.ap)], [cstb, gt.buf], [SM.buf])
                        mm(SM.ap[:, 4:6], [(ONES32, gt.ap)], [cstb, gt.buf], [SM.buf])
                        yield
                        act(sc.ap[:, 20:24], SM.ap[:, 2:6], AF.Copy, [SM.buf], [sc.buf])
                        ts("dve", sc.ap[:, 24:26], sc.ap[:, 20:22], -1.0, None, ALU.mult, None, [sc.buf], [sc.buf])
                        D_ = Dm[bk % 2]; DT_ = DTm[bk % 2]; DS_ = DSm[bk % 2]
                        Gbs = [Gbt[h] for h in range(2)]
                        for h in range(2):
                            ts("dve", Gbs[h].ap, ONES32, gt.ap[:, h:h + 1], None, ALU.mult, None, [cstb, gt.buf], [Gbs[h].buf])
                        GCBs = [R(2, (2 * par + h) * 128, (2 * par + h + 1) * 128) for h in range(2)]
                        for h in range(2):
                            mm(GCBs[h].ap, [(Gbs[h].ap, UT32)], [Gbs[h].buf, cstb], [GCBs[h].buf])
                        yield
                        for h in range(2):
                            ngb = ngbt[h]; gbt = gbtt[h]
                            stt("dve", ngb.ap, GCBs[h].ap, -1.0, cst[:, C_NEGM:C_NEGM + 128], ALU.mult, ALU.add, [GCBs[h].buf, cstb], [ngb.buf])
                            tt("dve", gbt.ap, GCBs[h].ap, cst[:, C_NEGMT:C_NEGMT + 128], ALU.add, [GCBs[h].buf, cstb], [gbt.buf])
                            act(D_.ap[:, h, :], ngb.ap, AF.Exp, [ngb.buf, sc.buf], [D_.buf], bias=sc.ap[:, 20 + h:21 + h])
                            act(DT_.ap[:, h, :], gbt.ap, AF.Exp, [gbt.buf, sc.buf], [DT_.buf], bias=sc.ap[:, 24 + h:25 + h])
                            tt("pool", DS_.ap[:, h, :], D_.ap[:, h, :], SM32, ALU.mult, [D_.buf, cstb], [DS_.buf])
                        kh = khat_[bk % 2]; khT = khT_[bk]; khT2 = khT2_[bk % 2]
                        act(kh.ap, TRK.ap[:, 0:128], AF.Copy, [TRK.buf, sc.buf], [kh.buf], scale=sc.ap[:, 1:2])
                        yield
                        TK2 = R("b", par * 512 + 384, par * 512 + 512)
                        trp(TK2.ap, kh.ap, idbf[:], [kh.buf, idbfb], [TK2.buf])
                        yield
                        act(khT.ap, TK2.ap, AF.Copy, [TK2.buf], [khT.buf])
                        cp("dve", khT2.ap, TK2.ap, [TK2.buf], [khT2.buf])
                        vb = vb_[bk]; ktl = ktl_[bk]
                        for h in range(2):
                            act(vb.ap[:, h, :], TRK.ap[:, 128 + h * 128:256 + h * 128], AF.Copy, [TRK.buf, sc.buf], [vb.buf], scale=sc.ap[:, 4 + h:5 + h])
                        tt("dve", sc.ap[:, 6:8], sc.ap[:, 22:24], sc.ap[:, 20:22], ALU.subtract, [sc.buf], [sc.buf])
                        act(sc.ap[:, 6:8], sc.ap[:, 6:8], AF.Exp, [sc.buf], [sc.buf])
                        act(sc.ap[:, 8:12], sc.ap[:, 20:24], AF.Exp, [sc.buf], [sc.buf])
                        stt("dve", sc.ap[:, 12:14], sc.ap[:, 4:6], -1.0, sc.ap[:, 8:10], ALU.mult, ALU.mult, [sc.buf], [sc.buf])
                        ts("dve", sc.ap[:, 14:15], sc.ap[:, 0:1], 128.0 ** -0.5, None, ALU.mult, None, [sc.buf], [sc.buf])
                        ts("dve", sc.ap[:, 16:18], sc.ap[:, 8:10], sc.ap[:, 14:15], None, ALU.mult, None, [sc.buf], [sc.buf])
                        ts("dve", sc.ap[:, 18:20], sc.ap[:, 4:6], -1.0, None, ALU.mult, None, [sc.buf], [sc.buf])
                        for h in range(2):
                            ts("dve", ktl.ap[:, h, :], kh.ap, sc.ap[:, 6 + h:7 + h], None, ALU.mult, None, [kh.buf, sc.buf], [ktl.buf])
                        yield
                        KK = R(2, par * 256, par * 256 + 128); QKT = R(2, par * 256 + 128, par * 256 + 256)
                        mm(KK.ap, [(khT.ap, khT2.ap)], [khT.buf, khT2.buf], [KK.buf])
                        mm(QKT.ap, [(khT.ap, cvo[:, 0, tb])], [khT.buf, cvob], [QKT.buf])
                        yield
                        NL = NLt[bk % 2]; np_ = NP[bk % 2]; qkd = QKD[bk]; TTm = TTt[bk]
                        for h in range(2):
                            stt("dve", NL.ap[:, h, :], KK.ap, sc.ap[:, 18 + h:19 + h], DS_.ap[:, h, :], ALU.mult, ALU.mult, [KK.buf, sc.buf, DS_.buf], [NL.buf])
                            tt("dve", qkd.ap[:, h, :], QKT.ap, DT_.ap[:, h, :], ALU.mult, [QKT.buf, DT_.buf], [qkd.buf])
                        yield
                        DBs = [R(3 + 2 * par + h, 0, 256) for h in range(2)]
                        TRF = [R(3 + 2 * par + h, 256, 384) for h in range(2)]
                        for h in range(2):
                            trp(TRF[h].ap, NL.ap[:, h, :], ID32, [NL.buf, cstb], [TRF[h].buf])
                        yield
                        for h in range(2):
                            act(np_.ap[:, h, 0:128], TRF[h].ap, AF.Copy, [TRF[h].buf], [np_.buf])
                            tt("dve", np_.ap[:, h, 128:256], TRF[h].ap, ID32, ALU.add, [TRF[h].buf, cstb], [np_.buf])
                        yield
                        for h in range(2):
                            mm(DBs[h].ap[:, 0:128], [(NL.ap[:, h, :], np_.ap[:, h, 0:128])], [NL.buf, np_.buf], [DBs[h].buf])
                        yield
                        for h in range(2):
                            act(np_.ap[:, h, 0:128], DBs[h].ap[:, 0:128], AF.Copy, [DBs[h].buf], [np_.buf])
                        yield
                        for h in range(2):
                            trp(TRF[h].ap, np_.ap[:, h, 0:128], ID32, [np_.buf, cstb], [TRF[h].buf])
                        yield
                        for h in range(2):
                            act(NL.ap[:, h, :], TRF[h].ap, AF.Copy, [TRF[h].buf], [NL.buf])
                        yield
                        for lev in range(5):
                            for h in range(2):
                                mm(DBs[h].ap, [(NL.ap[:, h, :], np_.ap[:, h, :])], [NL.buf, np_.buf], [DBs[h].buf])
                            yield
                            for h in range(2):
                                act(np_.ap[:, h, 0:128], DBs[h].ap[:, 0:128], AF.Copy, [DBs[h].buf], [np_.buf])
                                tt("dve", np_.ap[:, h, 128:256], np_.ap[:, h, 128:256], DBs[h].ap[:, 128:256], ALU.add, [DBs[h].buf, np_.buf], [np_.buf])
                            yield
                            for h in range(2):
                                trp(TRF[h].ap, np_.ap[:, h, 0:128], ID32, [np_.buf, cstb], [TRF[h].buf])
                            yield
                            for h in range(2):
                                act(NL.ap[:, h, :], TRF[h].ap, AF.Copy, [TRF[h].buf], [NL.buf])
                            yield
                        for h in range(2):
                            mm(DBs[h].ap[:, 0:128], [(NL.ap[:, h, :], np_.ap[:, h, 128:256])], [NL.buf, np_.buf], [DBs[h].buf])
                        yield
                        for h in range(2):
                            tt("dve", TTm.ap[:, h, :], np_.ap[:, h, 128:256], DBs[h].ap[:, 0:128], ALU.add, [DBs[h].buf, np_.buf], [TTm.buf])

                    for bk0 in range(0, NBK, 2):
                        alive = [pre(bk) for bk in range(bk0, min(NBK, bk0 + 2))]
                        while alive:
                            nxt = []
                            for gen_ in alive:
                                try:
                                    next(gen_)
                                    nxt.append(gen_)
                                except StopIteration:
                                    pass
                            alive = nxt
                    for bk in range(NBK):
                        it = git; git += 1
                        blk = tt_i * NBK + bk
                        tb = slice(bk * 128, (bk + 1) * 128)
                        sc = scal[bk]; khT = khT_[bk]; vb = vb_[bk]; ktl = ktl_[bk]; qkd = QKD[bk]; TTm = TTt[bk]; zs = zsb_[bk]
                        sprev = Sbf[sidx % 2]; snext = Sbf[(sidx + 1) % 2]; sidx += 1
                        rr = r_[it % 2]; qs_ = qSs[it % 2]; vn = vn_[it % 2]; o_ = osb[it % 2]; st = st4[it % 2]; jk = junk[it % 2]
                        for h in range(2):
                            kS = R(3 + h, 0, 128)
                            qS = R(3 + h, 128, 256)
                            mm(kS.ap, [(khT.ap, sprev.ap[:, h, :])], [khT.buf, sprev.buf], [kS.buf])
                            mm(qS.ap, [(cvo[:, 0, tb], sprev.ap[:, h, :])], [cvob, sprev.buf], [qS.buf])
                        for h in range(2):
                            kS = R(3 + h, 0, 128)
                            qS = R(3 + h, 128, 256)
                            stt("dve", rr.ap[:, h, :], kS.ap, sc.ap[:, 12 + h:13 + h], vb.ap[:, h, :], ALU.mult, ALU.add, [kS.buf, sc.buf, vb.buf], [rr.buf])
                            act(qs_.ap[:, h, :], qS.ap, AF.Copy, [qS.buf, sc.buf], [qs_.buf], scale=sc.ap[:, 16 + h:17 + h])
                        for h in range(2):
                            VN = R(5 + h, 0, 128)
                            mm(VN.ap, [(TTm.ap[:, h, :], rr.ap[:, h, :])], [TTm.buf, rr.buf], [VN.buf])
                        for h in range(2):
                            VN = R(5 + h, 0, 128)
                            act(vn.ap[:, h, :], VN.ap, AF.Copy, [VN.buf], [vn.buf])
                        for h in range(2):
                            O2 = R(5 + h, 128, 256)
                            KVr = R(5 + h, 256, 384)
                            mm(O2.ap, [(qkd.ap[:, h, :], vn.ap[:, h, :])], [qkd.buf, vn.buf], [O2.buf])
                            mm(KVr.ap, [(ktl.ap[:, h, :], vn.ap[:, h, :])], [ktl.buf, vn.buf], [KVr.buf])
                        for h in range(2):
                            O2 = R(5 + h, 128, 256)
                            KVr = R(5 + h, 256, 384)
                            stt("dve", S32[:, h, :], S32[:, h, :], sc.ap[:, 10 + h:11 + h], KVr.ap, ALU.mult, ALU.add, [S32b, sc.buf, KVr.buf], [S32b])
                            act(snext.ap[:, h, :], S32[:, h, :], AF.Copy, [S32b], [snext.buf])
                            stt("dve", o_.ap[:, h * 128:(h + 1) * 128], O2.ap, sc.ap[:, 14:15], qs_.ap[:, h, :], ALU.mult, ALU.add,
                                [O2.buf, sc.buf, qs_.buf], [o_.buf])
                            act(jk.ap[:, h * 128:(h + 1) * 128], o_.ap[:, h * 128:(h + 1) * 128], AF.Square, [o_.buf], [jk.buf, st.buf], accum_out=st.ap[:, h:h + 1])
                        act(st.ap[:, 2:4], st.ap[:, 0:2], AF.Ln, [st.buf, epsGb], [st.buf], scale=1.0 / 128, bias=epsG[:, 0:1])
                        act(st.ap[:, 2:4], st.ap[:, 2:4], AF.Exp, [st.buf], [st.buf], scale=-0.5)
                        bt = brt[it % 3]
                        for h in range(2):
                            stt("dve", bt.ap[:, h * 128:(h + 1) * 128], o_.ap[:, h * 128:(h + 1) * 128], st.ap[:, 2 + h:3 + h], zs.ap[:, h * 128:(h + 1) * 128],
                                ALU.mult, ALU.mult, [o_.buf, st.buf, zs.buf], [bt.buf])
                        store_branch(bt, blk, g * 256, 256)
                for h in range(2):
                    c.dma("sp", st_d[l][2 * g + h][:, 0:128], S32[:, h, :], [S32b], [], S32b)
                c.dma("sp", cst8_d[l][g].rearrange("p (a b) -> p a b", a=4), hraw[:, :, 0:3], [hrawb], [], hrawb)

        def phase_b(l, s, last):
            tok0 = s * SEGT
            TT = min(512, SEGT)
            NBK = TT // 128
            c.begin_phase()
            with contextlib.ExitStack() as ps:
                brT = wtiles(ps, "brT", [128, 32, TT], BF16, 2)
                wo = wtiles(ps, "wo", [128, 32, 256], BF16, 2)
                xr = wtiles(ps, "xr", [128, D], F32, NBK)
                y = xr
                jk, jkb = sbt(ps, "jkB", [128, D], F32)
                lng, lngb = load_bcast(ps, "lng", lng_d[l], D)
                lnb, lnbb = load_bcast(ps, "lnb", lnb_d[l], D)
                st = wtiles(ps, "stB", [128, 8], F32, 2)
                epsT, epsTb = sbt(ps, "epsB", [128, 1], F32)
                c.op("dve", lambda: nc.vector.memset(epsT[:], LN_EPS), [], [epsTb])
                PS = [Reg(ps.enter_context(nc.psum_tensor(uname("pB"), [128, 512], F32))[:], Buf("pB%d" % i, excl=True)) for i in range(8)]
                src = x_d if l == 0 else xres_d
                dst = out_d if last else xres_d
                pit = 0
                wit = 0
                for tt_i in range(SEGT // TT):
                    t0 = tok0 + tt_i * TT
                    bT = brT[tt_i % 2]
                    for fc in range(32):
                        c.dma("sp", bT.ap[:, fc, :], br_d[t0:t0 + TT, fc * 128:(fc + 1) * 128], [], [bT.buf], bT.buf, transpose=True)
                    for bk in range(NBK):
                        c.dma("sp", xr[bk].ap, src[t0 + bk * 128:t0 + (bk + 1) * 128, :], [], [xr[bk].buf], xr[bk].buf)
                    for n in range(8):
                        w = wo[wit % 2]; wit += 1
                        for q4 in range(4):
                            c.dma("pool", w.ap[:, q4 * 8:(q4 + 1) * 8, :], wo_d[l][n][:, q4 * 8:(q4 + 1) * 8, :], [], [w.buf], w.buf)
                        for bk in range(NBK):
                            p = PS[pit % 8]; pit += 1
                            mm(p.ap[:, 0:256], [(bT.ap[:, fc, bk * 128:(bk + 1) * 128], w.ap[:, fc, :]) for fc in range(32)],
                               [bT.buf, w.buf], [p.buf])
                            stt("dve", y[bk].ap[:, n * 256:(n + 1) * 256], xr[bk].ap[:, n * 256:(n + 1) * 256], ALPHA, p.ap[:, 0:256],
                                ALU.mult, ALU.add, [xr[bk].buf, p.buf], [y[bk].buf])
                    for bk in range(NBK):
                        s_ = st[bk % 2]; yy = y[bk]
                        act(jk[:], yy.ap, AF.Copy, [yy.buf], [jkb, s_.buf], accum_out=s_.ap[:, 0:1])
                        act(jk[:], yy.ap, AF.Square, [yy.buf], [jkb, s_.buf], accum_out=s_.ap[:, 1:2])
                        ts("dve", s_.ap[:, 2:3], s_.ap[:, 0:1], 1.0 / D, None, ALU.mult, None, [s_.buf], [s_.buf])
                        tt("dve", s_.ap[:, 3:4], s_.ap[:, 2:3], s_.ap[:, 2:3], ALU.mult, [s_.buf], [s_.buf])
                        stt("dve", s_.ap[:, 4:5], s_.ap[:, 1:2], 1.0 / D, s_.ap[:, 3:4], ALU.mult, ALU.subtract, [s_.buf], [s_.buf])
                        act(s_.ap[:, 5:6], s_.ap[:, 4:5], AF.Ln, [s_.buf, epsTb], [s_.buf], bias=epsT[:, 0:1])
                        act(s_.ap[:, 5:6], s_.ap[:, 5:6], AF.Exp, [s_.buf], [s_.buf], scale=-0.5)
                        ts("dve", yy.ap, yy.ap, s_.ap[:, 2:3], s_.ap[:, 5:6], ALU.subtract, ALU.mult, [yy.buf, s_.buf], [yy.buf])
                        tt("pool", yy.ap, yy.ap, lng[:], ALU.mult, [yy.buf, lngb], [yy.buf])
                        tt("pool", yy.ap, yy.ap, lnb[:], ALU.add, [yy.buf, lnbb], [yy.buf])
                        r0 = t0 + bk * 128
                        c.dma("sp", dst[r0:r0 + 128, :], yy.ap, [yy.buf], [], yy.buf)
                        if not last:
                            c.dma("pool", xb_d[r0:r0 + 128, :], yy.ap, [yy.buf], [], yy.buf)
                c.end_phase()

        for l in range(NL):
            mem_kv(l)
            for s in range(NSEG):
                phase_a(l, s)
                phase_b(l, s, l == NL - 1)
        if dbg:
            pass
        c.barrier()
    return nc


def _consts():
    cst = np.zeros((128, C_END), np.float32)
    i = np.arange(128)
    cst[:, C_ID:C_ID + 128] = np.eye(128, dtype=np.float32)
    cst[:, C_UT:C_UT + 128] = (i[None, :] >= i[:, None]).astype(np.float32)
    cst[:, C_SM:C_SM + 128] = (i[:, None] > i[None, :]).astype(np.float32)
    cst[:, C_NEGM:C_NEGM + 128] = np.where(i[None, :] > i[:, None], -30000.0, 0.0)
    cst[:, C_NEGMT:C_NEGMT + 128] = np.where(i[:, None] > i[None, :], -30000.0, 0.0)
    cst[:, C_ONES:C_ONES + 128] = 1.0
    for h in range(12):
        gam = 1.0 - 2.0 ** (-5.0 - h)
        cst[:, C_QS + h] = gam ** (i + 1.0)
        cst[:, C_KS + h] = gam ** (-(i + 1.0)) * 128.0 ** -0.5
    half = 64
    invf = (10000.0 ** (-np.arange(half, dtype=np.float32) / half)).astype(np.float32)
    cst[:, C_INVF:C_INVF + 64] = invf[None, :]
    return cst


def _kc_layout(w, ncols_pad=None):
    n = w.shape[1]
    a = np.ascontiguousarray(w.reshape(KC, 128, n).transpose(1, 0, 2))
    if ncols_pad is not None and ncols_pad != n:
        o = np.zeros((128, KC, ncols_pad), np.float32)
        o[:, :, :n] = a
        return o
    return a


def prep_weights(inp, kinds):
    ws = {"consts": _consts()}
    for l, kind in enumerate(kinds):
        j = l // 2
        wg = np.zeros((16, 128, KC, GW), np.float32)
        if kind == "ret":
            w = np.asarray(inp["w_in_ret"][j])
            for h in range(12):
                cols = np.concatenate([np.arange(h * 128, (h + 1) * 128), 1536 + np.arange(h * 128, (h + 1) * 128),
                                       3072 + np.arange(h * 256, (h + 1) * 256), 7168 + np.arange(h * 256, (h + 1) * 256)])
                wg[h] = _kc_layout(w[:, cols], GW)
            ws["rng%d" % l] = np.ascontiguousarray(inp["ret_norm_g"][j], dtype=np.float32)
        else:
            w = np.asarray(inp["w_in_gdn"][j])
            cwl = np.zeros((128, 192), np.float32)
            cw = np.asarray(inp["conv_w"][j])
            for g in range(12):
                chans = [g * 128, 1536 + g * 128, 3072 + (2 * g) * 128, 3072 + (2 * g + 1) * 128]
                cols = np.concatenate([np.arange(ch, ch + 128) for ch in chans] + [7168 + np.arange(g * 256, (g + 1) * 256),
                                      np.array([11264 + 2 * g, 11264 + 2 * g + 1, 11288 + 2 * g, 11288 + 2 * g + 1])])
                wg[g] = _kc_layout(w[:, cols], GW)
                for ct, ch in enumerate(chans):
                    for jj in range(4):
                        cwl[:, g * 16 + ct * 4 + jj] = cw[jj, ch:ch + 128]
            ws["cw%d" % l] = cwl
            ws["gng%d" % l] = np.ascontiguousarray(inp["gdn_norm_g"][j], dtype=np.float32)
            ws["alog%d" % l] = np.ascontiguousarray(inp["a_log"][j], dtype=np.float32)
            ws["dtb%d" % l] = np.ascontiguousarray(inp["dt_bias"][j], dtype=np.float32)
        for m in range(4):
            cols = np.concatenate([6144 + np.arange(m * 256, (m + 1) * 256), 7168 + 3072 + np.arange(m * 256, (m + 1) * 256)])
            wg[12 + m] = _kc_layout(w[:, cols], GW)
        ws["wg%d" % l] = wg
        wo = np.asarray(inp["w_out"][l])
        ws["wo%d" % l] = np.ascontiguousarray(wo.reshape(32, 128, 8, 256).transpose(2, 1, 0, 3))
        wkv = np.asarray(inp["w_mem_kv"][l])
        ws["wkv%d" % l] = np.ascontiguousarray(wkv.reshape(KC, 128, 4, 512).transpose(2, 1, 0, 3))
        ws["lng%d" % l] = np.ascontiguousarray(inp["ln_g"][l], dtype=np.float32)
        ws["lnb%d" % l] = np.ascontiguousarray(inp["ln_b"][l], dtype=np.float32)
    return ws


def core_map(ws, x_b, mem_b, pos_b):
    T = x_b.shape[0]
    m = dict(ws)
    m["x"] = np.ascontiguousarray(x_b, dtype=np.float32)
    m["mem"] = np.ascontiguousarray(mem_b, dtype=np.float32)
    m["pos"] = np.ascontiguousarray(np.asarray(pos_b, dtype=np.int32).reshape(T // 128, 128).T)
    return m


KINDS = ["ret", "gdn", "ret", "gdn"]


def kernel(x, mem, positions, w_in_ret, ret_norm_g, w_in_gdn, conv_w, a_log, dt_bias, gdn_norm_g,
           w_mem_kv, w_out, ln_g, ln_b):
    inp = dict(w_in_ret=w_in_ret, ret_norm_g=ret_norm_g, w_in_gdn=w_in_gdn, conv_w=conv_w, a_log=a_log, dt_bias=dt_bias,
               gdn_norm_g=gdn_norm_g, w_mem_kv=w_mem_kv, w_out=w_out, ln_g=ln_g, ln_b=ln_b)
    x = np.asarray(x); mem = np.asarray(mem); positions = np.asarray(positions)
    B, S, _ = x.shape
    ws = prep_weights(inp, KINDS)
    nc = build(2048, S // 2048, KINDS)
    in_maps = [core_map(ws, x[b % B], mem[b % B], positions[b % B]) for b in range(8)]
    res = run_bass_kernel_spmd(nc, in_maps, core_ids=list(range(8)))
    return np.stack([res.results[b]["out"] for b in range(B)], axis=0).astype(np.float32)
```

```python
import contextlib
import os
import numpy as np
GSTOP = float(os.environ.get('GSTOP', '9'))
import concourse.bass as bass
import concourse.mybir as mybir
from concourse.bass_utils import run_bass_kernel_spmd

F32 = mybir.dt.float32
BF16 = mybir.dt.bfloat16
I32 = mybir.dt.int32
AF = mybir.ActivationFunctionType
ALU = mybir.AluOpType
AX = mybir.AxisListType

D = 2048
KC = 16
NMEM = 256
DEPTH = 4
ALPHA = (2.0 * DEPTH) ** 0.25
LN_EPS = 1e-5
NORM_EPS = 1e-6
GW = 772
TWO_PI = float(2 * np.pi)
C_ID, C_UT, C_SM, C_NEGM, C_NEGMT, C_ONES, C_QS, C_KS, C_INVF, C_END = 0, 128, 256, 384, 512, 640, 768, 780, 792, 856


class Buf:
    __slots__ = ("name", "w", "r", "dsem", "excl")

    def __init__(self, name, excl=False):
        self.name = name
        self.w = None
        self.r = []
        self.dsem = None
        self.excl = excl


class Ctx:
    def __init__(self, nc, es):
        self.nc = nc
        self.es = es
        self.eng = {"pe": nc.tensor, "act": nc.scalar, "dve": nc.vector, "pool": nc.gpsimd, "sp": nc.sync}
        self.sem = {}
        self.cnt = {}
        for k in self.eng:
            self.sem[k] = es.enter_context(nc.semaphore("s_" + k))
            self.cnt[k] = 0
        self.waited = {k: {} for k in self.eng}
        self.semobj = {k: self.sem[k] for k in self.eng}
        self.latest = {k: 0 for k in self.eng}
        self.ndsem = 0
        self.free_dsems = []
        self.in_phase = False
        self.phase_keys = []

    def _wait(self, e, ev):
        if ev is None:
            return
        key, val = ev
        if key == e:
            if e == "pe":
                return
            if self.cnt[e] - val >= 2:
                return
        if key not in self.eng:
            val = max(val, self.latest.get(key, val))
        if self.waited[e].get(key, 0) >= val:
            return
        self.waited[e][key] = val
        self.eng[e].wait_ge(self.semobj[key], val)

    def deps(self, e, reads, writes):
        for b in reads:
            self._wait(e, b.w)
            if b.excl:
                for ev in b.r:
                    if ev[0] != e:
                        self._wait(e, ev)
        for b in writes:
            self._wait(e, b.w)
            for ev in b.r:
                self._wait(e, ev)

    def record(self, ev, reads, writes):
        for b in writes:
            b.w = ev
            b.r = []
        for b in reads:
            b.r = [x for x in b.r if x[0] != ev[0]] + [ev]

    def op(self, e, fn, reads=(), writes=(), inc=True):
        self.deps(e, reads, writes)
        ins = fn()
        if inc:
            self.cnt[e] += 1
            ins.then_inc(self.sem[e], 1)
            self.latest[e] = self.cnt[e]
            ev = (e, self.cnt[e])
        else:
            ev = (e, self.cnt[e] + 1)
        self.record(ev, reads, writes)
        return ins

    def dsem_of(self, buf):
        if buf.dsem is None:
            if self.free_dsems:
                key = self.free_dsems.pop()
            else:
                self.ndsem += 1
                s = self.es.enter_context(self.nc.semaphore("d%d" % self.ndsem))
                key = "d%d" % self.ndsem
                self.semobj[key] = s
                self.latest[key] = 0
            buf.dsem = key
            if self.in_phase:
                self.phase_keys.append(key)
        return buf.dsem

    def begin_phase(self):
        self.in_phase = True
        self.phase_keys = []

    def end_phase(self):
        self.barrier()
        self.free_dsems.extend(self.phase_keys)
        self.phase_keys = []
        self.in_phase = False

    def dma(self, q, out, in_, reads, writes, sembuf, transpose=False):
        self.deps(q, reads, writes)
        key = self.dsem_of(sembuf)
        if transpose:
            ins = self.eng[q].dma_start_transpose(out=out, in_=in_)
        else:
            ins = self.eng[q].dma_start(out=out, in_=in_)
        ins.then_inc(self.semobj[key], 16)
        self.latest[key] += 16
        self.record((key, self.latest[key]), reads, writes)
        return ins

    def barrier(self, engines=("pe", "act", "dve", "pool", "sp")):
        for e in engines:
            for key, val in self.latest.items():
                if val <= 0 or key == e:
                    continue
                if self.waited[e].get(key, 0) >= val:
                    continue
                self.waited[e][key] = val
                self.eng[e].wait_ge(self.semobj[key], val)


class Reg:
    __slots__ = ("ap", "buf")

    def __init__(self, ap, buf):
        self.ap = ap
        self.buf = buf


def build(SEGT, NSEG, kinds, dbg=False):
    NB = SEGT // 128
    T = SEGT * NSEG
    NBT = T // 128
    NL = len(kinds)
    nc = bass.Bass("TRN2", target_bir_lowering=False)
    dt = nc.dram_tensor
    x_d = dt("x", [T, D], F32, kind="ExternalInput").ap()
    mem_d = dt("mem", [NMEM, D], F32, kind="ExternalInput").ap()
    pos_d = dt("pos", [128, NBT], I32, kind="ExternalInput").ap()
    cst_d = dt("consts", [128, C_END], F32, kind="ExternalInput").ap()
    wg_d, wo_d, wkv_d, lng_d, lnb_d, p1_d, p2_d, p3_d, p4_d = [], [], [], [], [], [], [], [], []
    for l in range(NL):
        wg_d.append(dt("wg%d" % l, [16, 128, KC, GW], F32, kind="ExternalInput").ap())
        wo_d.append(dt("wo%d" % l, [8, 128, 32, 256], F32, kind="ExternalInput").ap())
        wkv_d.append(dt("wkv%d" % l, [4, 128, KC, 512], F32, kind="ExternalInput").ap())
        lng_d.append(dt("lng%d" % l, [D], F32, kind="ExternalInput").ap())
        lnb_d.append(dt("lnb%d" % l, [D], F32, kind="ExternalInput").ap())
        if kinds[l] == "ret":
            p1_d.append(dt("rng%d" % l, [3072], F32, kind="ExternalInput").ap())
            p2_d.append(None); p3_d.append(None); p4_d.append(None)
        else:
            p1_d.append(dt("gng%d" % l, [128], F32, kind="ExternalInput").ap())
            p2_d.append(dt("cw%d" % l, [128, 192], F32, kind="ExternalInput").ap())
            p3_d.append(dt("alog%d" % l, [24], F32, kind="ExternalInput").ap())
            p4_d.append(dt("dtb%d" % l, [24], F32, kind="ExternalInput").ap())
    out_d = dt("out", [T, D], F32, kind="ExternalOutput").ap()
    xres_d = dt("xres", [T, D], F32).ap()
    xb_d = dt("xb", [T, D], BF16).ap()
    br_d = dt("br", [T, 4096], BF16).ap()
    memb_d = dt("memb", [NMEM, D], BF16).ap()
    st_d = [dt("st%d" % l, [24, 128, 256], F32).ap() for l in range(NL)]
    wob_d = [dt("wob%d" % l, [8, 128, 32, 256], BF16).ap() for l in range(NL)]
    cst8_d = [dt("cvst%d" % l, [12, 128, 12], F32).ap() for l in range(NL)]
    if dbg:
        dbg_d = dt("dbg_br", [T, 4096], F32, kind="ExternalOutput").ap()

    with contextlib.ExitStack() as es:
        c = Ctx(nc, es)

        _uid = [0]

        def uname(name):
            _uid[0] += 1
            return "%s_u%d" % (name, _uid[0])

        def sbt(stack, name, shape, dtype):
            t = stack.enter_context(nc.sbuf_tensor(uname(name), shape, dtype))
            return t, Buf(name)

        def act(out, in_, func, reads, writes, **kw):
            return c.op("act", lambda: nc.scalar.activation(out=out, in_=in_, func=func, **kw), reads, writes)

        def tt(e, out, in0, in1, op, reads, writes):
            eng = c.eng[e]
            return c.op(e, lambda: eng.tensor_tensor(out=out, in0=in0, in1=in1, op=op), reads, writes)

        def ts(e, out, in0, s1, s2, op0, op1, reads, writes):
            eng = c.eng[e]
            if op1 is None:
                return c.op(e, lambda: eng.tensor_scalar(out=out, in0=in0, scalar1=s1, scalar2=None, op0=op0), reads, writes)
            return c.op(e, lambda: eng.tensor_scalar(out=out, in0=in0, scalar1=s1, scalar2=s2, op0=op0, op1=op1), reads, writes)

        def stt(e, out, in0, scalar, in1, op0, op1, reads, writes):
            eng = c.eng[e]
            return c.op(e, lambda: eng.scalar_tensor_tensor(out=out, in0=in0, scalar=scalar, in1=in1, op0=op0, op1=op1), reads, writes)

        def cp(e, out, in_, reads, writes):
            eng = c.eng[e]
            return c.op(e, lambda: eng.tensor_copy(out=out, in_=in_), reads, writes)

        def mm(out, pairs, reads, writes):
            n = len(pairs)
            for i, (l_, r_) in enumerate(pairs):
                last = i == n - 1
                c.op("pe", lambda: nc.tensor.matmul(out, l_, r_, start=(i == 0), stop=last),
                     reads if (last or i == 0) else (), writes if (last or i == 0) else (), inc=last)

        def trp(out, in_, ident, reads, writes):
            return c.op("pe", lambda: nc.tensor.transpose(out, in_, ident), reads, writes)

        cst, cstb = sbt(es, "cst", [128, C_END], F32)
        c.dma("sp", cst[:], cst_d, [], [cstb], cstb)
        idbf, idbfb = sbt(es, "idbf", [128, 128], BF16)
        cp("dve", idbf[:], cst[:, C_ID:C_ID + 128], [cstb], [idbfb])
        ID32 = cst[:, C_ID:C_ID + 128]
        UT32 = cst[:, C_UT:C_UT + 128]
        SM32 = cst[:, C_SM:C_SM + 128]
        ONES32 = cst[:, C_ONES:C_ONES + 128]
        onebf, onebfb = sbt(es, "onebf", [128, 128], BF16)
        cp("dve", onebf[:], ONES32, [cstb], [onebfb])
        negmbf, negmbfb = sbt(es, "negmbf", [128, 256], BF16)
        cp("dve", negmbf[:], cst[:, C_NEGM:C_NEGM + 256], [cstb], [negmbfb])

        dummy = Buf("dummy")
        wobD = Buf("wobD")
        for r0 in range(0, T, 512):
            c.dma("pool", xb_d[r0:r0 + 512, :], x_d[r0:r0 + 512, :], [], [], dummy)
        c.dma("pool", memb_d, mem_d, [], [], dummy)
        c.barrier(["sp", "pool"])
        memT, memTb = sbt(es, "memT", [128, KC, NMEM], BF16)
        for kc in range(KC):
            c.dma("sp", memT[:, kc, :], memb_d[:, kc * 128:(kc + 1) * 128], [], [memTb], memTb, transpose=True)
        mkT, mkTb = sbt(es, "mkT", [128, 8, NMEM], BF16)
        mv, mvb = sbt(es, "mv", [128, 2, 1024], BF16)

        has_ret = "ret" in kinds
        rot_d = dt("rot_d", [3, 128, NBT, 64], F32).ap()
        if has_ret:
            c.begin_phase()
            with contextlib.ExitStack() as ps:
                cosT, cosb = sbt(ps, "cosT0", [128, NBT, 64], F32)
                sinT, sinb = sbt(ps, "sinT0", [128, NBT, 64], F32)
                nsinT, nsinb = sbt(ps, "nsinT0", [128, NBT, 64], F32)
                pi_, pib = sbt(ps, "posi", [128, NBT], I32)
                pf_, pfb = sbt(ps, "posf", [128, NBT], F32)
                ang, angb = sbt(ps, "ang", [128, NBT, 64], F32)
                nf, nfb = sbt(ps, "nf", [128, NBT, 64], F32)
                ni, nib = sbt(ps, "ni", [128, NBT, 64], I32)
                c.dma("sp", pi_[:], pos_d, [], [pib], pib)
                cp("dve", pf_[:], pi_[:], [pib], [pfb])
                invf = cst[:, C_INVF:C_INVF + 64]
                for b in range(NBT):
                    ts("dve", ang[:, b, :], invf, pf_[:, b:b + 1], None, ALU.mult, None, [cstb, pfb], [angb])

                def reduce_and_sin(dst, dstb, shift):
                    ts("dve", nf[:], ang[:], shift, 1.0 / TWO_PI, ALU.add, ALU.mult, [angb], [nfb])
                    cp("dve", ni[:], nf[:], [nfb], [nib])
                    cp("dve", nf[:], ni[:], [nib], [nfb])
                    stt("dve", nf[:], nf[:], -TWO_PI, ang[:], ALU.mult, ALU.add, [nfb, angb], [nfb])
                    if shift != 0.0:
                        ts("dve", nf[:], nf[:], shift, None, ALU.add, None, [nfb], [nfb])
                    ni_f = ni[:].bitcast(F32)
                    ts("dve", ni_f, nf[:], float(np.pi), -TWO_PI, ALU.is_gt, ALU.mult, [nfb], [nib])
                    tt("dve", nf[:], nf[:], ni_f, ALU.add, [nfb, nib], [nfb])
                    ts("dve", ni_f, nf[:], -float(np.pi), TWO_PI, ALU.is_lt, ALU.mult, [nfb], [nib])
                    tt("dve", nf[:], nf[:], ni_f, ALU.add, [nfb, nib], [nfb])
                    act(dst[:], nf[:], AF.Sin, [nfb], [dstb])

                reduce_and_sin(sinT, sinb, 0.0)
                reduce_and_sin(cosT, cosb, float(np.pi / 2))
                ts("dve", nsinT[:], sinT[:], -1.0, None, ALU.mult, None, [sinb], [nsinb])
                c.dma("sp", rot_d[0], cosT[:], [cosb], [], cosb)
                c.dma("sp", rot_d[1], sinT[:], [sinb], [], sinb)
                c.dma("sp", rot_d[2], nsinT[:], [nsinb], [], nsinb)
                c.end_phase()
        c.barrier()

        def wtiles(stack, name, shape, dtype, n=2):
            return [Reg(*_mk(stack, "%s%d" % (name, i), shape, dtype)) for i in range(n)]

        def _mk(stack, name, shape, dtype):
            t, b = sbt(stack, name, shape, dtype)
            return t[:], b

        def load_bcast(stack, name, src, n, q="sp"):
            t, b = sbt(stack, name, [128, n], F32)
            c.dma(q, t[:], src.partition_broadcast(128), [], [b], b)
            return t, b

        def mem_kv(l):
            c.begin_phase()
            with contextlib.ExitStack() as ps:
                wk = wtiles(ps, "wkv", [128, KC, 512], BF16, 2)
                pk = [Reg(ps.enter_context(nc.psum_tensor(uname("pkv"), [128, 512], F32))[:], Buf("pkv%d" % i, excl=True)) for i in range(2)]
                it = 0
                for ch in range(4):
                    w = wk[ch % 2]
                    for q4 in range(4):
                        c.dma("pool", w.ap[:, q4 * 4:(q4 + 1) * 4, :], wkv_d[l][ch][:, q4 * 4:(q4 + 1) * 4, :], [], [w.buf], w.buf)
                    if ch < 2:
                        for ct in range(4):
                            p = pk[it % 2]; it += 1
                            mm(p.ap[:, 0:NMEM], [(w.ap[:, kc, ct * 128:(ct + 1) * 128], memT[:, kc, :]) for kc in range(KC)],
                               [w.buf, memTb], [p.buf])
                            act(mkT[:, ch * 4 + ct, :], p.ap[:, 0:NMEM], AF.Copy, [p.buf], [mkTb])
                    else:
                        for mc in range(2):
                            p = pk[it % 2]; it += 1
                            mm(p.ap[:, :], [(memT[:, kc, mc * 128:(mc + 1) * 128], w.ap[:, kc, :]) for kc in range(KC)],
                               [w.buf, memTb], [p.buf])
                            act(mv[:, mc, (ch - 2) * 512:(ch - 1) * 512], p.ap[:, :], AF.Copy, [p.buf], [mvb])
                c.end_phase()

        def phase_a(l, s):
            kind = kinds[l]
            tok0 = s * SEGT
            c.begin_phase()
            with contextlib.ExitStack() as ps:
                xT, xTb = sbt(ps, "xT", [128, KC, SEGT], BF16)
                for kc in range(KC):
                    for r0 in range(0, SEGT, 512):
                        r1 = min(SEGT, r0 + 512)
                        c.dma("sp", xT[:, kc, r0:r1], xb_d[tok0 + r0:tok0 + r1, kc * 128:(kc + 1) * 128], [], [xTb], xTb, transpose=True)
                Wt = wtiles(ps, "W", [128, KC, GW], BF16, 2)
                PS = [ps.enter_context(nc.psum_tensor(uname("ps"), [128, 512], F32)) for i in range(7)]
                PSB = ps.enter_context(nc.psum_tensor(uname("psb"), [128, 1024], BF16))
                _regs = {}
                _bankbuf = {}

                def R(bank, c0, c1):
                    k = (bank, c0, c1)
                    if k not in _regs:
                        if bank not in _bankbuf:
                            _bankbuf[bank] = Buf("psbank_%s" % bank, excl=True)
                        if bank == "b":
                            _regs[k] = Reg(PSB[:, c0:c1], _bankbuf[bank])
                        else:
                            _regs[k] = Reg(PS[bank][:, c0:c1], _bankbuf[bank])
                    return _regs[k]

                def load_w(g, ncols):
                    w = Wt[g % 2]
                    for q4 in range(4):
                        c.dma("pool", w.ap[:, q4 * 4:(q4 + 1) * 4, 0:ncols], wg_d[l][g][:, q4 * 4:(q4 + 1) * 4, 0:ncols], [], [w.buf], w.buf)
                    return w

                def store_branch(t, blk, col0, ncol):
                    r0 = tok0 + blk * 128
                    c.dma("sp", br_d[r0:r0 + 128, col0:col0 + ncol], t.ap, [t.buf], [], t.buf)

                gcols = [768] * 12 + [512] * 4 if kind == "ret" else [GW] * 12 + [512] * 4
                load_w(0, gcols[0])
                if s == 0:
                    for n8 in range(8):
                        c.dma("pool", wob_d[l][n8], wo_d[l][n8], [], [], wobD)

                st4 = wtiles(ps, "st4", [128, 16], F32, 2)
                e_t = wtiles(ps, "e_t", [128, 256], F32, 2)
                zs_t = wtiles(ps, "zs_t", [128, 256], F32, 2)
                osb = wtiles(ps, "osb", [128, 256], F32, 2)
                junk = wtiles(ps, "junk", [128, 256], BF16, 2)
                brt = wtiles(ps, "brt", [128, 256], BF16, 3)

                def gate_from_z(zreg, it, width=256):
                    e = e_t[it % 2]; zs = zs_t[it % 2]
                    act(e.ap[:, 0:width], zreg.ap, AF.Exp, [zreg.buf], [e.buf], scale=-1.0)
                    ts("dve", e.ap[:, 0:width], e.ap[:, 0:width], 1.0, None, ALU.add, None, [e.buf], [e.buf])
                    c.op("dve", lambda: nc.vector.reciprocal(out=e.ap[:, 0:width], in_=e.ap[:, 0:width]), [e.buf], [e.buf])
                    tt("dve", zs.ap[:, 0:width], zreg.ap, e.ap[:, 0:width], ALU.mult, [zreg.buf, e.buf], [zs.buf])
                    return zs

                def rstd_from(stt_, col_in, col_out, scale, eps):
                    act(stt_.ap[:, col_out:col_out + 1], stt_.ap[:, col_in:col_in + 1], AF.Ln, [stt_.buf, epsTb], [stt_.buf], scale=scale, bias=epsT[:, eps:eps + 1])
                    act(stt_.ap[:, col_out:col_out + 1], stt_.ap[:, col_out:col_out + 1], AF.Exp, [stt_.buf], [stt_.buf], scale=-0.5)

                epsT, epsTb = sbt(ps, "epsT", [128, 2], F32)
                c.op("dve", lambda: nc.vector.memset(epsT[:, 0:1], NORM_EPS), [], [epsTb])
                c.op("dve", lambda: nc.vector.memset(epsT[:, 1:2], LN_EPS), [], [epsTb])

                git = 0
                if kind == "ret":
                    gam, gamb = load_bcast(ps, "rng", p1_d[l], 3072)
                    b0 = tok0 // 128
                    cosT, cosb = sbt(ps, "cosT", [128, NB, 64], F32)
                    sinT, sinb = sbt(ps, "sinT", [128, NB, 64], F32)
                    nsinT, nsinb = sbt(ps, "nsinT", [128, NB, 64], F32)
                    c.dma("sp", cosT[:], rot_d[0][:, b0:b0 + NB, :], [], [cosb], cosb)
                    c.dma("sp", sinT[:], rot_d[1][:, b0:b0 + NB, :], [], [sinb], sinb)
                    c.dma("sp", nsinT[:], rot_d[2][:, b0:b0 + NB, :], [], [nsinb], nsinb)
                    qks = wtiles(ps, "qks", [128, 256], F32, 2)
                    vbf = wtiles(ps, "vbf", [128, 256], BF16, 2)
                    rA = wtiles(ps, "rA", [128, 256], F32, 2)
                    rB = wtiles(ps, "rB", [128, 256], F32, 2)
                    rot = wtiles(ps, "rot", [128, 256], BF16, 2)
                    qkT = wtiles(ps, "qkT", [128, 256], BF16, 2)
                    PT = wtiles(ps, "PT", [128, 128], BF16, 2)
                    Y, Yb = sbt(ps, "Y", [128, 256], F32)
                    Sbf = wtiles(ps, "Sbf", [128, 256], BF16, 2)
                    for h in range(12):
                        w = Wt[h % 2]
                        load_w(h + 1, gcols[h + 1])
                        g128 = float((1.0 - 2.0 ** (-5 - h)) ** 128)
                        have_state = s > 0
                        if have_state:
                            c.dma("sp", Y[:], st_d[l][h], [], [Yb], Yb)
                            sb0 = Sbf[0]
                            act(sb0.ap, Y[:], AF.Copy, [Yb], [sb0.buf], scale=g128)
                        git0 = git; git += NB
                        hs = [have_state]

                        def a_proj(blk):
                            it = git0 + blk
                            tok = slice(blk * 128, (blk + 1) * 128)
                            gb = blk
                            PA = R(it % 2, 0, 512)
                            PZ = R(2 + it % 2, 0, 256)
                            mm(PA.ap, [(xT[:, kc, tok], w.ap[:, kc, 0:512]) for kc in range(KC)], [xTb, w.buf], [PA.buf])
                            mm(PZ.ap, [(xT[:, kc, tok], w.ap[:, kc, 512:768]) for kc in range(KC)], [xTb, w.buf], [PZ.buf])

                        def a_rest(blk):
                            it = git0 + blk
                            tok = slice(blk * 128, (blk + 1) * 128)
                            gb = blk
                            PA = R(it % 2, 0, 512)
                            PZ = R(2 + it % 2, 0, 256)
                            qk = qks[it % 2]; v = vbf[it % 2]
                            act(qk.ap[:, 0:128], PA.ap[:, 0:128], AF.Copy, [PA.buf, cstb], [qk.buf], scale=cst[:, C_QS + h:C_QS + h + 1])
                            act(qk.ap[:, 128:256], PA.ap[:, 128:256], AF.Copy, [PA.buf, cstb], [qk.buf], scale=cst[:, C_KS + h:C_KS + h + 1])
                            act(v.ap, PA.ap[:, 256:512], AF.Copy, [PA.buf], [v.buf])
                            gate_from_z(PZ, it)
                            yield
                            a_ = rA[it % 2]; b_ = rB[it % 2]; ro = rot[it % 2]
                            qk4 = qk.ap.rearrange("p (a b d) -> p a b d", a=2, b=2)
                            a4 = a_.ap.rearrange("p (a b d) -> p a b d", a=2, b=2)
                            b4 = b_.ap.rearrange("p (a b d) -> p a b d", a=2, b=2)
                            cosb4 = cosT[:, gb, :].unsqueeze(1).unsqueeze(1).to_broadcast([128, 2, 2, 64])
                            sinb3 = sinT[:, gb, :].unsqueeze(1).to_broadcast([128, 2, 64])
                            nsinb3 = nsinT[:, gb, :].unsqueeze(1).to_broadcast([128, 2, 64])
                            tt("dve", a4, qk4, cosb4, ALU.mult, [qk.buf, cosb], [a_.buf])
                            tt("pool", b4[:, :, 0, :], qk4[:, :, 1, :], nsinb3, ALU.mult, [qk.buf, nsinb], [b_.buf])
                            tt("pool", b4[:, :, 1, :], qk4[:, :, 0, :], sinb3, ALU.mult, [qk.buf, sinb], [b_.buf])
                            yield
                            tt("dve", ro.ap, a_.ap, b_.ap, ALU.add, [a_.buf, b_.buf], [ro.buf])
                            yield
                            TR = R("b", 0, 256)
                            trp(TR.ap[:, 0:128], ro.ap[:, 0:128], idbf[:], [ro.buf, idbfb], [TR.buf])
                            trp(TR.ap[:, 128:256], ro.ap[:, 128:256], idbf[:], [ro.buf, idbfb], [TR.buf])
                            yield
                            qt = qkT[it % 2]
                            act(qt.ap, TR.ap, AF.Copy, [TR.buf], [qt.buf])
                            yield
                            SC = R(4, 0, 128)
                            mm(SC.ap, [(qt.ap[:, 128:256], qt.ap[:, 0:128])], [qt.buf], [SC.buf])
                            yield
                            pt = PT[it % 2]
                            tt("dve", pt.ap, SC.ap, UT32, ALU.mult, [SC.buf, cstb], [pt.buf])
                            yield

                        def b_part(blk):
                            it = git0 + blk
                            tok = slice(blk * 128, (blk + 1) * 128)
                            gb = blk
                            PA = R(it % 2, 0, 512)
                            PZ = R(2 + it % 2, 0, 256)
                            qk = qks[it % 2]; v = vbf[it % 2]
                            a_ = rA[it % 2]; b_ = rB[it % 2]; ro = rot[it % 2]
                            qt = qkT[it % 2]; pt = PT[it % 2]
                            O = R(5, 0, 256)
                            sprev = Sbf[blk % 2]
                            if hs[0]:
                                mm(O.ap, [(pt.ap, v.ap), (qt.ap[:, 0:128], sprev.ap)], [pt.buf, v.buf, qt.buf, sprev.buf], [O.buf])
                            else:
                                mm(O.ap, [(pt.ap, v.ap)], [pt.buf, v.buf], [O.buf])
                            KV = R(6, 0, 256)
                            mm(KV.ap, [(ro.ap[:, 128:256], v.ap)], [ro.buf, v.buf], [KV.buf])
                            yield
                            if hs[0]:
                                stt("dve", Y[:], Y[:], g128, KV.ap, ALU.mult, ALU.add, [Yb, KV.buf], [Yb])
                            else:
                                cp("dve", Y[:], KV.ap, [KV.buf], [Yb])
                            hs[0] = True
                            snext = Sbf[(blk + 1) % 2]
                            act(snext.ap, Y[:], AF.Copy, [Yb], [snext.buf], scale=g128)
                            st = st4[it % 2]; o_ = osb[it % 2]; jk = junk[it % 2]
                            act(o_.ap, O.ap, AF.Copy, [O.buf], [o_.buf, st.buf], accum_out=st.ap[:, 0:1])
                            act(jk.ap, O.ap, AF.Square, [O.buf], [jk.buf, st.buf], accum_out=st.ap[:, 1:2])
                            yield
                            ts("dve", st.ap[:, 2:3], st.ap[:, 0:1], 1.0 / 256, None, ALU.mult, None, [st.buf], [st.buf])
                            tt("dve", st.ap[:, 3:4], st.ap[:, 2:3], st.ap[:, 2:3], ALU.mult, [st.buf], [st.buf])
                            stt("dve", st.ap[:, 4:5], st.ap[:, 1:2], 1.0 / 256, st.ap[:, 3:4], ALU.mult, ALU.subtract, [st.buf], [st.buf])
                            yield
                            rstd_from(st, 4, 5, 1.0, 0)
                            yield
                            ts("dve", o_.ap, o_.ap, st.ap[:, 2:3], st.ap[:, 5:6], ALU.subtract, ALU.mult, [o_.buf, st.buf], [o_.buf])
                            zs = zs_t[it % 2]
                            yield
                            tt("pool", o_.ap, o_.ap, gam[:, h * 256:(h + 1) * 256], ALU.mult, [o_.buf, gamb], [o_.buf])
                            yield
                            bt = brt[it % 3]
                            tt("dve", bt.ap, o_.ap, zs.ap, ALU.mult, [o_.buf, zs.buf], [bt.buf])
                            store_branch(bt, blk, h * 256, 256)

                        def rr_run(gl):
                            alive = list(gl)
                            while alive:
                                nxt = []
                                for gen_ in alive:
                                    try:
                                        next(gen_)
                                        nxt.append(gen_)
                                    except StopIteration:
                                        pass
                                alive = nxt

                        a_proj(0)
                        rr_run([a_rest(0)])
                        for blk in range(NB):
                            if blk + 1 < NB:
                                a_proj(blk + 1)
                                rr_run([b_part(blk), a_rest(blk + 1)])
                            else:
                                rr_run([b_part(blk)])
                        c.dma("sp", st_d[l][h], Y[:], [Yb], [], Yb)
                else:
                    gdn_heads(ps, l, s, tok0, xT, xTb, Wt, load_w, gcols, R, store_branch, gate_from_z, rstd_from,
                              st4, e_t, zs_t, osb, junk, brt)
                    git = 1000
                mqT = wtiles(ps, "mqT", [128, 2, 128], BF16, 2)
                pbf = wtiles(ps, "pbf", [128, 256], BF16, 2)
                pTt = wtiles(ps, "pTt", [128, 256], BF16, 2)
                for m in range(4):
                    g = 12 + m
                    w = Wt[g % 2]
                    if g + 1 < 16:
                        load_w(g + 1, gcols[g + 1])
                    for blk in range(NB):
                        it = git; git += 1
                        tok = slice(blk * 128, (blk + 1) * 128)
                        PQ = R(it % 2, 0, 256)
                        PZ = R(2 + it % 2, 0, 256)
                        for hf in range(2):
                            mm(PQ.ap[:, hf * 128:(hf + 1) * 128], [(w.ap[:, kc, hf * 128:(hf + 1) * 128], xT[:, kc, tok]) for kc in range(KC)],
                               [xTb, w.buf], [PQ.buf])
                        mm(PZ.ap, [(xT[:, kc, tok], w.ap[:, kc, 256:512]) for kc in range(KC)], [xTb, w.buf], [PZ.buf])
                        mq = mqT[it % 2]
                        act(mq.ap.rearrange("p a b -> p (a b)"), PQ.ap, AF.Copy, [PQ.buf], [mq.buf])
                        SC = R(4, 0, 256)
                        mm(SC.ap, [(mq.ap[:, hf, :], mkT[:, m * 2 + hf, :]) for hf in range(2)], [mq.buf, mkTb], [SC.buf])
                        st = st4[it % 2]
                        c.op("dve", lambda: nc.vector.reduce_max(out=st.ap[:, 0:1], in_=SC.ap, axis=AX.X), [SC.buf], [st.buf])
                        ts("dve", st.ap[:, 1:2], st.ap[:, 0:1], -1.0 / 16, None, ALU.mult, None, [st.buf], [st.buf])
                        pb_ = pbf[it % 2]
                        act(pb_.ap, SC.ap, AF.Exp, [SC.buf, st.buf], [pb_.buf, st.buf], scale=1.0 / 16, bias=st.ap[:, 1:2], accum_out=st.ap[:, 2:3])
                        TR = R("b", 0, 256)
                        trp(TR.ap[:, 0:128], pb_.ap[:, 0:128], idbf[:], [pb_.buf, idbfb], [TR.buf])
                        trp(TR.ap[:, 128:256], pb_.ap[:, 128:256], idbf[:], [pb_.buf, idbfb], [TR.buf])
                        pT_ = pTt[it % 2]
                        cp("dve", pT_.ap, TR.ap, [TR.buf], [pT_.buf])
                        O = R(5, 0, 256)
                        mm(O.ap, [(pT_.ap[:, mc * 128:(mc + 1) * 128], mv[:, mc, m * 256:(m + 1) * 256]) for mc in range(2)],
                           [pT_.buf, mvb], [O.buf])
                        c.op("dve", lambda: nc.vector.reciprocal(out=st.ap[:, 3:4], in_=st.ap[:, 2:3]), [st.buf], [st.buf])
                        zs = gate_from_z(PZ, it)
                        bt = brt[it % 3]
                        stt("dve", bt.ap, O.ap, st.ap[:, 3:4], zs.ap, ALU.mult, ALU.mult, [O.buf, st.buf, zs.buf], [bt.buf])
                        store_branch(bt, blk, 3072 + m * 256, 256)
                c.end_phase()

        def gdn_heads(ps, l, s, tok0, xT, xTb, Wt, load_w, gcols, R, store_branch, gate_from_z, rstd_from,
                      st4, e_t, zs_t, osb, junk, brt):
            TT = min(512, SEGT)
            NBK = TT // 128
            cwt, cwtb = sbt(ps, "cwt", [128, 192], F32)
            c.dma("sp", cwt[:], p2_d[l], [], [cwtb], cwtb)
            negA, negAb = load_bcast(ps, "negA", p3_d[l], 24)
            act(negA[:], negA[:], AF.Exp, [negAb], [negAb])
            ts("dve", negA[:], negA[:], -1.0, None, ALU.mult, None, [negAb], [negAb])
            dtb, dtbb = load_bcast(ps, "dtb", p4_d[l], 24)
            gg, ggb = sbt(ps, "gg", [128, 256], F32)
            c.dma("sp", gg[:, 0:128], p1_d[l].partition_broadcast(128), [], [ggb], ggb)
            c.dma("sp", gg[:, 128:256], p1_d[l].partition_broadcast(128), [], [ggb], ggb)
            epsG, epsGb = sbt(ps, "epsG", [128, 1], F32)
            c.op("dve", lambda: nc.vector.memset(epsG[:], NORM_EPS), [], [epsGb])
            hraw, hrawb = sbt(ps, "hraw", [128, 4, 3 + TT], F32)
            acc, accb = sbt(ps, "acc", [128, 2, TT], F32)
            etmp, etmpb = sbt(ps, "etmp", [128, TT], F32)
            cvo, cvob = sbt(ps, "cvo", [128, 4, TT], BF16)
            qsq, qsqb = sbt(ps, "qsq", [128, 2, TT], BF16)
            S32, S32b = sbt(ps, "S32", [128, 2, 128], F32)
            Sbf = wtiles(ps, "gSbf", [128, 2, 128], BF16, 2)
            scal = wtiles(ps, "scal", [128, 32], F32, 4)
            gt_ = wtiles(ps, "gt", [128, 2], F32, 4)
            ez_ = wtiles(ps, "ez", [128, 256], F32, 2)
            zsb_ = wtiles(ps, "zsb", [128, 256], F32, 4)
            Gbt = wtiles(ps, "Gbt", [128, 128], F32, 2)
            ngbt = wtiles(ps, "ngbt", [128, 128], F32, 2)
            gbtt = wtiles(ps, "gbtt", [128, 128], F32, 2)
            Dm = wtiles(ps, "Dm", [128, 2, 128], F32, 2)
            DTm = wtiles(ps, "DTm", [128, 2, 128], F32, 2)
            DSm = wtiles(ps, "DSm", [128, 2, 128], F32, 2)
            khat_ = wtiles(ps, "khat", [128, 128], BF16, 2)
            khT_ = wtiles(ps, "khT", [128, 128], BF16, 4)
            khT2_ = wtiles(ps, "khT2", [128, 128], BF16, 2)
            vb_ = wtiles(ps, "vb", [128, 2, 128], F32, 4)
            ktl_ = wtiles(ps, "ktl", [128, 2, 128], BF16, 4)
            NLt = wtiles(ps, "NLt", [128, 2, 128], F32, 2)
            NP = wtiles(ps, "NP", [128, 2, 256], F32, 2)
            QKD = wtiles(ps, "QKD", [128, 2, 128], BF16, 4)
            TTt = wtiles(ps, "TTt", [128, 2, 128], BF16, 4)
            r_ = wtiles(ps, "r_", [128, 2, 128], BF16, 2)
            qSs = wtiles(ps, "qSs", [128, 2, 128], F32, 2)
            vn_ = wtiles(ps, "vn", [128, 2, 128], BF16, 2)
            ones_row = cst[0:1, C_ONES:C_ONES + 128]
            git = 0
            for g in range(12):
                w = Wt[g % 2]
                load_w(g + 1, gcols[g + 1])
                if s > 0:
                    for h in range(2):
                        c.dma("sp", S32[:, h, :], st_d[l][2 * g + h][:, 0:128], [], [S32b], S32b)
                    c.dma("sp", hraw[:, :, 0:3], cst8_d[l][g].rearrange("p (a b) -> p a b", a=4), [], [hrawb], hrawb)
                else:
                    c.op("dve", lambda: nc.vector.memset(S32[:], 0.0), [], [S32b])
                    c.op("dve", lambda: nc.vector.memset(hraw[:, :, 0:3], 0.0), [], [hrawb])
                cp("dve", Sbf[0].ap, S32[:], [S32b], [Sbf[0].buf])
                sidx = 0
                for tt_i in range(SEGT // TT):
                    t0 = tt_i * TT
                    for ct in range(4):
                        PJ = R(1, 0, TT)
                        mm(PJ.ap, [(w.ap[:, kc, ct * 128:(ct + 1) * 128], xT[:, kc, t0:t0 + TT]) for kc in range(KC)], [xTb, w.buf], [PJ.buf])
                        act(hraw[:, ct, 3:3 + TT], PJ.ap, AF.Copy, [PJ.buf], [hrawb])
                        cb = g * 16 + ct * 4
                        ts("dve", acc[:, ct % 2, :], hraw[:, ct, 0:TT], cwt[:, cb:cb + 1], None, ALU.mult, None, [hrawb, cwtb], [accb])
                        for jj in range(1, 4):
                            stt("dve", acc[:, ct % 2, :], hraw[:, ct, jj:jj + TT], cwt[:, cb + jj:cb + jj + 1], acc[:, ct % 2, :], ALU.mult, ALU.add,
                                [hrawb, cwtb, accb], [accb])
                        act(etmp[:], acc[:, ct % 2, :], AF.Exp, [accb], [etmpb], scale=-1.0)
                        ts("dve", etmp[:], etmp[:], 1.0, None, ALU.add, None, [etmpb], [etmpb])
                        c.op("dve", lambda: nc.vector.reciprocal(out=etmp[:], in_=etmp[:]), [etmpb], [etmpb])
                        tt("dve", cvo[:, ct, :], acc[:, ct % 2, :], etmp[:], ALU.mult, [accb, etmpb], [cvob])
                        if ct < 2:
                            act(qsq[:, ct, :], cvo[:, ct, :], AF.Square, [cvob], [qsqb])
                    cp("dve", hraw[:, :, 0:3], hraw[:, :, TT:TT + 3], [hrawb], [hrawb])
                    def pre(bk, tt_i=tt_i, t0=t0):
                        blk = tt_i * NBK + bk
                        tb = slice(bk * 128, (bk + 1) * 128)
                        tokb = slice(t0 + bk * 128, t0 + (bk + 1) * 128)
                        par = bk % 2
                        PZ = R(par, 0, 260)
                        mm(PZ.ap, [(xT[:, kc, tokb], w.ap[:, kc, 512:772]) for kc in range(KC)], [xTb, w.buf], [PZ.buf])
                        TRK = R("b", par * 512, par * 512 + 384)
                        for i3 in range(3):
                            trp(TRK.ap[:, i3 * 128:(i3 + 1) * 128], cvo[:, 1 + i3, tb], idbf[:], [cvob, idbfb], [TRK.buf])
                        SM = R(par, 264, 272)
                        mm(SM.ap[:, 0:1], [(qsq[:, 0, tb], onebf[:, 0:1])], [qsqb, onebfb], [SM.buf])
                        mm(SM.ap[:, 1:2], [(qsq[:, 1, tb], onebf[:, 0:1])], [qsqb, onebfb], [SM.buf])
                        yield
                        sc = scal[bk]; gt = gt_[bk]
                        act(sc.ap[:, 0:2], SM.ap[:, 0:2], AF.Ln, [SM.buf, epsGb], [sc.buf], bias=epsG[:, 0:1])
                        act(sc.ap[:, 0:2], sc.ap[:, 0:2], AF.Exp, [sc.buf], [sc.buf], scale=-0.5)
                        tt("dve", sc.ap[:, 2:4], PZ.ap[:, 256:258], dtb[:, 2 * g:2 * g + 2], ALU.add, [PZ.buf, dtbb], [sc.buf])
                        act(sc.ap[:, 2:4], sc.ap[:, 2:4], AF.Exp, [sc.buf], [sc.buf])
                        act(sc.ap[:, 2:4], sc.ap[:, 2:4], AF.Ln, [sc.buf], [sc.buf], bias=1.0)
                        tt("dve", gt.ap, sc.ap[:, 2:4], negA[:, 2 * g:2 * g + 2], ALU.mult, [sc.buf, negAb], [gt.buf])
                        act(sc.ap[:, 4:6], PZ.ap[:, 258:260], AF.Exp, [PZ.buf], [sc.buf], scale=-1.0)
                        ts("dve", sc.ap[:, 4:6], sc.ap[:, 4:6], 1.0, None, ALU.add, None, [sc.buf], [sc.buf])
                        c.op("dve", lambda: nc.vector.reciprocal(out=sc.ap[:, 4:6], in_=sc.ap[:, 4:6]), [sc.buf], [sc.buf])
                        e = ez_[bk % 2]; zs = zsb_[bk]
                        act(e.ap, PZ.ap[:, 0:256], AF.Exp, [PZ.buf], [e.buf], scale=-1.0)
                        ts("dve", e.ap, e.ap, 1.0, None, ALU.add, None, [e.buf], [e.buf])
                        c.op("dve", lambda: nc.vector.reciprocal(out=e.ap, in_=e.ap), [e.buf], [e.buf])
                        tt("dve", zs.ap, PZ.ap[:, 0:256], e.ap, ALU.mult, [PZ.buf, e.buf], [zs.buf])
                        tt("pool", zs.ap, zs.ap, gg[:], ALU.mult, [zs.buf, ggb], [zs.buf])
                        yield
                        mm(SM.ap[:, 2:4], [(UT32, gt.ap)], [cstb, gt.buf], [SM.buf])
                        mm(SM.ap[:, 4:6], [(ONES32, gt.ap)], [cstb, gt.buf], [SM.buf])
                        yield
                        act(sc.ap[:, 20:24], SM.ap[:, 2:6], AF.Copy, [SM.buf], [sc.buf])
                        ts("dve", sc.ap[:, 24:26], sc.ap[:, 20:22], -1.0, None, ALU.mult, None, [sc.buf], [sc.buf])
                        D_ = Dm[bk % 2]; DT_ = DTm[bk % 2]; DS_ = DSm[bk % 2]
                        Gbs = [Gbt[h] for h in range(2)]
                        for h in range(2):
                            ts("dve", Gbs[h].ap, ONES32, gt.ap[:, h:h + 1], None, ALU.mult, None, [cstb, gt.buf], [Gbs[h].buf])
                        GCBs = [R(2, (2 * par + h) * 128, (2 * par + h + 1) * 128) for h in range(2)]
                        for h in range(2):
                            mm(GCBs[h].ap, [(Gbs[h].ap, UT32)], [Gbs[h].buf, cstb], [GCBs[h].buf])
                        yield
                        for h in range(2):
                            ngb = ngbt[h]; gbt = gbtt[h]
                            stt("dve", ngb.ap, GCBs[h].ap, -1.0, cst[:, C_NEGM:C_NEGM + 128], ALU.mult, ALU.add, [GCBs[h].buf, cstb], [ngb.buf])
                            tt("dve", gbt.ap, GCBs[h].ap, cst[:, C_NEGMT:C_NEGMT + 128], ALU.add, [GCBs[h].buf, cstb], [gbt.buf])
                            act(D_.ap[:, h, :], ngb.ap, AF.Exp, [ngb.buf, sc.buf], [D_.buf], bias=sc.ap[:, 20 + h:21 + h])
                            act(DT_.ap[:, h, :], gbt.ap, AF.Exp, [gbt.buf, sc.buf], [DT_.buf], bias=sc.ap[:, 24 + h:25 + h])
                            tt("pool", DS_.ap[:, h, :], D_.ap[:, h, :], SM32, ALU.mult, [D_.buf, cstb], [DS_.buf])
                        kh = khat_[bk % 2]; khT = khT_[bk]; khT2 = khT2_[bk % 2]
                        act(kh.ap, TRK.ap[:, 0:128], AF.Copy, [TRK.buf, sc.buf], [kh.buf], scale=sc.ap[:, 1:2])
                        yield
                        TK2 = R("b", par * 512 + 384, par * 512 + 512)
                        trp(TK2.ap, kh.ap, idbf[:], [kh.buf, idbfb], [TK2.buf])
                        yield
                        act(khT.ap, TK2.ap, AF.Copy, [TK2.buf], [khT.buf])
                        cp("dve", khT2.ap, TK2.ap, [TK2.buf], [khT2.buf])
                        vb = vb_[bk]; ktl = ktl_[bk]
                        for h in range(2):
                            act(vb.ap[:, h, :], TRK.ap[:, 128 + h * 128:256 + h * 128], AF.Copy, [TRK.buf, sc.buf], [vb.buf], scale=sc.ap[:, 4 + h:5 + h])
                        tt("dve", sc.ap[:, 6:8], sc.ap[:, 22:24], sc.ap[:, 20:22], ALU.subtract, [sc.buf], [sc.buf])
                        act(sc.ap[:, 6:8], sc.ap[:, 6:8], AF.Exp, [sc.buf], [sc.buf])
                        act(sc.ap[:, 8:12], sc.ap[:, 20:24], AF.Exp, [sc.buf], [sc.buf])
                        stt("dve", sc.ap[:, 12:14], sc.ap[:, 4:6], -1.0, sc.ap[:, 8:10], ALU.mult, ALU.mult, [sc.buf], [sc.buf])
                        ts("dve", sc.ap[:, 14:15], sc.ap[:, 0:1], 128.0 ** -0.5, None, ALU.mult, None, [sc.buf], [sc.buf])
                        ts("dve", sc.ap[:, 16:18], sc.ap[:, 8:10], sc.ap[:, 14:15], None, ALU.mult, None, [sc.buf], [sc.buf])
                        ts("dve", sc.ap[:, 18:20], sc.ap[:, 4:6], -1.0, None, ALU.mult, None, [sc.buf], [sc.buf])
                        for h in range(2):
                            ts("dve", ktl.ap[:, h, :], kh.ap, sc.ap[:, 6 + h:7 + h], None, ALU.mult, None, [kh.buf, sc.buf], [ktl.buf])
                        yield
                        KK = R(2, par * 256, par * 256 + 128); QKT = R(2, par * 256 + 128, par * 256 + 256)
                        mm(KK.ap, [(khT.ap, khT2.ap)], [khT.buf, khT2.buf], [KK.buf])
                        mm(QKT.ap, [(khT.ap, cvo[:, 0, tb])], [khT.buf, cvob], [QKT.buf])
                        yield
                        NL = NLt[bk % 2]; np_ = NP[bk % 2]; qkd = QKD[bk]; TTm = TTt[bk]
                        for h in range(2):
                            stt("dve", NL.ap[:, h, :], KK.ap, sc.ap[:, 18 + h:19 + h], DS_.ap[:, h, :], ALU.mult, ALU.mult, [KK.buf, sc.buf, DS_.buf], [NL.buf])
                            tt("dve", qkd.ap[:, h, :], QKT.ap, DT_.ap[:, h, :], ALU.mult, [QKT.buf, DT_.buf], [qkd.buf])
                        yield
                        DBs = [R(3 + 2 * par + h, 0, 256) for h in range(2)]
                        TRF = [R(3 + 2 * par + h, 256, 384) for h in range(2)]
                        for h in range(2):
                            trp(TRF[h].ap, NL.ap[:, h, :], ID32, [NL.buf, cstb], [TRF[h].buf])
                        yield
                        for h in range(2):
                            act(np_.ap[:, h, 0:128], TRF[h].ap, AF.Copy, [TRF[h].buf], [np_.buf])
                            tt("dve", np_.ap[:, h, 128:256], TRF[h].ap, ID32, ALU.add, [TRF[h].buf, cstb], [np_.buf])
                        yield
                        for h in range(2):
                            mm(DBs[h].ap[:, 0:128], [(NL.ap[:, h, :], np_.ap[:, h, 0:128])], [NL.buf, np_.buf], [DBs[h].buf])
                        yield
                        for h in range(2):
                            act(np_.ap[:, h, 0:128], DBs[h].ap[:, 0:128], AF.Copy, [DBs[h].buf], [np_.buf])
                        yield
                        for h in range(2):
                            trp(TRF[h].ap, np_.ap[:, h, 0:128], ID32, [np_.buf, cstb], [TRF[h].buf])
                        yield
                        for h in range(2):
                            act(NL.ap[:, h, :], TRF[h].ap, AF.Copy, [TRF[h].buf], [NL.buf])
                        yield
                        for lev in range(5):
                            for h in range(2):
                                mm(DBs[h].ap, [(NL.ap[:, h, :], np_.ap[:, h, :])], [NL.buf, np_.buf], [DBs[h].buf])
                            yield
                            for h in range(2):
                                act(np_.ap[:, h, 0:128], DBs[h].ap[:, 0:128], AF.Copy, [DBs[h].buf], [np_.buf])
                                tt("dve", np_.ap[:, h, 128:256], np_.ap[:, h, 128:256], DBs[h].ap[:, 128:256], ALU.add, [DBs[h].buf, np_.buf], [np_.buf])
                            yield
                            for h in range(2):
                                trp(TRF[h].ap, np_.ap[:, h, 0:128], ID32, [np_.buf, cstb], [TRF[h].buf])
                            yield
                            for h in range(2):
                                act(NL.ap[:, h, :], TRF[h].ap, AF.Copy, [TRF[h].buf], [NL.buf])
                            yield
                        for h in range(2):
                            mm(DBs[h].ap[:, 0:128], [(NL.ap[:, h, :], np_.ap[:, h, 128:256])], [NL.buf, np_.buf], [DBs[h].buf])
                        yield
                        for h in range(2):
                            tt("dve", TTm.ap[:, h, :], np_.ap[:, h, 128:256], DBs[h].ap[:, 0:128], ALU.add, [DBs[h].buf, np_.buf], [TTm.buf])

                    for bk0 in range(0, NBK, 2):
                        alive = [pre(bk) for bk in range(bk0, min(NBK, bk0 + 2))]
                        while alive:
                            nxt = []
                            for gen_ in alive:
                                try:
                                    next(gen_)
                                    nxt.append(gen_)
                                except StopIteration:
                                    pass
                            alive = nxt
                    for bk in range(NBK):
                        it = git; git += 1
                        blk = tt_i * NBK + bk
                        tb = slice(bk * 128, (bk + 1) * 128)
                        sc = scal[bk]; khT = khT_[bk]; vb = vb_[bk]; ktl = ktl_[bk]; qkd = QKD[bk]; TTm = TTt[bk]; zs = zsb_[bk]
                        sprev = Sbf[sidx % 2]; snext = Sbf[(sidx + 1) % 2]; sidx += 1
                        rr = r_[it % 2]; qs_ = qSs[it % 2]; vn = vn_[it % 2]; o_ = osb[it % 2]; st = st4[it % 2]; jk = junk[it % 2]
                        for h in range(2):
                            kS = R(3 + h, 0, 128)
                            qS = R(3 + h, 128, 256)
                            mm(kS.ap, [(khT.ap, sprev.ap[:, h, :])], [khT.buf, sprev.buf], [kS.buf])
                            mm(qS.ap, [(cvo[:, 0, tb], sprev.ap[:, h, :])], [cvob, sprev.buf], [qS.buf])
                        for h in range(2):
                            kS = R(3 + h, 0, 128)
                            qS = R(3 + h, 128, 256)
                            stt("dve", rr.ap[:, h, :], kS.ap, sc.ap[:, 12 + h:13 + h], vb.ap[:, h, :], ALU.mult, ALU.add, [kS.buf, sc.buf, vb.buf], [rr.buf])
                            act(qs_.ap[:, h, :], qS.ap, AF.Copy, [qS.buf, sc.buf], [qs_.buf], scale=sc.ap[:, 16 + h:17 + h])
                        for h in range(2):
                            VN = R(5 + h, 0, 128)
                            mm(VN.ap, [(TTm.ap[:, h, :], rr.ap[:, h, :])], [TTm.buf, rr.buf], [VN.buf])
                        for h in range(2):
                            VN = R(5 + h, 0, 128)
                            act(vn.ap[:, h, :], VN.ap, AF.Copy, [VN.buf], [vn.buf])
                        for h in range(2):
                            O2 = R(5 + h, 128, 256)
                            KVr = R(5 + h, 256, 384)
                            mm(O2.ap, [(qkd.ap[:, h, :], vn.ap[:, h, :])], [qkd.buf, vn.buf], [O2.buf])
                            mm(KVr.ap, [(ktl.ap[:, h, :], vn.ap[:, h, :])], [ktl.buf, vn.buf], [KVr.buf])
                        for h in range(2):
                            O2 = R(5 + h, 128, 256)
                            KVr = R(5 + h, 256, 384)
                            stt("dve", S32[:, h, :], S32[:, h, :], sc.ap[:, 10 + h:11 + h], KVr.ap, ALU.mult, ALU.add, [S32b, sc.buf, KVr.buf], [S32b])
                            act(snext.ap[:, h, :], S32[:, h, :], AF.Copy, [S32b], [snext.buf])
                            stt("dve", o_.ap[:, h * 128:(h + 1) * 128], O2.ap, sc.ap[:, 14:15], qs_.ap[:, h, :], ALU.mult, ALU.add,
                                [O2.buf, sc.buf, qs_.buf], [o_.buf])
                            act(jk.ap[:, h * 128:(h + 1) * 128], o_.ap[:, h * 128:(h + 1) * 128], AF.Square, [o_.buf], [jk.buf, st.buf], accum_out=st.ap[:, h:h + 1])
                        act(st.ap[:, 2:4], st.ap[:, 0:2], AF.Ln, [st.buf, epsGb], [st.buf], scale=1.0 / 128, bias=epsG[:, 0:1])
                        act(st.ap[:, 2:4], st.ap[:, 2:4], AF.Exp, [st.buf], [st.buf], scale=-0.5)
                        bt = brt[it % 3]
                        for h in range(2):
                            stt("dve", bt.ap[:, h * 128:(h + 1) * 128], o_.ap[:, h * 128:(h + 1) * 128], st.ap[:, 2 + h:3 + h], zs.ap[:, h * 128:(h + 1) * 128],
                                ALU.mult, ALU.mult, [o_.buf, st.buf, zs.buf], [bt.buf])
                        store_branch(bt, blk, g * 256, 256)
                for h in range(2):
                    c.dma("sp", st_d[l][2 * g + h][:, 0:128], S32[:, h, :], [S32b], [], S32b)
                c.dma("sp", cst8_d[l][g].rearrange("p (a b) -> p a b", a=4), hraw[:, :, 0:3], [hrawb], [], hrawb)

        def phase_b(l, s, last):
            tok0 = s * SEGT
            TT = min(512, SEGT)
            NBK = TT // 128
            c.begin_phase()
            with contextlib.ExitStack() as ps:
                brT = wtiles(ps, "brT", [128, 32, TT], BF16, 2)
                wo = wtiles(ps, "wo", [128, 32, 256], BF16, 2)
                xr = wtiles(ps, "xr", [128, D], F32, NBK)
                y = xr
                jk, jkb = sbt(ps, "jkB", [128, D], F32)
                lng, lngb = load_bcast(ps, "lng", lng_d[l], D)
                lnb, lnbb = load_bcast(ps, "lnb", lnb_d[l], D)
                st = wtiles(ps, "stB", [128, 8], F32, 2)
                epsT, epsTb = sbt(ps, "epsB", [128, 1], F32)
                c.op("dve", lambda: nc.vector.memset(epsT[:], LN_EPS), [], [epsTb])
                PS = [Reg(ps.enter_context(nc.psum_tensor(uname("pB"), [128, 512], F32))[:], Buf("pB%d" % i, excl=True)) for i in range(8)]
                src = x_d if l == 0 else xres_d
                dst = out_d if last else xres_d
                pit = 0
                wit = 0
                for tt_i in range(SEGT // TT):
                    t0 = tok0 + tt_i * TT
                    bT = brT[tt_i % 2]
                    for fc in range(32):
                        c.dma("sp", bT.ap[:, fc, :], br_d[t0:t0 + TT, fc * 128:(fc + 1) * 128], [], [bT.buf], bT.buf, transpose=True)
                    for bk in range(NBK):
                        c.dma("sp", xr[bk].ap, src[t0 + bk * 128:t0 + (bk + 1) * 128, :], [], [xr[bk].buf], xr[bk].buf)
                    for n in range(8):
                        w = wo[wit % 2]; wit += 1
                        for q4 in range(2):
                            c.dma("sp", w.ap[:, q4 * 16:(q4 + 1) * 16, :], wob_d[l][n][:, q4 * 16:(q4 + 1) * 16, :], [], [w.buf], w.buf)
                        for bk in range(NBK):
                            p = PS[pit % 8]; pit += 1
                            mm(p.ap[:, 0:256], [(bT.ap[:, fc, bk * 128:(bk + 1) * 128], w.ap[:, fc, :]) for fc in range(32)],
                               [bT.buf, w.buf], [p.buf])
                            stt("dve", y[bk].ap[:, n * 256:(n + 1) * 256], xr[bk].ap[:, n * 256:(n + 1) * 256], ALPHA, p.ap[:, 0:256],
                                ALU.mult, ALU.add, [xr[bk].buf, p.buf], [y[bk].buf])
                    for bk in range(NBK):
                        s_ = st[bk % 2]; yy = y[bk]
                        act(jk[:], yy.ap, AF.Copy, [yy.buf], [jkb, s_.buf], accum_out=s_.ap[:, 0:1])
                        act(jk[:], yy.ap, AF.Square, [yy.buf], [jkb, s_.buf], accum_out=s_.ap[:, 1:2])
                        ts("dve", s_.ap[:, 2:3], s_.ap[:, 0:1], 1.0 / D, None, ALU.mult, None, [s_.buf], [s_.buf])
                        tt("dve", s_.ap[:, 3:4], s_.ap[:, 2:3], s_.ap[:, 2:3], ALU.mult, [s_.buf], [s_.buf])
                        stt("dve", s_.ap[:, 4:5], s_.ap[:, 1:2], 1.0 / D, s_.ap[:, 3:4], ALU.mult, ALU.subtract, [s_.buf], [s_.buf])
                        act(s_.ap[:, 5:6], s_.ap[:, 4:5], AF.Ln, [s_.buf, epsTb], [s_.buf], bias=epsT[:, 0:1])
                        act(s_.ap[:, 5:6], s_.ap[:, 5:6], AF.Exp, [s_.buf], [s_.buf], scale=-0.5)
                        ts("dve", yy.ap, yy.ap, s_.ap[:, 2:3], s_.ap[:, 5:6], ALU.subtract, ALU.mult, [yy.buf, s_.buf], [yy.buf])
                        tt("pool", yy.ap, yy.ap, lng[:], ALU.mult, [yy.buf, lngb], [yy.buf])
                        tt("pool", yy.ap, yy.ap, lnb[:], ALU.add, [yy.buf, lnbb], [yy.buf])
                        r0 = t0 + bk * 128
                        c.dma("sp", dst[r0:r0 + 128, :], yy.ap, [yy.buf], [], yy.buf)
                        if not last:
                            c.dma("pool", xb_d[r0:r0 + 128, :], yy.ap, [yy.buf], [], yy.buf)
                c.end_phase()

        for l in range(NL):
            mem_kv(l)
            for s in range(NSEG):
                phase_a(l, s)
                phase_b(l, s, l == NL - 1)
        if dbg:
            pass
        c.barrier()
    return nc


def _consts():
    cst = np.zeros((128, C_END), np.float32)
    i = np.arange(128)
    cst[:, C_ID:C_ID + 128] = np.eye(128, dtype=np.float32)
    cst[:, C_UT:C_UT + 128] = (i[None, :] >= i[:, None]).astype(np.float32)
    cst[:, C_SM:C_SM + 128] = (i[:, None] > i[None, :]).astype(np.float32)
    cst[:, C_NEGM:C_NEGM + 128] = np.where(i[None, :] > i[:, None], -30000.0, 0.0)
    cst[:, C_NEGMT:C_NEGMT + 128] = np.where(i[:, None] > i[None, :], -30000.0, 0.0)
    cst[:, C_ONES:C_ONES + 128] = 1.0
    for h in range(12):
        gam = 1.0 - 2.0 ** (-5.0 - h)
        cst[:, C_QS + h] = gam ** (i + 1.0)
        cst[:, C_KS + h] = gam ** (-(i + 1.0)) * 128.0 ** -0.5
    half = 64
    invf = (10000.0 ** (-np.arange(half, dtype=np.float32) / half)).astype(np.float32)
    cst[:, C_INVF:C_INVF + 64] = invf[None, :]
    return cst


def _kc_layout(w, ncols_pad=None):
    n = w.shape[1]
    a = np.ascontiguousarray(w.reshape(KC, 128, n).transpose(1, 0, 2))
    if ncols_pad is not None and ncols_pad != n:
        o = np.zeros((128, KC, ncols_pad), np.float32)
        o[:, :, :n] = a
        return o
    return a


def prep_weights(inp, kinds):
    ws = {"consts": _consts()}
    for l, kind in enumerate(kinds):
        j = l // 2
        wg = np.zeros((16, 128, KC, GW), np.float32)
        if kind == "ret":
            w = np.asarray(inp["w_in_ret"][j])
            for h in range(12):
                cols = np.concatenate([np.arange(h * 128, (h + 1) * 128), 1536 + np.arange(h * 128, (h + 1) * 128),
                                       3072 + np.arange(h * 256, (h + 1) * 256), 7168 + np.arange(h * 256, (h + 1) * 256)])
                wg[h] = _kc_layout(w[:, cols], GW)
            ws["rng%d" % l] = np.ascontiguousarray(inp["ret_norm_g"][j], dtype=np.float32)
        else:
            w = np.asarray(inp["w_in_gdn"][j])
            cwl = np.zeros((128, 192), np.float32)
            cw = np.asarray(inp["conv_w"][j])
            for g in range(12):
                chans = [g * 128, 1536 + g * 128, 3072 + (2 * g) * 128, 3072 + (2 * g + 1) * 128]
                cols = np.concatenate([np.arange(ch, ch + 128) for ch in chans] + [7168 + np.arange(g * 256, (g + 1) * 256),
                                      np.array([11264 + 2 * g, 11264 + 2 * g + 1, 11288 + 2 * g, 11288 + 2 * g + 1])])
                wg[g] = _kc_layout(w[:, cols], GW)
                for ct, ch in enumerate(chans):
                    for jj in range(4):
                        cwl[:, g * 16 + ct * 4 + jj] = cw[jj, ch:ch + 128]
            ws["cw%d" % l] = cwl
            ws["gng%d" % l] = np.ascontiguousarray(inp["gdn_norm_g"][j], dtype=np.float32)
            ws["alog%d" % l] = np.ascontiguousarray(inp["a_log"][j], dtype=np.float32)
            ws["dtb%d" % l] = np.ascontiguousarray(inp["dt_bias"][j], dtype=np.float32)
        for m in range(4):
            cols = np.concatenate([6144 + np.arange(m * 256, (m + 1) * 256), 7168 + 3072 + np.arange(m * 256, (m + 1) * 256)])
            wg[12 + m] = _kc_layout(w[:, cols], GW)
        ws["wg%d" % l] = wg
        wo = np.asarray(inp["w_out"][l])
        ws["wo%d" % l] = np.ascontiguousarray(wo.reshape(32, 128, 8, 256).transpose(2, 1, 0, 3))
        wkv = np.asarray(inp["w_mem_kv"][l])
        ws["wkv%d" % l] = np.ascontiguousarray(wkv.reshape(KC, 128, 4, 512).transpose(2, 1, 0, 3))
        ws["lng%d" % l] = np.ascontiguousarray(inp["ln_g"][l], dtype=np.float32)
        ws["lnb%d" % l] = np.ascontiguousarray(inp["ln_b"][l], dtype=np.float32)
    return ws


def core_map(ws, x_b, mem_b, pos_b):
    T = x_b.shape[0]
    m = dict(ws)
    m["x"] = np.ascontiguousarray(x_b, dtype=np.float32)
    m["mem"] = np.ascontiguousarray(mem_b, dtype=np.float32)
    m["pos"] = np.ascontiguousarray(np.asarray(pos_b, dtype=np.int32).reshape(T // 128, 128).T)
    return m


KINDS = ["ret", "gdn", "ret", "gdn"]


def kernel(x, mem, positions, w_in_ret, ret_norm_g, w_in_gdn, conv_w, a_log, dt_bias, gdn_norm_g,
           w_mem_kv, w_out, ln_g, ln_b):
    inp = dict(w_in_ret=w_in_ret, ret_norm_g=ret_norm_g, w_in_gdn=w_in_gdn, conv_w=conv_w, a_log=a_log, dt_bias=dt_bias,
               gdn_norm_g=gdn_norm_g, w_mem_kv=w_mem_kv, w_out=w_out, ln_g=ln_g, ln_b=ln_b)
    x = np.asarray(x); mem = np.asarray(mem); positions = np.asarray(positions)
    B, S, _ = x.shape
    ws = prep_weights(inp, KINDS)
    nc = build(2048, S // 2048, KINDS)
    in_maps = [core_map(ws, x[b % B], mem[b % B], positions[b % B]) for b in range(8)]
    res = run_bass_kernel_spmd(nc, in_maps, core_ids=list(range(8)))
    return np.stack([res.results[b]["out"] for b in range(B)], axis=0).astype(np.float32)
```

```python
import contextlib
import os
import numpy as np
GSTOP = float(os.environ.get('GSTOP', '9'))
import concourse.bass as bass
import concourse.mybir as mybir
from concourse.bass_utils import run_bass_kernel_spmd

F32 = mybir.dt.float32
BF16 = mybir.dt.bfloat16
I32 = mybir.dt.int32
AF = mybir.ActivationFunctionType
ALU = mybir.AluOpType
AX = mybir.AxisListType

D = 2048
KC = 16
NMEM = 256
DEPTH = 4
ALPHA = (2.0 * DEPTH) ** 0.25
LN_EPS = 1e-5
NORM_EPS = 1e-6
GW = 772
TWO_PI = float(2 * np.pi)
C_ID, C_UT, C_SM, C_NEGM, C_NEGMT, C_ONES, C_QS, C_KS, C_INVF, C_END = 0, 128, 256, 384, 512, 640, 768, 780, 792, 856


class Buf:
    __slots__ = ("name", "w", "r", "dsem", "excl")

    def __init__(self, name, excl=False):
        self.name = name
        self.w = None
        self.r = []
        self.dsem = None
        self.excl = excl


class Ctx:
    def __init__(self, nc, es):
        self.nc = nc
        self.es = es
        self.eng = {"pe": nc.tensor, "act": nc.scalar, "dve": nc.vector, "pool": nc.gpsimd, "sp": nc.sync}
        self.sem = {}
        self.cnt = {}
        for k in self.eng:
            self.sem[k] = es.enter_context(nc.semaphore("s_" + k))
            self.cnt[k] = 0
        self.waited = {k: {} for k in self.eng}
        self.semobj = {k: self.sem[k] for k in self.eng}
        self.latest = {k: 0 for k in self.eng}
        self.ndsem = 0
        self.free_dsems = []
        self.in_phase = False
        self.phase_keys = []

    def _wait(self, e, ev):
        if ev is None:
            return
        key, val = ev
        if key == e:
            if e == "pe":
                return
            if self.cnt[e] - val >= 2:
                return
        if key not in self.eng:
            val = max(val, self.latest.get(key, val))
        if self.waited[e].get(key, 0) >= val:
            return
        self.waited[e][key] = val
        self.eng[e].wait_ge(self.semobj[key], val)

    def deps(self, e, reads, writes):
        for b in reads:
            self._wait(e, b.w)
            if b.excl:
                for ev in b.r:
                    if ev[0] != e:
                        self._wait(e, ev)
        for b in writes:
            self._wait(e, b.w)
            for ev in b.r:
                self._wait(e, ev)

    def record(self, ev, reads, writes):
        for b in writes:
            b.w = ev
            b.r = []
        for b in reads:
            b.r = [x for x in b.r if x[0] != ev[0]] + [ev]

    def op(self, e, fn, reads=(), writes=(), inc=True):
        self.deps(e, reads, writes)
        ins = fn()
        if inc:
            self.cnt[e] += 1
            ins.then_inc(self.sem[e], 1)
            self.latest[e] = self.cnt[e]
            ev = (e, self.cnt[e])
        else:
            ev = (e, self.cnt[e] + 1)
        self.record(ev, reads, writes)
        return ins

    def dsem_of(self, buf):
        if buf.dsem is None:
            if self.free_dsems:
                key = self.free_dsems.pop()
            else:
                self.ndsem += 1
                s = self.es.enter_context(self.nc.semaphore("d%d" % self.ndsem))
                key = "d%d" % self.ndsem
                self.semobj[key] = s
                self.latest[key] = 0
            buf.dsem = key
            if self.in_phase:
                self.phase_keys.append(key)
        return buf.dsem

    def begin_phase(self):
        self.in_phase = True
        self.phase_keys = []

    def end_phase(self):
        self.barrier()
        self.free_dsems.extend(self.phase_keys)
        self.phase_keys = []
        self.in_phase = False

    def dma(self, q, out, in_, reads, writes, sembuf, transpose=False):
        self.deps(q, reads, writes)
        key = self.dsem_of(sembuf)
        if transpose:
            ins = self.eng[q].dma_start_transpose(out=out, in_=in_)
        else:
            ins = self.eng[q].dma_start(out=out, in_=in_)
        ins.then_inc(self.semobj[key], 16)
        self.latest[key] += 16
        self.record((key, self.latest[key]), reads, writes)
        return ins

    def barrier(self, engines=("pe", "act", "dve", "pool", "sp")):
        for e in engines:
            for key, val in self.latest.items():
                if val <= 0 or key == e:
                    continue
                if self.waited[e].get(key, 0) >= val:
                    continue
                self.waited[e][key] = val
                self.eng[e].wait_ge(self.semobj[key], val)


class Reg:
    __slots__ = ("ap", "buf")

    def __init__(self, ap, buf):
        self.ap = ap
        self.buf = buf


def build(SEGT, NSEG, kinds, dbg=False):
    NB = SEGT // 128
    T = SEGT * NSEG
    NBT = T // 128
    NL = len(kinds)
    nc = bass.Bass("TRN2", target_bir_lowering=False)
    dt = nc.dram_tensor
    x_d = dt("x", [T, D], F32, kind="ExternalInput").ap()
    mem_d = dt("mem", [NMEM, D], F32, kind="ExternalInput").ap()
    pos_d = dt("pos", [128, NBT], I32, kind="ExternalInput").ap()
    cst_d = dt("consts", [128, C_END], F32, kind="ExternalInput").ap()
    wg_d, wo_d, wkv_d, lng_d, lnb_d, p1_d, p2_d, p3_d, p4_d = [], [], [], [], [], [], [], [], []
    for l in range(NL):
        wg_d.append(dt("wg%d" % l, [16, 128, KC, GW], F32, kind="ExternalInput").ap())
        wo_d.append(dt("wo%d" % l, [8, 128, 32, 256], F32, kind="ExternalInput").ap())
        wkv_d.append(dt("wkv%d" % l, [4, 128, KC, 512], F32, kind="ExternalInput").ap())
        lng_d.append(dt("lng%d" % l, [D], F32, kind="ExternalInput").ap())
        lnb_d.append(dt("lnb%d" % l, [D], F32, kind="ExternalInput").ap())
        if kinds[l] == "ret":
            p1_d.append(dt("rng%d" % l, [3072], F32, kind="ExternalInput").ap())
            p2_d.append(None); p3_d.append(None); p4_d.append(None)
        else:
            p1_d.append(dt("gng%d" % l, [128], F32, kind="ExternalInput").ap())
            p2_d.append(dt("cw%d" % l, [128, 192], F32, kind="ExternalInput").ap())
            p3_d.append(dt("alog%d" % l, [24], F32, kind="ExternalInput").ap())
            p4_d.append(dt("dtb%d" % l, [24], F32, kind="ExternalInput").ap())
    out_d = dt("out", [T, D], F32, kind="ExternalOutput").ap()
    xres_d = dt("xres", [T, D], F32).ap()
    xb_d = dt("xb", [T, D], BF16).ap()
    br_d = dt("br", [T, 4096], BF16).ap()
    memb_d = dt("memb", [NMEM, D], BF16).ap()
    st_d = [dt("st%d" % l, [24, 128, 256], F32).ap() for l in range(NL)]
    wob_d = [dt("wob%d" % l, [8, 128, 32, 256], BF16).ap() for l in range(NL)]
    cst8_d = [dt("cvst%d" % l, [12, 128, 12], F32).ap() for l in range(NL)]
    if dbg:
        dbg_d = dt("dbg_br", [T, 4096], F32, kind="ExternalOutput").ap()

    with contextlib.ExitStack() as es:
        c = Ctx(nc, es)

        _uid = [0]

        def uname(name):
            _uid[0] += 1
            return "%s_u%d" % (name, _uid[0])

        def sbt(stack, name, shape, dtype):
            t = stack.enter_context(nc.sbuf_tensor(uname(name), shape, dtype))
            return t, Buf(name)

        def act(out, in_, func, reads, writes, **kw):
            return c.op("act", lambda: nc.scalar.activation(out=out, in_=in_, func=func, **kw), reads, writes)

        def tt(e, out, in0, in1, op, reads, writes):
            eng = c.eng[e]
            return c.op(e, lambda: eng.tensor_tensor(out=out, in0=in0, in1=in1, op=op), reads, writes)

        def ts(e, out, in0, s1, s2, op0, op1, reads, writes):
            eng = c.eng[e]
            if op1 is None:
                return c.op(e, lambda: eng.tensor_scalar(out=out, in0=in0, scalar1=s1, scalar2=None, op0=op0), reads, writes)
            return c.op(e, lambda: eng.tensor_scalar(out=out, in0=in0, scalar1=s1, scalar2=s2, op0=op0, op1=op1), reads, writes)

        def stt(e, out, in0, scalar, in1, op0, op1, reads, writes):
            eng = c.eng[e]
            return c.op(e, lambda: eng.scalar_tensor_tensor(out=out, in0=in0, scalar=scalar, in1=in1, op0=op0, op1=op1), reads, writes)

        def cp(e, out, in_, reads, writes):
            eng = c.eng[e]
            return c.op(e, lambda: eng.tensor_copy(out=out, in_=in_), reads, writes)

        def mm(out, pairs, reads, writes):
            n = len(pairs)
            for i, (l_, r_) in enumerate(pairs):
                last = i == n - 1
                c.op("pe", lambda: nc.tensor.matmul(out, l_, r_, start=(i == 0), stop=last),
                     reads if (last or i == 0) else (), writes if (last or i == 0) else (), inc=last)

        def trp(out, in_, ident, reads, writes):
            return c.op("pe", lambda: nc.tensor.transpose(out, in_, ident), reads, writes)

        cst, cstb = sbt(es, "cst", [128, C_END], F32)
        c.dma("sp", cst[:], cst_d, [], [cstb], cstb)
        idbf, idbfb = sbt(es, "idbf", [128, 128], BF16)
        cp("dve", idbf[:], cst[:, C_ID:C_ID + 128], [cstb], [idbfb])
        ID32 = cst[:, C_ID:C_ID + 128]
        UT32 = cst[:, C_UT:C_UT + 128]
        SM32 = cst[:, C_SM:C_SM + 128]
        ONES32 = cst[:, C_ONES:C_ONES + 128]
        onebf, onebfb = sbt(es, "onebf", [128, 128], BF16)
        cp("dve", onebf[:], ONES32, [cstb], [onebfb])
        negmbf, negmbfb = sbt(es, "negmbf", [128, 256], BF16)
        cp("dve", negmbf[:], cst[:, C_NEGM:C_NEGM + 256], [cstb], [negmbfb])

        dummy = Buf("dummy")
        wobD = Buf("wobD")
        for r0 in range(0, T, 512):
            c.dma("pool", xb_d[r0:r0 + 512, :], x_d[r0:r0 + 512, :], [], [], dummy)
        c.dma("pool", memb_d, mem_d, [], [], dummy)
        c.barrier(["sp", "pool"])
        memT, memTb = sbt(es, "memT", [128, KC, NMEM], BF16)
        for kc in range(KC):
            c.dma("sp", memT[:, kc, :], memb_d[:, kc * 128:(kc + 1) * 128], [], [memTb], memTb, transpose=True)
        mkT, mkTb = sbt(es, "mkT", [128, 8, NMEM], BF16)
        mv, mvb = sbt(es, "mv", [128, 2, 1024], BF16)

        has_ret = "ret" in kinds
        rot_d = dt("rot_d", [3, 128, NBT, 64], F32).ap()
        if has_ret:
            c.begin_phase()
            with contextlib.ExitStack() as ps:
                cosT, cosb = sbt(ps, "cosT0", [128, NBT, 64], F32)
                sinT, sinb = sbt(ps, "sinT0", [128, NBT, 64], F32)
                nsinT, nsinb = sbt(ps, "nsinT0", [128, NBT, 64], F32)
                pi_, pib = sbt(ps, "posi", [128, NBT], I32)
                pf_, pfb = sbt(ps, "posf", [128, NBT], F32)
                ang, angb = sbt(ps, "ang", [128, NBT, 64], F32)
                nf, nfb = sbt(ps, "nf", [128, NBT, 64], F32)
                ni, nib = sbt(ps, "ni", [128, NBT, 64], I32)
                c.dma("sp", pi_[:], pos_d, [], [pib], pib)
                cp("dve", pf_[:], pi_[:], [pib], [pfb])
                invf = cst[:, C_INVF:C_INVF + 64]
                for b in range(NBT):
                    ts("dve", ang[:, b, :], invf, pf_[:, b:b + 1], None, ALU.mult, None, [cstb, pfb], [angb])

                def reduce_and_sin(dst, dstb, shift):
                    ts("dve", nf[:], ang[:], shift, 1.0 / TWO_PI, ALU.add, ALU.mult, [angb], [nfb])
                    cp("dve", ni[:], nf[:], [nfb], [nib])
                    cp("dve", nf[:], ni[:], [nib], [nfb])
                    stt("dve", nf[:], nf[:], -TWO_PI, ang[:], ALU.mult, ALU.add, [nfb, angb], [nfb])
                    if shift != 0.0:
                        ts("dve", nf[:], nf[:], shift, None, ALU.add, None, [nfb], [nfb])
                    ni_f = ni[:].bitcast(F32)
                    ts("dve", ni_f, nf[:], float(np.pi), -TWO_PI, ALU.is_gt, ALU.mult, [nfb], [nib])
                    tt("dve", nf[:], nf[:], ni_f, ALU.add, [nfb, nib], [nfb])
                    ts("dve", ni_f, nf[:], -float(np.pi), TWO_PI, ALU.is_lt, ALU.mult, [nfb], [nib])
                    tt("dve", nf[:], nf[:], ni_f, ALU.add, [nfb, nib], [nfb])
                    act(dst[:], nf[:], AF.Sin, [nfb], [dstb])

                reduce_and_sin(sinT, sinb, 0.0)
                reduce_and_sin(cosT, cosb, float(np.pi / 2))
                ts("dve", nsinT[:], sinT[:], -1.0, None, ALU.mult, None, [sinb], [nsinb])
                c.dma("sp", rot_d[0], cosT[:], [cosb], [], cosb)
                c.dma("sp", rot_d[1], sinT[:], [sinb], [], sinb)
                c.dma("sp", rot_d[2], nsinT[:], [nsinb], [], nsinb)
                c.end_phase()
        c.barrier()

        def wtiles(stack, name, shape, dtype, n=2):
            return [Reg(*_mk(stack, "%s%d" % (name, i), shape, dtype)) for i in range(n)]

        def _mk(stack, name, shape, dtype):
            t, b = sbt(stack, name, shape, dtype)
            return t[:], b

        def load_bcast(stack, name, src, n, q="sp"):
            t, b = sbt(stack, name, [128, n], F32)
            c.dma(q, t[:], src.partition_broadcast(128), [], [b], b)
            return t, b

        def mem_kv(l):
            c.begin_phase()
            with contextlib.ExitStack() as ps:
                wk = wtiles(ps, "wkv", [128, KC, 512], BF16, 2)
                pk = [Reg(ps.enter_context(nc.psum_tensor(uname("pkv"), [128, 512], F32))[:], Buf("pkv%d" % i, excl=True)) for i in range(2)]
                it = 0
                for ch in range(4):
                    w = wk[ch % 2]
                    for q4 in range(4):
                        c.dma("pool", w.ap[:, q4 * 4:(q4 + 1) * 4, :], wkv_d[l][ch][:, q4 * 4:(q4 + 1) * 4, :], [], [w.buf], w.buf)
                    if ch < 2:
                        for ct in range(4):
                            p = pk[it % 2]; it += 1
                            mm(p.ap[:, 0:NMEM], [(w.ap[:, kc, ct * 128:(ct + 1) * 128], memT[:, kc, :]) for kc in range(KC)],
                               [w.buf, memTb], [p.buf])
                            act(mkT[:, ch * 4 + ct, :], p.ap[:, 0:NMEM], AF.Copy, [p.buf], [mkTb])
                    else:
                        for mc in range(2):
                            p = pk[it % 2]; it += 1
                            mm(p.ap[:, :], [(memT[:, kc, mc * 128:(mc + 1) * 128], w.ap[:, kc, :]) for kc in range(KC)],
                               [w.buf, memTb], [p.buf])
                            act(mv[:, mc, (ch - 2) * 512:(ch - 1) * 512], p.ap[:, :], AF.Copy, [p.buf], [mvb])
                c.end_phase()

        def phase_a(l, s):
            kind = kinds[l]
            tok0 = s * SEGT
            c.begin_phase()
            with contextlib.ExitStack() as ps:
                xT, _xTb0 = sbt(ps, "xT", [128, KC, SEGT], BF16)
                xTbs = [Buf("xT%d" % i) for i in range((SEGT + 511) // 512)]
                for r0 in range(0, SEGT, 512):
                    r1 = min(SEGT, r0 + 512)
                    for kc in range(KC):
                        c.dma("sp", xT[:, kc, r0:r1], xb_d[tok0 + r0:tok0 + r1, kc * 128:(kc + 1) * 128], [], [xTbs[r0 // 512]], xTbs[r0 // 512], transpose=True)
                Wt = wtiles(ps, "W", [128, KC, GW], BF16, 2)
                PS = [ps.enter_context(nc.psum_tensor(uname("ps"), [128, 512], F32)) for i in range(7)]
                PSB = ps.enter_context(nc.psum_tensor(uname("psb"), [128, 1024], BF16))
                _regs = {}
                _bankbuf = {}

                def R(bank, c0, c1):
                    k = (bank, c0, c1)
                    if k not in _regs:
                        if bank not in _bankbuf:
                            _bankbuf[bank] = Buf("psbank_%s" % bank, excl=True)
                        if bank == "b":
                            _regs[k] = Reg(PSB[:, c0:c1], _bankbuf[bank])
                        else:
                            _regs[k] = Reg(PS[bank][:, c0:c1], _bankbuf[bank])
                    return _regs[k]

                def load_w(g, ncols):
                    w = Wt[g % 2]
                    for q4 in range(4):
                        c.dma("pool", w.ap[:, q4 * 4:(q4 + 1) * 4, 0:ncols], wg_d[l][g][:, q4 * 4:(q4 + 1) * 4, 0:ncols], [], [w.buf], w.buf)
                    return w

                def store_branch(t, blk, col0, ncol):
                    r0 = tok0 + blk * 128
                    c.dma("sp", br_d[r0:r0 + 128, col0:col0 + ncol], t.ap, [t.buf], [], t.buf)

                gcols = [768] * 12 + [512] * 4 if kind == "ret" else [GW] * 12 + [512] * 4
                load_w(0, gcols[0])
                if s == 0:
                    for n8 in range(8):
                        c.dma("pool", wob_d[l][n8], wo_d[l][n8], [], [], wobD)

                st4 = wtiles(ps, "st4", [128, 16], F32, 2)
                e_t = wtiles(ps, "e_t", [128, 256], F32, 2)
                zs_t = wtiles(ps, "zs_t", [128, 256], F32, 2)
                osb = wtiles(ps, "osb", [128, 256], F32, 2)
                junk = wtiles(ps, "junk", [128, 256], BF16, 2)
                brt = wtiles(ps, "brt", [128, 256], BF16, 3)

                def gate_from_z(zreg, it, width=256):
                    e = e_t[it % 2]; zs = zs_t[it % 2]
                    act(e.ap[:, 0:width], zreg.ap, AF.Exp, [zreg.buf], [e.buf], scale=-1.0)
                    ts("dve", e.ap[:, 0:width], e.ap[:, 0:width], 1.0, None, ALU.add, None, [e.buf], [e.buf])
                    c.op("dve", lambda: nc.vector.reciprocal(out=e.ap[:, 0:width], in_=e.ap[:, 0:width]), [e.buf], [e.buf])
                    tt("dve", zs.ap[:, 0:width], zreg.ap, e.ap[:, 0:width], ALU.mult, [zreg.buf, e.buf], [zs.buf])
                    return zs

                def rstd_from(stt_, col_in, col_out, scale, eps):
                    act(stt_.ap[:, col_out:col_out + 1], stt_.ap[:, col_in:col_in + 1], AF.Ln, [stt_.buf, epsTb], [stt_.buf], scale=scale, bias=epsT[:, eps:eps + 1])
                    act(stt_.ap[:, col_out:col_out + 1], stt_.ap[:, col_out:col_out + 1], AF.Exp, [stt_.buf], [stt_.buf], scale=-0.5)

                epsT, epsTb = sbt(ps, "epsT", [128, 2], F32)
                c.op("dve", lambda: nc.vector.memset(epsT[:, 0:1], NORM_EPS), [], [epsTb])
                c.op("dve", lambda: nc.vector.memset(epsT[:, 1:2], LN_EPS), [], [epsTb])

                git = 0
                if kind == "ret":
                    gam, gamb = load_bcast(ps, "rng", p1_d[l], 3072)
                    b0 = tok0 // 128
                    cosT, cosb = sbt(ps, "cosT", [128, NB, 64], F32)
                    sinT, sinb = sbt(ps, "sinT", [128, NB, 64], F32)
                    nsinT, nsinb = sbt(ps, "nsinT", [128, NB, 64], F32)
                    c.dma("sp", cosT[:], rot_d[0][:, b0:b0 + NB, :], [], [cosb], cosb)
                    c.dma("sp", sinT[:], rot_d[1][:, b0:b0 + NB, :], [], [sinb], sinb)
                    c.dma("sp", nsinT[:], rot_d[2][:, b0:b0 + NB, :], [], [nsinb], nsinb)
                    qks = wtiles(ps, "qks", [128, 256], F32, 2)
                    vbf = wtiles(ps, "vbf", [128, 256], BF16, 2)
                    rA = wtiles(ps, "rA", [128, 256], F32, 2)
                    rB = wtiles(ps, "rB", [128, 256], F32, 2)
                    rot = wtiles(ps, "rot", [128, 256], BF16, 2)
                    qkT = wtiles(ps, "qkT", [128, 256], BF16, 2)
                    PT = wtiles(ps, "PT", [128, 128], BF16, 2)
                    Y, Yb = sbt(ps, "Y", [128, 256], F32)
                    Sbf = wtiles(ps, "Sbf", [128, 256], BF16, 2)
                    for h in range(12):
                        w = Wt[h % 2]
                        load_w(h + 1, gcols[h + 1])
                        g128 = float((1.0 - 2.0 ** (-5 - h)) ** 128)
                        have_state = s > 0
                        if have_state:
                            c.dma("sp", Y[:], st_d[l][h], [], [Yb], Yb)
                            sb0 = Sbf[0]
                            act(sb0.ap, Y[:], AF.Copy, [Yb], [sb0.buf], scale=g128)
                        git0 = git; git += NB
                        hs = [have_state]

                        def a_proj(blk):
                            it = git0 + blk
                            tok = slice(blk * 128, (blk + 1) * 128)
                            gb = blk
                            PA = R(it % 2, 0, 512)
                            PZ = R(2 + it % 2, 0, 256)
                            mm(PA.ap, [(xT[:, kc, tok], w.ap[:, kc, 0:512]) for kc in range(KC)], [xTbs[blk // 4], w.buf], [PA.buf])
                            mm(PZ.ap, [(xT[:, kc, tok], w.ap[:, kc, 512:768]) for kc in range(KC)], [xTbs[blk // 4], w.buf], [PZ.buf])

                        def a_rest(blk):
                            it = git0 + blk
                            tok = slice(blk * 128, (blk + 1) * 128)
                            gb = blk
                            PA = R(it % 2, 0, 512)
                            PZ = R(2 + it % 2, 0, 256)
                            qk = qks[it % 2]; v = vbf[it % 2]
                            act(qk.ap[:, 0:128], PA.ap[:, 0:128], AF.Copy, [PA.buf, cstb], [qk.buf], scale=cst[:, C_QS + h:C_QS + h + 1])
                            act(qk.ap[:, 128:256], PA.ap[:, 128:256], AF.Copy, [PA.buf, cstb], [qk.buf], scale=cst[:, C_KS + h:C_KS + h + 1])
                            act(v.ap, PA.ap[:, 256:512], AF.Copy, [PA.buf], [v.buf])
                            gate_from_z(PZ, it)
                            yield
                            a_ = rA[it % 2]; b_ = rB[it % 2]; ro = rot[it % 2]
                            qk4 = qk.ap.rearrange("p (a b d) -> p a b d", a=2, b=2)
                            a4 = a_.ap.rearrange("p (a b d) -> p a b d", a=2, b=2)
                            b4 = b_.ap.rearrange("p (a b d) -> p a b d", a=2, b=2)
                            cosb4 = cosT[:, gb, :].unsqueeze(1).unsqueeze(1).to_broadcast([128, 2, 2, 64])
                            sinb3 = sinT[:, gb, :].unsqueeze(1).to_broadcast([128, 2, 64])
                            nsinb3 = nsinT[:, gb, :].unsqueeze(1).to_broadcast([128, 2, 64])
                            tt("dve", a4, qk4, cosb4, ALU.mult, [qk.buf, cosb], [a_.buf])
                            tt("pool", b4[:, :, 0, :], qk4[:, :, 1, :], nsinb3, ALU.mult, [qk.buf, nsinb], [b_.buf])
                            tt("pool", b4[:, :, 1, :], qk4[:, :, 0, :], sinb3, ALU.mult, [qk.buf, sinb], [b_.buf])
                            yield
                            tt("dve", ro.ap, a_.ap, b_.ap, ALU.add, [a_.buf, b_.buf], [ro.buf])
                            yield
                            TR = R("b", 0, 256)
                            trp(TR.ap[:, 0:128], ro.ap[:, 0:128], idbf[:], [ro.buf, idbfb], [TR.buf])
                            trp(TR.ap[:, 128:256], ro.ap[:, 128:256], idbf[:], [ro.buf, idbfb], [TR.buf])
                            yield
                            qt = qkT[it % 2]
                            act(qt.ap, TR.ap, AF.Copy, [TR.buf], [qt.buf])
                            yield
                            SC = R(4, 0, 128)
                            mm(SC.ap, [(qt.ap[:, 128:256], qt.ap[:, 0:128])], [qt.buf], [SC.buf])
                            yield
                            pt = PT[it % 2]
                            tt("dve", pt.ap, SC.ap, UT32, ALU.mult, [SC.buf, cstb], [pt.buf])
                            yield

                        def b_part(blk):
                            it = git0 + blk
                            tok = slice(blk * 128, (blk + 1) * 128)
                            gb = blk
                            PA = R(it % 2, 0, 512)
                            PZ = R(2 + it % 2, 0, 256)
                            qk = qks[it % 2]; v = vbf[it % 2]
                            a_ = rA[it % 2]; b_ = rB[it % 2]; ro = rot[it % 2]
                            qt = qkT[it % 2]; pt = PT[it % 2]
                            O = R(5, 0, 256)
                            sprev = Sbf[blk % 2]
                            if hs[0]:
                                mm(O.ap, [(pt.ap, v.ap), (qt.ap[:, 0:128], sprev.ap)], [pt.buf, v.buf, qt.buf, sprev.buf], [O.buf])
                            else:
                                mm(O.ap, [(pt.ap, v.ap)], [pt.buf, v.buf], [O.buf])
                            KV = R(6, 0, 256)
                            mm(KV.ap, [(ro.ap[:, 128:256], v.ap)], [ro.buf, v.buf], [KV.buf])
                            yield
                            if hs[0]:
                                stt("dve", Y[:], Y[:], g128, KV.ap, ALU.mult, ALU.add, [Yb, KV.buf], [Yb])
                            else:
                                cp("dve", Y[:], KV.ap, [KV.buf], [Yb])
                            hs[0] = True
                            snext = Sbf[(blk + 1) % 2]
                            act(snext.ap, Y[:], AF.Copy, [Yb], [snext.buf], scale=g128)
                            st = st4[it % 2]; o_ = osb[it % 2]; jk = junk[it % 2]
                            act(o_.ap, O.ap, AF.Copy, [O.buf], [o_.buf, st.buf], accum_out=st.ap[:, 0:1])
                            act(jk.ap, O.ap, AF.Square, [O.buf], [jk.buf, st.buf], accum_out=st.ap[:, 1:2])
                            yield
                            ts("dve", st.ap[:, 2:3], st.ap[:, 0:1], 1.0 / 256, None, ALU.mult, None, [st.buf], [st.buf])
                            tt("dve", st.ap[:, 3:4], st.ap[:, 2:3], st.ap[:, 2:3], ALU.mult, [st.buf], [st.buf])
                            stt("dve", st.ap[:, 4:5], st.ap[:, 1:2], 1.0 / 256, st.ap[:, 3:4], ALU.mult, ALU.subtract, [st.buf], [st.buf])
                            yield
                            rstd_from(st, 4, 5, 1.0, 0)
                            yield
                            ts("dve", o_.ap, o_.ap, st.ap[:, 2:3], st.ap[:, 5:6], ALU.subtract, ALU.mult, [o_.buf, st.buf], [o_.buf])
                            zs = zs_t[it % 2]
                            yield
                            tt("pool", o_.ap, o_.ap, gam[:, h * 256:(h + 1) * 256], ALU.mult, [o_.buf, gamb], [o_.buf])
                            yield
                            bt = brt[it % 3]
                            tt("dve", bt.ap, o_.ap, zs.ap, ALU.mult, [o_.buf, zs.buf], [bt.buf])
                            store_branch(bt, blk, h * 256, 256)

                        def rr_run(gl):
                            alive = list(gl)
                            while alive:
                                nxt = []
                                for gen_ in alive:
                                    try:
                                        next(gen_)
                                        nxt.append(gen_)
                                    except StopIteration:
                                        pass
                                alive = nxt

                        a_proj(0)
                        rr_run([a_rest(0)])
                        for blk in range(NB):
                            if blk + 1 < NB:
                                a_proj(blk + 1)
                                rr_run([b_part(blk), a_rest(blk + 1)])
                            else:
                                rr_run([b_part(blk)])
                        c.dma("sp", st_d[l][h], Y[:], [Yb], [], Yb)
                else:
                    gdn_heads(ps, l, s, tok0, xT, xTbs, Wt, load_w, gcols, R, store_branch, gate_from_z, rstd_from,
                              st4, e_t, zs_t, osb, junk, brt)
                    git = 1000
                mqT = wtiles(ps, "mqT", [128, 2, 128], BF16, 2)
                pbf = wtiles(ps, "pbf", [128, 256], BF16, 2)
                pTt = wtiles(ps, "pTt", [128, 256], BF16, 2)
                for m in range(4):
                    g = 12 + m
                    w = Wt[g % 2]
                    if g + 1 < 16:
                        load_w(g + 1, gcols[g + 1])
                    for blk in range(NB):
                        it = git; git += 1
                        tok = slice(blk * 128, (blk + 1) * 128)
                        PQ = R(it % 2, 0, 256)
                        PZ = R(2 + it % 2, 0, 256)
                        for hf in range(2):
                            mm(PQ.ap[:, hf * 128:(hf + 1) * 128], [(w.ap[:, kc, hf * 128:(hf + 1) * 128], xT[:, kc, tok]) for kc in range(KC)],
                               [xTbs[blk // 4], w.buf], [PQ.buf])
                        mm(PZ.ap, [(xT[:, kc, tok], w.ap[:, kc, 256:512]) for kc in range(KC)], [xTbs[blk // 4], w.buf], [PZ.buf])
                        mq = mqT[it % 2]
                        act(mq.ap.rearrange("p a b -> p (a b)"), PQ.ap, AF.Copy, [PQ.buf], [mq.buf])
                        SC = R(4, 0, 256)
                        mm(SC.ap, [(mq.ap[:, hf, :], mkT[:, m * 2 + hf, :]) for hf in range(2)], [mq.buf, mkTb], [SC.buf])
                        st = st4[it % 2]
                        c.op("dve", lambda: nc.vector.reduce_max(out=st.ap[:, 0:1], in_=SC.ap, axis=AX.X), [SC.buf], [st.buf])
                        ts("dve", st.ap[:, 1:2], st.ap[:, 0:1], -1.0 / 16, None, ALU.mult, None, [st.buf], [st.buf])
                        pb_ = pbf[it % 2]
                        act(pb_.ap, SC.ap, AF.Exp, [SC.buf, st.buf], [pb_.buf, st.buf], scale=1.0 / 16, bias=st.ap[:, 1:2], accum_out=st.ap[:, 2:3])
                        TR = R("b", 0, 256)
                        trp(TR.ap[:, 0:128], pb_.ap[:, 0:128], idbf[:], [pb_.buf, idbfb], [TR.buf])
                        trp(TR.ap[:, 128:256], pb_.ap[:, 128:256], idbf[:], [pb_.buf, idbfb], [TR.buf])
                        pT_ = pTt[it % 2]
                        cp("dve", pT_.ap, TR.ap, [TR.buf], [pT_.buf])
                        O = R(5, 0, 256)
                        mm(O.ap, [(pT_.ap[:, mc * 128:(mc + 1) * 128], mv[:, mc, m * 256:(m + 1) * 256]) for mc in range(2)],
                           [pT_.buf, mvb], [O.buf])
                        c.op("dve", lambda: nc.vector.reciprocal(out=st.ap[:, 3:4], in_=st.ap[:, 2:3]), [st.buf], [st.buf])
                        zs = gate_from_z(PZ, it)
                        bt = brt[it % 3]
                        stt("dve", bt.ap, O.ap, st.ap[:, 3:4], zs.ap, ALU.mult, ALU.mult, [O.buf, st.buf, zs.buf], [bt.buf])
                        store_branch(bt, blk, 3072 + m * 256, 256)
                c.end_phase()

        def gdn_heads(ps, l, s, tok0, xT, xTbs, Wt, load_w, gcols, R, store_branch, gate_from_z, rstd_from,
                      st4, e_t, zs_t, osb, junk, brt):
            TT = min(512, SEGT)
            NBK = TT // 128
            cwt, cwtb = sbt(ps, "cwt", [128, 192], F32)
            c.dma("sp", cwt[:], p2_d[l], [], [cwtb], cwtb)
            negA, negAb = load_bcast(ps, "negA", p3_d[l], 24)
            act(negA[:], negA[:], AF.Exp, [negAb], [negAb])
            ts("dve", negA[:], negA[:], -1.0, None, ALU.mult, None, [negAb], [negAb])
            dtb, dtbb = load_bcast(ps, "dtb", p4_d[l], 24)
            gg, ggb = sbt(ps, "gg", [128, 256], F32)
            c.dma("sp", gg[:, 0:128], p1_d[l].partition_broadcast(128), [], [ggb], ggb)
            c.dma("sp", gg[:, 128:256], p1_d[l].partition_broadcast(128), [], [ggb], ggb)
            epsG, epsGb = sbt(ps, "epsG", [128, 1], F32)
            c.op("dve", lambda: nc.vector.memset(epsG[:], NORM_EPS), [], [epsGb])
            hraw, hrawb = sbt(ps, "hraw", [128, 4, 3 + TT], F32)
            acc, accb = sbt(ps, "acc", [128, 2, TT], F32)
            etmp, etmpb = sbt(ps, "etmp", [128, TT], F32)
            cvo, cvob = sbt(ps, "cvo", [128, 4, TT], BF16)
            qsq, qsqb = sbt(ps, "qsq", [128, 2, TT], BF16)
            S32, S32b = sbt(ps, "S32", [128, 2, 128], F32)
            Sbf = wtiles(ps, "gSbf", [128, 2, 128], BF16, 2)
            scal = wtiles(ps, "scal", [128, 32], F32, 4)
            gt_ = wtiles(ps, "gt", [128, 2], F32, 4)
            ez_ = wtiles(ps, "ez", [128, 256], F32, 2)
            zsb_ = wtiles(ps, "zsb", [128, 256], F32, 4)
            Gbt = wtiles(ps, "Gbt", [128, 128], F32, 2)
            ngbt = wtiles(ps, "ngbt", [128, 128], F32, 2)
            gbtt = wtiles(ps, "gbtt", [128, 128], F32, 2)
            Dm = wtiles(ps, "Dm", [128, 2, 128], F32, 2)
            DTm = wtiles(ps, "DTm", [128, 2, 128], F32, 2)
            DSm = wtiles(ps, "DSm", [128, 2, 128], F32, 2)
            khat_ = wtiles(ps, "khat", [128, 128], BF16, 2)
            khT_ = wtiles(ps, "khT", [128, 128], BF16, 4)
            khT2_ = wtiles(ps, "khT2", [128, 128], BF16, 2)
            vb_ = wtiles(ps, "vb", [128, 2, 128], F32, 4)
            ktl_ = wtiles(ps, "ktl", [128, 2, 128], BF16, 4)
            XD = wtiles(ps, "XD", [128, 2, 384], F32, 2)
            QKD = wtiles(ps, "QKD", [128, 2, 128], BF16, 4)
            TTt = wtiles(ps, "TTt", [128, 2, 128], BF16, 4)
            r_ = wtiles(ps, "r_", [128, 2, 128], BF16, 2)
            qSs = wtiles(ps, "qSs", [128, 2, 128], F32, 2)
            vn_ = wtiles(ps, "vn", [128, 2, 128], BF16, 2)
            ones_row = cst[0:1, C_ONES:C_ONES + 128]
            git = 0
            for g in range(12):
                w = Wt[g % 2]
                load_w(g + 1, gcols[g + 1])
                if s > 0:
                    for h in range(2):
                        c.dma("sp", S32[:, h, :], st_d[l][2 * g + h][:, 0:128], [], [S32b], S32b)
                    c.dma("sp", hraw[:, :, 0:3], cst8_d[l][g].rearrange("p (a b) -> p a b", a=4), [], [hrawb], hrawb)
                else:
                    c.op("dve", lambda: nc.vector.memset(S32[:], 0.0), [], [S32b])
                    c.op("dve", lambda: nc.vector.memset(hraw[:, :, 0:3], 0.0), [], [hrawb])
                cp("dve", Sbf[0].ap, S32[:], [S32b], [Sbf[0].buf])
                sidx = 0
                for tt_i in range(SEGT // TT):
                    t0 = tt_i * TT
                    for ct in range(4):
                        PJ = R(1, 0, TT)
                        mm(PJ.ap, [(w.ap[:, kc, ct * 128:(ct + 1) * 128], xT[:, kc, t0:t0 + TT]) for kc in range(KC)], [xTbs[tt_i], w.buf], [PJ.buf])
                        act(hraw[:, ct, 3:3 + TT], PJ.ap, AF.Copy, [PJ.buf], [hrawb])
                        cb = g * 16 + ct * 4
                        ts("dve", acc[:, ct % 2, :], hraw[:, ct, 0:TT], cwt[:, cb:cb + 1], None, ALU.mult, None, [hrawb, cwtb], [accb])
                        for jj in range(1, 4):
                            stt("dve", acc[:, ct % 2, :], hraw[:, ct, jj:jj + TT], cwt[:, cb + jj:cb + jj + 1], acc[:, ct % 2, :], ALU.mult, ALU.add,
                                [hrawb, cwtb, accb], [accb])
                        act(etmp[:], acc[:, ct % 2, :], AF.Exp, [accb], [etmpb], scale=-1.0)
                        ts("dve", etmp[:], etmp[:], 1.0, None, ALU.add, None, [etmpb], [etmpb])
                        c.op("dve", lambda: nc.vector.reciprocal(out=etmp[:], in_=etmp[:]), [etmpb], [etmpb])
                        tt("dve", cvo[:, ct, :], acc[:, ct % 2, :], etmp[:], ALU.mult, [accb, etmpb], [cvob])
                        if ct < 2:
                            act(qsq[:, ct, :], cvo[:, ct, :], AF.Square, [cvob], [qsqb])
                    cp("dve", hraw[:, :, 0:3], hraw[:, :, TT:TT + 3], [hrawb], [hrawb])
                    def pre(bk, tt_i=tt_i, t0=t0):
                        blk = tt_i * NBK + bk
                        tb = slice(bk * 128, (bk + 1) * 128)
                        tokb = slice(t0 + bk * 128, t0 + (bk + 1) * 128)
                        par = bk % 2
                        PZ = R(par, 0, 260)
                        mm(PZ.ap, [(xT[:, kc, tokb], w.ap[:, kc, 512:772]) for kc in range(KC)], [xTbs[tt_i], w.buf], [PZ.buf])
                        TRK = R("b", par * 512, par * 512 + 384)
                        for i3 in range(3):
                            trp(TRK.ap[:, i3 * 128:(i3 + 1) * 128], cvo[:, 1 + i3, tb], idbf[:], [cvob, idbfb], [TRK.buf])
                        SM = R(par, 264, 272)
                        mm(SM.ap[:, 0:1], [(qsq[:, 0, tb], onebf[:, 0:1])], [qsqb, onebfb], [SM.buf])
                        mm(SM.ap[:, 1:2], [(qsq[:, 1, tb], onebf[:, 0:1])], [qsqb, onebfb], [SM.buf])
                        yield
                        sc = scal[bk]; gt = gt_[bk]
                        act(sc.ap[:, 0:2], SM.ap[:, 0:2], AF.Ln, [SM.buf, epsGb], [sc.buf], bias=epsG[:, 0:1])
                        act(sc.ap[:, 0:2], sc.ap[:, 0:2], AF.Exp, [sc.buf], [sc.buf], scale=-0.5)
                        tt("dve", sc.ap[:, 2:4], PZ.ap[:, 256:258], dtb[:, 2 * g:2 * g + 2], ALU.add, [PZ.buf, dtbb], [sc.buf])
                        act(sc.ap[:, 2:4], sc.ap[:, 2:4], AF.Exp, [sc.buf], [sc.buf])
                        act(sc.ap[:, 2:4], sc.ap[:, 2:4], AF.Ln, [sc.buf], [sc.buf], bias=1.0)
                        tt("dve", gt.ap, sc.ap[:, 2:4], negA[:, 2 * g:2 * g + 2], ALU.mult, [sc.buf, negAb], [gt.buf])
                        act(sc.ap[:, 4:6], PZ.ap[:, 258:260], AF.Exp, [PZ.buf], [sc.buf], scale=-1.0)
                        ts("dve", sc.ap[:, 4:6], sc.ap[:, 4:6], 1.0, None, ALU.add, None, [sc.buf], [sc.buf])
                        c.op("dve", lambda: nc.vector.reciprocal(out=sc.ap[:, 4:6], in_=sc.ap[:, 4:6]), [sc.buf], [sc.buf])
                        e = ez_[bk % 2]; zs = zsb_[bk]
                        act(e.ap, PZ.ap[:, 0:256], AF.Exp, [PZ.buf], [e.buf], scale=-1.0)
                        ts("dve", e.ap, e.ap, 1.0, None, ALU.add, None, [e.buf], [e.buf])
                        c.op("dve", lambda: nc.vector.reciprocal(out=e.ap, in_=e.ap), [e.buf], [e.buf])
                        tt("dve", zs.ap, PZ.ap[:, 0:256], e.ap, ALU.mult, [PZ.buf, e.buf], [zs.buf])
                        tt("pool", zs.ap, zs.ap, gg[:], ALU.mult, [zs.buf, ggb], [zs.buf])
                        yield
                        mm(SM.ap[:, 2:4], [(UT32, gt.ap)], [cstb, gt.buf], [SM.buf])
                        mm(SM.ap[:, 4:6], [(ONES32, gt.ap)], [cstb, gt.buf], [SM.buf])
                        yield
                        act(sc.ap[:, 20:24], SM.ap[:, 2:6], AF.Copy, [SM.buf], [sc.buf])
                        ts("dve", sc.ap[:, 24:26], sc.ap[:, 20:22], -1.0, None, ALU.mult, None, [sc.buf], [sc.buf])
                        D_ = Dm[bk % 2]; DT_ = DTm[bk % 2]; DS_ = DSm[bk % 2]
                        Gbs = [Gbt[h] for h in range(2)]
                        for h in range(2):
                            ts("dve", Gbs[h].ap, ONES32, gt.ap[:, h:h + 1], None, ALU.mult, None, [cstb, gt.buf], [Gbs[h].buf])
                        GCBs = [R(2, (2 * par + h) * 128, (2 * par + h + 1) * 128) for h in range(2)]
                        for h in range(2):
                            mm(GCBs[h].ap, [(Gbs[h].ap, UT32)], [Gbs[h].buf, cstb], [GCBs[h].buf])
                        yield
                        for h in range(2):
                            ngb = ngbt[h]; gbt = gbtt[h]
                            stt("dve", ngb.ap, GCBs[h].ap, -1.0, cst[:, C_NEGM:C_NEGM + 128], ALU.mult, ALU.add, [GCBs[h].buf, cstb], [ngb.buf])
                            tt("dve", gbt.ap, GCBs[h].ap, cst[:, C_NEGMT:C_NEGMT + 128], ALU.add, [GCBs[h].buf, cstb], [gbt.buf])
                            act(D_.ap[:, h, :], ngb.ap, AF.Exp, [ngb.buf, sc.buf], [D_.buf], bias=sc.ap[:, 20 + h:21 + h])
                            act(DT_.ap[:, h, :], gbt.ap, AF.Exp, [gbt.buf, sc.buf], [DT_.buf], bias=sc.ap[:, 24 + h:25 + h])
                            tt("pool", DS_.ap[:, h, :], D_.ap[:, h, :], SM32, ALU.mult, [D_.buf, cstb], [DS_.buf])
                        kh = khat_[bk % 2]; khT = khT_[bk]; khT2 = khT2_[bk % 2]
                        act(kh.ap, TRK.ap[:, 0:128], AF.Copy, [TRK.buf, sc.buf], [kh.buf], scale=sc.ap[:, 1:2])
                        yield
                        TK2 = R("b", par * 512 + 384, par * 512 + 512)
                        trp(TK2.ap, kh.ap, idbf[:], [kh.buf, idbfb], [TK2.buf])
                        yield
                        act(khT.ap, TK2.ap, AF.Copy, [TK2.buf], [khT.buf])
                        cp("dve", khT2.ap, TK2.ap, [TK2.buf], [khT2.buf])
                        vb = vb_[bk]; ktl = ktl_[bk]
                        for h in range(2):
                            act(vb.ap[:, h, :], TRK.ap[:, 128 + h * 128:256 + h * 128], AF.Copy, [TRK.buf, sc.buf], [vb.buf], scale=sc.ap[:, 4 + h:5 + h])
                        tt("dve", sc.ap[:, 6:8], sc.ap[:, 22:24], sc.ap[:, 20:22], ALU.subtract, [sc.buf], [sc.buf])
                        act(sc.ap[:, 6:8], sc.ap[:, 6:8], AF.Exp, [sc.buf], [sc.buf])
                        act(sc.ap[:, 8:12], sc.ap[:, 20:24], AF.Exp, [sc.buf], [sc.buf])
                        stt("dve", sc.ap[:, 12:14], sc.ap[:, 4:6], -1.0, sc.ap[:, 8:10], ALU.mult, ALU.mult, [sc.buf], [sc.buf])
                        ts("dve", sc.ap[:, 14:15], sc.ap[:, 0:1], 128.0 ** -0.5, None, ALU.mult, None, [sc.buf], [sc.buf])
                        ts("dve", sc.ap[:, 16:18], sc.ap[:, 8:10], sc.ap[:, 14:15], None, ALU.mult, None, [sc.buf], [sc.buf])
                        ts("dve", sc.ap[:, 18:20], sc.ap[:, 4:6], -1.0, None, ALU.mult, None, [sc.buf], [sc.buf])
                        for h in range(2):
                            ts("dve", ktl.ap[:, h, :], kh.ap, sc.ap[:, 6 + h:7 + h], None, ALU.mult, None, [kh.buf, sc.buf], [ktl.buf])
                        yield
                        KK = R(2, par * 256, par * 256 + 128); QKT = R(2, par * 256 + 128, par * 256 + 256)
                        mm(KK.ap, [(khT.ap, khT2.ap)], [khT.buf, khT2.buf], [KK.buf])
                        mm(QKT.ap, [(khT.ap, cvo[:, 0, tb])], [khT.buf, cvob], [QKT.buf])
                        yield
                        X = XD[bk % 2]; qkd = QKD[bk]; TTm = TTt[bk]
                        for h in range(2):
                            stt("dve", X.ap[:, h, 0:128], KK.ap, sc.ap[:, 18 + h:19 + h], DS_.ap[:, h, :], ALU.mult, ALU.mult, [KK.buf, sc.buf, DS_.buf], [X.buf])
                            tt("dve", qkd.ap[:, h, :], QKT.ap, DT_.ap[:, h, :], ALU.mult, [QKT.buf, DT_.buf], [qkd.buf])
                        yield
                        DBs = [R(3 + 2 * par + h, 0, 384) for h in range(2)]
                        for h in range(2):
                            trp(DBs[h].ap[:, 128:256], X.ap[:, h, 0:128], ID32, [X.buf, cstb], [DBs[h].buf])
                        yield
                        for h in range(2):
                            act(X.ap[:, h, 128:256], DBs[h].ap[:, 128:256], AF.Copy, [DBs[h].buf], [X.buf])
                            tt("dve", X.ap[:, h, 256:384], DBs[h].ap[:, 128:256], ID32, ALU.add, [DBs[h].buf, cstb], [X.buf])
                        yield
                        for h in range(2):
                            mm(DBs[h].ap[:, 0:128], [(X.ap[:, h, 128:256], X.ap[:, h, 0:128])], [X.buf], [DBs[h].buf])
                            mm(DBs[h].ap[:, 128:256], [(X.ap[:, h, 0:128], X.ap[:, h, 128:256])], [X.buf], [DBs[h].buf])
                        yield
                        for h in range(2):
                            act(X.ap[:, h, 0:256], DBs[h].ap[:, 0:256], AF.Copy, [DBs[h].buf], [X.buf])
                        yield
                        for lev in range(5):
                            for h in range(2):
                                mm(DBs[h].ap[:, 0:128], [(X.ap[:, h, 128:256], X.ap[:, h, 0:128])], [X.buf], [DBs[h].buf])
                                mm(DBs[h].ap[:, 128:384], [(X.ap[:, h, 0:128], X.ap[:, h, 128:384])], [X.buf], [DBs[h].buf])
                            yield
                            for h in range(2):
                                act(X.ap[:, h, 0:256], DBs[h].ap[:, 0:256], AF.Copy, [DBs[h].buf], [X.buf])
                                tt("dve", X.ap[:, h, 256:384], X.ap[:, h, 256:384], DBs[h].ap[:, 256:384], ALU.add, [DBs[h].buf, X.buf], [X.buf])
                            yield
                        for h in range(2):
                            mm(DBs[h].ap[:, 0:128], [(X.ap[:, h, 0:128], X.ap[:, h, 256:384])], [X.buf], [DBs[h].buf])
                        yield
                        for h in range(2):
                            tt("dve", TTm.ap[:, h, :], X.ap[:, h, 256:384], DBs[h].ap[:, 0:128], ALU.add, [DBs[h].buf, X.buf], [TTm.buf])

                    for bk0 in range(0, NBK, 2):
                        alive = [pre(bk) for bk in range(bk0, min(NBK, bk0 + 2))]
                        while alive:
                            nxt = []
                            for gen_ in alive:
                                try:
                                    next(gen_)
                                    nxt.append(gen_)
                                except StopIteration:
                                    pass
                            alive = nxt
                    for bk in range(NBK):
                        it = git; git += 1
                        blk = tt_i * NBK + bk
                        tb = slice(bk * 128, (bk + 1) * 128)
                        sc = scal[bk]; khT = khT_[bk]; vb = vb_[bk]; ktl = ktl_[bk]; qkd = QKD[bk]; TTm = TTt[bk]; zs = zsb_[bk]
                        sprev = Sbf[sidx % 2]; snext = Sbf[(sidx + 1) % 2]; sidx += 1
                        rr = r_[it % 2]; qs_ = qSs[it % 2]; vn = vn_[it % 2]; o_ = osb[it % 2]; st = st4[it % 2]; jk = junk[it % 2]
                        for h in range(2):
                            kS = R(3 + h, 0, 128)
                            qS = R(3 + h, 128, 256)
                            mm(kS.ap, [(khT.ap, sprev.ap[:, h, :])], [khT.buf, sprev.buf], [kS.buf])
                            mm(qS.ap, [(cvo[:, 0, tb], sprev.ap[:, h, :])], [cvob, sprev.buf], [qS.buf])
                        for h in range(2):
                            kS = R(3 + h, 0, 128)
                            qS = R(3 + h, 128, 256)
                            stt("dve", rr.ap[:, h, :], kS.ap, sc.ap[:, 12 + h:13 + h], vb.ap[:, h, :], ALU.mult, ALU.add, [kS.buf, sc.buf, vb.buf], [rr.buf])
                            act(qs_.ap[:, h, :], qS.ap, AF.Copy, [qS.buf, sc.buf], [qs_.buf], scale=sc.ap[:, 16 + h:17 + h])
                        for h in range(2):
                            VN = R(5 + h, 0, 128)
                            mm(VN.ap, [(TTm.ap[:, h, :], rr.ap[:, h, :])], [TTm.buf, rr.buf], [VN.buf])
                        for h in range(2):
                            VN = R(5 + h, 0, 128)
                            act(vn.ap[:, h, :], VN.ap, AF.Copy, [VN.buf], [vn.buf])
                        for h in range(2):
                            O2 = R(5 + h, 128, 256)
                            KVr = R(5 + h, 256, 384)
                            mm(O2.ap, [(qkd.ap[:, h, :], vn.ap[:, h, :])], [qkd.buf, vn.buf], [O2.buf])
                            mm(KVr.ap, [(ktl.ap[:, h, :], vn.ap[:, h, :])], [ktl.buf, vn.buf], [KVr.buf])
                        for h in range(2):
                            O2 = R(5 + h, 128, 256)
                            KVr = R(5 + h, 256, 384)
                            stt("dve", S32[:, h, :], S32[:, h, :], sc.ap[:, 10 + h:11 + h], KVr.ap, ALU.mult, ALU.add, [S32b, sc.buf, KVr.buf], [S32b])
                            act(snext.ap[:, h, :], S32[:, h, :], AF.Copy, [S32b], [snext.buf])
                            stt("dve", o_.ap[:, h * 128:(h + 1) * 128], O2.ap, sc.ap[:, 14:15], qs_.ap[:, h, :], ALU.mult, ALU.add,
                                [O2.buf, sc.buf, qs_.buf], [o_.buf])
                            act(jk.ap[:, h * 128:(h + 1) * 128], o_.ap[:, h * 128:(h + 1) * 128], AF.Square, [o_.buf], [jk.buf, st.buf], accum_out=st.ap[:, h:h + 1])
                        act(st.ap[:, 2:4], st.ap[:, 0:2], AF.Ln, [st.buf, epsGb], [st.buf], scale=1.0 / 128, bias=epsG[:, 0:1])
                        act(st.ap[:, 2:4], st.ap[:, 2:4], AF.Exp, [st.buf], [st.buf], scale=-0.5)
                        bt = brt[it % 3]
                        for h in range(2):
                            stt("dve", bt.ap[:, h * 128:(h + 1) * 128], o_.ap[:, h * 128:(h + 1) * 128], st.ap[:, 2 + h:3 + h], zs.ap[:, h * 128:(h + 1) * 128],
                                ALU.mult, ALU.mult, [o_.buf, st.buf, zs.buf], [bt.buf])
                        store_branch(bt, blk, g * 256, 256)
                for h in range(2):
                    c.dma("sp", st_d[l][2 * g + h][:, 0:128], S32[:, h, :], [S32b], [], S32b)
                c.dma("sp", cst8_d[l][g].rearrange("p (a b) -> p a b", a=4), hraw[:, :, 0:3], [hrawb], [], hrawb)

        def phase_b(l, s, last):
            tok0 = s * SEGT
            TT = min(512, SEGT)
            NBK = TT // 128
            c.begin_phase()
            with contextlib.ExitStack() as ps:
                brT = wtiles(ps, "brT", [128, 32, TT], BF16, 2)
                wo = wtiles(ps, "wo", [128, 32, 256], BF16, 2)
                xr2 = [wtiles(ps, "xr%d" % i, [128, D], F32, NBK) for i in range(2)]
                jk, jkb = sbt(ps, "jkB", [128, D], F32)
                lng, lngb = load_bcast(ps, "lng", lng_d[l], D)
                lnb, lnbb = load_bcast(ps, "lnb", lnb_d[l], D)
                st = wtiles(ps, "stB", [128, 8], F32, 2)
                epsT, epsTb = sbt(ps, "epsB", [128, 1], F32)
                c.op("dve", lambda: nc.vector.memset(epsT[:], LN_EPS), [], [epsTb])
                PS = [Reg(ps.enter_context(nc.psum_tensor(uname("pB"), [128, 512], F32))[:], Buf("pB%d" % i, excl=True)) for i in range(8)]
                src = x_d if l == 0 else xres_d
                dst = out_d if last else xres_d
                pit = 0
                wit = 0

                def tile_loads(ti):
                    tl0 = tok0 + ti * TT
                    bT_ = brT[ti % 2]
                    for fc in range(32):
                        c.dma("sp", bT_.ap[:, fc, :], br_d[tl0:tl0 + TT, fc * 128:(fc + 1) * 128], [], [bT_.buf], bT_.buf, transpose=True)
                    for bk in range(NBK):
                        xx = xr2[ti % 2][bk]
                        c.dma("sp", xx.ap, src[tl0 + bk * 128:tl0 + (bk + 1) * 128, :], [], [xx.buf], xx.buf)

                tile_loads(0)
                for tt_i in range(SEGT // TT):
                    t0 = tok0 + tt_i * TT
                    bT = brT[tt_i % 2]
                    xr = xr2[tt_i % 2]
                    y = xr
                    if tt_i + 1 < SEGT // TT:
                        tile_loads(tt_i + 1)
                    for n in range(8):
                        w = wo[wit % 2]; wit += 1
                        for q4 in range(2):
                            c.dma("act", w.ap[:, q4 * 16:(q4 + 1) * 16, :], wob_d[l][n][:, q4 * 16:(q4 + 1) * 16, :], [], [w.buf], w.buf)
                        for bk in range(NBK):
                            p = PS[pit % 8]; pit += 1
                            mm(p.ap[:, 0:256], [(bT.ap[:, fc, bk * 128:(bk + 1) * 128], w.ap[:, fc, :]) for fc in range(32)],
                               [bT.buf, w.buf], [p.buf])
                            stt("dve", y[bk].ap[:, n * 256:(n + 1) * 256], xr[bk].ap[:, n * 256:(n + 1) * 256], ALPHA, p.ap[:, 0:256],
                                ALU.mult, ALU.add, [xr[bk].buf, p.buf], [y[bk].buf])
                    for bk in range(NBK):
                        s_ = st[bk % 2]; yy = y[bk]
                        act(jk[:], yy.ap, AF.Copy, [yy.buf], [jkb, s_.buf], accum_out=s_.ap[:, 0:1])
                        act(jk[:], yy.ap, AF.Square, [yy.buf], [jkb, s_.buf], accum_out=s_.ap[:, 1:2])
                        ts("dve", s_.ap[:, 2:3], s_.ap[:, 0:1], 1.0 / D, None, ALU.mult, None, [s_.buf], [s_.buf])
                        tt("dve", s_.ap[:, 3:4], s_.ap[:, 2:3], s_.ap[:, 2:3], ALU.mult, [s_.buf], [s_.buf])
                        stt("dve", s_.ap[:, 4:5], s_.ap[:, 1:2], 1.0 / D, s_.ap[:, 3:4], ALU.mult, ALU.subtract, [s_.buf], [s_.buf])
                        act(s_.ap[:, 5:6], s_.ap[:, 4:5], AF.Ln, [s_.buf, epsTb], [s_.buf], bias=epsT[:, 0:1])
                        act(s_.ap[:, 5:6], s_.ap[:, 5:6], AF.Exp, [s_.buf], [s_.buf], scale=-0.5)
                        ts("dve", yy.ap, yy.ap, s_.ap[:, 2:3], s_.ap[:, 5:6], ALU.subtract, ALU.mult, [yy.buf, s_.buf], [yy.buf])
                        tt("pool", yy.ap, yy.ap, lng[:], ALU.mult, [yy.buf, lngb], [yy.buf])
                        tt("pool", yy.ap, yy.ap, lnb[:], ALU.add, [yy.buf, lnbb], [yy.buf])
                        r0 = t0 + bk * 128
                        c.dma("sp", dst[r0:r0 + 128, :], yy.ap, [yy.buf], [], yy.buf)
                        if not last:
                            c.dma("pool", xb_d[r0:r0 + 128, :], yy.ap, [yy.buf], [], yy.buf)
                c.end_phase()

        for l in range(NL):
            mem_kv(l)
            for s in range(NSEG):
                phase_a(l, s)
                phase_b(l, s, l == NL - 1)
        if dbg:
            pass
        c.barrier()
    return nc


def _consts():
    cst = np.zeros((128, C_END), np.float32)
    i = np.arange(128)
    cst[:, C_ID:C_ID + 128] = np.eye(128, dtype=np.float32)
    cst[:, C_UT:C_UT + 128] = (i[None, :] >= i[:, None]).astype(np.float32)
    cst[:, C_SM:C_SM + 128] = (i[:, None] > i[None, :]).astype(np.float32)
    cst[:, C_NEGM:C_NEGM + 128] = np.where(i[None, :] > i[:, None], -30000.0, 0.0)
    cst[:, C_NEGMT:C_NEGMT + 128] = np.where(i[:, None] > i[None, :], -30000.0, 0.0)
    cst[:, C_ONES:C_ONES + 128] = 1.0
    for h in range(12):
        gam = 1.0 - 2.0 ** (-5.0 - h)
        cst[:, C_QS + h] = gam ** (i + 1.0)
        cst[:, C_KS + h] = gam ** (-(i + 1.0)) * 128.0 ** -0.5
    half = 64
    invf = (10000.0 ** (-np.arange(half, dtype=np.float32) / half)).astype(np.float32)
    cst[:, C_INVF:C_INVF + 64] = invf[None, :]
    return cst


def _kc_layout(w, ncols_pad=None):
    n = w.shape[1]
    a = np.ascontiguousarray(w.reshape(KC, 128, n).transpose(1, 0, 2))
    if ncols_pad is not None and ncols_pad != n:
        o = np.zeros((128, KC, ncols_pad), np.float32)
        o[:, :, :n] = a
        return o
    return a


def prep_weights(inp, kinds):
    ws = {"consts": _consts()}
    for l, kind in enumerate(kinds):
        j = l // 2
        wg = np.zeros((16, 128, KC, GW), np.float32)
        if kind == "ret":
            w = np.asarray(inp["w_in_ret"][j])
            for h in range(12):
                cols = np.concatenate([np.arange(h * 128, (h + 1) * 128), 1536 + np.arange(h * 128, (h + 1) * 128),
                                       3072 + np.arange(h * 256, (h + 1) * 256), 7168 + np.arange(h * 256, (h + 1) * 256)])
                wg[h] = _kc_layout(w[:, cols], GW)
            ws["rng%d" % l] = np.ascontiguousarray(inp["ret_norm_g"][j], dtype=np.float32)
        else:
            w = np.asarray(inp["w_in_gdn"][j])
            cwl = np.zeros((128, 192), np.float32)
            cw = np.asarray(inp["conv_w"][j])
            for g in range(12):
                chans = [g * 128, 1536 + g * 128, 3072 + (2 * g) * 128, 3072 + (2 * g + 1) * 128]
                cols = np.concatenate([np.arange(ch, ch + 128) for ch in chans] + [7168 + np.arange(g * 256, (g + 1) * 256),
                                      np.array([11264 + 2 * g, 11264 + 2 * g + 1, 11288 + 2 * g, 11288 + 2 * g + 1])])
                wg[g] = _kc_layout(w[:, cols], GW)
                for ct, ch in enumerate(chans):
                    for jj in range(4):
                        cwl[:, g * 16 + ct * 4 + jj] = cw[jj, ch:ch + 128]
            ws["cw%d" % l] = cwl
            ws["gng%d" % l] = np.ascontiguousarray(inp["gdn_norm_g"][j], dtype=np.float32)
            ws["alog%d" % l] = np.ascontiguousarray(inp["a_log"][j], dtype=np.float32)
            ws["dtb%d" % l] = np.ascontiguousarray(inp["dt_bias"][j], dtype=np.float32)
        for m in range(4):
            cols = np.concatenate([6144 + np.arange(m * 256, (m + 1) * 256), 7168 + 3072 + np.arange(m * 256, (m + 1) * 256)])
            wg[12 + m] = _kc_layout(w[:, cols], GW)
        ws["wg%d" % l] = wg
        wo = np.asarray(inp["w_out"][l])
        ws["wo%d" % l] = np.ascontiguousarray(wo.reshape(32, 128, 8, 256).transpose(2, 1, 0, 3))
        wkv = np.asarray(inp["w_mem_kv"][l])
        ws["wkv%d" % l] = np.ascontiguousarray(wkv.reshape(KC, 128, 4, 512).transpose(2, 1, 0, 3))
        ws["lng%d" % l] = np.ascontiguousarray(inp["ln_g"][l], dtype=np.float32)
        ws["lnb%d" % l] = np.ascontiguousarray(inp["ln_b"][l], dtype=np.float32)
    return ws


def core_map(ws, x_b, mem_b, pos_b):
    T = x_b.shape[0]
    m = dict(ws)
    m["x"] = np.ascontiguousarray(x_b, dtype=np.float32)
    m["mem"] = np.ascontiguousarray(mem_b, dtype=np.float32)
    m["pos"] = np.ascontiguousarray(np.asarray(pos_b, dtype=np.int32).reshape(T // 128, 128).T)
    return m


KINDS = ["ret", "gdn", "ret", "gdn"]


def kernel(x, mem, positions, w_in_ret, ret_norm_g, w_in_gdn, conv_w, a_log, dt_bias, gdn_norm_g,
           w_mem_kv, w_out, ln_g, ln_b):
    inp = dict(w_in_ret=w_in_ret, ret_norm_g=ret_norm_g, w_in_gdn=w_in_gdn, conv_w=conv_w, a_log=a_log, dt_bias=dt_bias,
               gdn_norm_g=gdn_norm_g, w_mem_kv=w_mem_kv, w_out=w_out, ln_g=ln_g, ln_b=ln_b)
    x = np.asarray(x); mem = np.asarray(mem); positions = np.asarray(positions)
    B, S, _ = x.shape
    ws = prep_weights(inp, KINDS)
    nc = build(2048, S // 2048, KINDS)
    in_maps = [core_map(ws, x[b % B], mem[b % B], positions[b % B]) for b in range(8)]
    res = run_bass_kernel_spmd(nc, in_maps, core_ids=list(range(8)))
    return np.stack([res.results[b]["out"] for b in range(B)], axis=0).astype(np.float32)
```

```python
import contextlib
import os
import numpy as np
GSTOP = float(os.environ.get('GSTOP', '9'))
import concourse.bass as bass
import concourse.mybir as mybir
from concourse.bass_utils import run_bass_kernel_spmd

F32 = mybir.dt.float32
BF16 = mybir.dt.bfloat16
I32 = mybir.dt.int32
AF = mybir.ActivationFunctionType
ALU = mybir.AluOpType
AX = mybir.AxisListType

D = 2048
KC = 16
NMEM = 256
DEPTH = 4
ALPHA = (2.0 * DEPTH) ** 0.25
LN_EPS = 1e-5
NORM_EPS = 1e-6
GW = 772
TWO_PI = float(2 * np.pi)
C_ID, C_UT, C_SM, C_NEGM, C_NEGMT, C_ONES, C_QS, C_KS, C_INVF, C_END = 0, 128, 256, 384, 512, 640, 768, 780, 792, 856


class Buf:
    __slots__ = ("name", "w", "r", "dsem", "excl")

    def __init__(self, name, excl=False):
        self.name = name
        self.w = None
        self.r = []
        self.dsem = None
        self.excl = excl


class Ctx:
    def __init__(self, nc, es):
        self.nc = nc
        self.es = es
        self.eng = {"pe": nc.tensor, "act": nc.scalar, "dve": nc.vector, "pool": nc.gpsimd, "sp": nc.sync}
        self.sem = {}
        self.cnt = {}
        for k in self.eng:
            self.sem[k] = es.enter_context(nc.semaphore("s_" + k))
            self.cnt[k] = 0
        self.waited = {k: {} for k in self.eng}
        self.semobj = {k: self.sem[k] for k in self.eng}
        self.latest = {k: 0 for k in self.eng}
        self.ndsem = 0
        self.free_dsems = []
        self.in_phase = False
        self.phase_keys = []

    def _wait(self, e, ev):
        if ev is None:
            return
        key, val = ev
        if key == e:
            if e == "pe":
                return
            if self.cnt[e] - val >= 2:
                return
        if key not in self.eng:
            val = max(val, self.latest.get(key, val))
        if self.waited[e].get(key, 0) >= val:
            return
        self.waited[e][key] = val
        self.eng[e].wait_ge(self.semobj[key], val)

    def deps(self, e, reads, writes):
        for b in reads:
            self._wait(e, b.w)
            if b.excl:
                for ev in b.r:
                    if ev[0] != e:
                        self._wait(e, ev)
        for b in writes:
            self._wait(e, b.w)
            for ev in b.r:
                self._wait(e, ev)

    def record(self, ev, reads, writes):
        for b in writes:
            b.w = ev
            b.r = []
        for b in reads:
            b.r = [x for x in b.r if x[0] != ev[0]] + [ev]

    def op(self, e, fn, reads=(), writes=(), inc=True):
        self.deps(e, reads, writes)
        ins = fn()
        if inc:
            self.cnt[e] += 1
            ins.then_inc(self.sem[e], 1)
            self.latest[e] = self.cnt[e]
            ev = (e, self.cnt[e])
        else:
            ev = (e, self.cnt[e] + 1)
        self.record(ev, reads, writes)
        return ins

    def dsem_of(self, buf):
        if buf.dsem is None:
            if self.free_dsems:
                key = self.free_dsems.pop()
            else:
                self.ndsem += 1
                s = self.es.enter_context(self.nc.semaphore("d%d" % self.ndsem))
                key = "d%d" % self.ndsem
                self.semobj[key] = s
                self.latest[key] = 0
            buf.dsem = key
            if self.in_phase:
                self.phase_keys.append(key)
        return buf.dsem

    def begin_phase(self):
        self.in_phase = True
        self.phase_keys = []

    def end_phase(self):
        self.barrier()
        self.free_dsems.extend(self.phase_keys)
        self.phase_keys = []
        self.in_phase = False

    def dma(self, q, out, in_, reads, writes, sembuf, transpose=False):
        self.deps(q, reads, writes)
        key = self.dsem_of(sembuf)
        if transpose:
            ins = self.eng[q].dma_start_transpose(out=out, in_=in_)
        else:
            ins = self.eng[q].dma_start(out=out, in_=in_)
        ins.then_inc(self.semobj[key], 16)
        self.latest[key] += 16
        self.record((key, self.latest[key]), reads, writes)
        return ins

    def barrier(self, engines=("pe", "act", "dve", "pool", "sp")):
        for e in engines:
            for key, val in self.latest.items():
                if val <= 0 or key == e:
                    continue
                if self.waited[e].get(key, 0) >= val:
                    continue
                self.waited[e][key] = val
                self.eng[e].wait_ge(self.semobj[key], val)


class Reg:
    __slots__ = ("ap", "buf")

    def __init__(self, ap, buf):
        self.ap = ap
        self.buf = buf


def build(SEGT, NSEG, kinds, dbg=False):
    NB = SEGT // 128
    T = SEGT * NSEG
    NBT = T // 128
    NL = len(kinds)
    nc = bass.Bass("TRN2", target_bir_lowering=False)
    dt = nc.dram_tensor
    x_d = dt("x", [T, D], F32, kind="ExternalInput").ap()
    mem_d = dt("mem", [NMEM, D], F32, kind="ExternalInput").ap()
    pos_d = dt("pos", [128, NBT], I32, kind="ExternalInput").ap()
    cst_d = dt("consts", [128, C_END], F32, kind="ExternalInput").ap()
    wg_d, wo_d, wkv_d, lng_d, lnb_d, p1_d, p2_d, p3_d, p4_d = [], [], [], [], [], [], [], [], []
    for l in range(NL):
        wg_d.append(dt("wg%d" % l, [16, 128, KC, GW], F32, kind="ExternalInput").ap())
        wo_d.append(dt("wo%d" % l, [8, 128, 32, 256], F32, kind="ExternalInput").ap())
        wkv_d.append(dt("wkv%d" % l, [4, 128, KC, 512], F32, kind="ExternalInput").ap())
        lng_d.append(dt("lng%d" % l, [D], F32, kind="ExternalInput").ap())
        lnb_d.append(dt("lnb%d" % l, [D], F32, kind="ExternalInput").ap())
        if kinds[l] == "ret":
            p1_d.append(dt("rng%d" % l, [3072], F32, kind="ExternalInput").ap())
            p2_d.append(None); p3_d.append(None); p4_d.append(None)
        else:
            p1_d.append(dt("gng%d" % l, [128], F32, kind="ExternalInput").ap())
            p2_d.append(dt("cw%d" % l, [128, 192], F32, kind="ExternalInput").ap())
            p3_d.append(dt("alog%d" % l, [24], F32, kind="ExternalInput").ap())
            p4_d.append(dt("dtb%d" % l, [24], F32, kind="ExternalInput").ap())
    out_d = dt("out", [T, D], F32, kind="ExternalOutput").ap()
    xres_d = dt("xres", [T, D], F32).ap()
    xb_d = dt("xb", [T, D], BF16).ap()
    br_d = dt("br", [T, 4096], BF16).ap()
    memb_d = dt("memb", [NMEM, D], BF16).ap()
    st_d = [dt("st%d" % l, [24, 128, 256], F32).ap() for l in range(NL)]
    wob_d = [dt("wob%d" % l, [8, 128, 32, 256], BF16).ap() for l in range(NL)]
    cst8_d = [dt("cvst%d" % l, [12, 128, 12], F32).ap() for l in range(NL)]
    if dbg:
        dbg_d = dt("dbg_br", [T, 4096], F32, kind="ExternalOutput").ap()

    with contextlib.ExitStack() as es:
        c = Ctx(nc, es)

        _uid = [0]

        def uname(name):
            _uid[0] += 1
            return "%s_u%d" % (name, _uid[0])

        def sbt(stack, name, shape, dtype):
            t = stack.enter_context(nc.sbuf_tensor(uname(name), shape, dtype))
            return t, Buf(name)

        def act(out, in_, func, reads, writes, **kw):
            return c.op("act", lambda: nc.scalar.activation(out=out, in_=in_, func=func, **kw), reads, writes)

        def tt(e, out, in0, in1, op, reads, writes):
            eng = c.eng[e]
            return c.op(e, lambda: eng.tensor_tensor(out=out, in0=in0, in1=in1, op=op), reads, writes)

        def ts(e, out, in0, s1, s2, op0, op1, reads, writes):
            eng = c.eng[e]
            if op1 is None:
                return c.op(e, lambda: eng.tensor_scalar(out=out, in0=in0, scalar1=s1, scalar2=None, op0=op0), reads, writes)
            return c.op(e, lambda: eng.tensor_scalar(out=out, in0=in0, scalar1=s1, scalar2=s2, op0=op0, op1=op1), reads, writes)

        def stt(e, out, in0, scalar, in1, op0, op1, reads, writes):
            eng = c.eng[e]
            return c.op(e, lambda: eng.scalar_tensor_tensor(out=out, in0=in0, scalar=scalar, in1=in1, op0=op0, op1=op1), reads, writes)

        def cp(e, out, in_, reads, writes):
            eng = c.eng[e]
            return c.op(e, lambda: eng.tensor_copy(out=out, in_=in_), reads, writes)

        def mm(out, pairs, reads, writes):
            n = len(pairs)
            for i, (l_, r_) in enumerate(pairs):
                last = i == n - 1
                c.op("pe", lambda: nc.tensor.matmul(out, l_, r_, start=(i == 0), stop=last),
                     reads if (last or i == 0) else (), writes if (last or i == 0) else (), inc=last)

        def trp(out, in_, ident, reads, writes):
            return c.op("pe", lambda: nc.tensor.transpose(out, in_, ident), reads, writes)

        cst, cstb = sbt(es, "cst", [128, C_END], F32)
        c.dma("sp", cst[:], cst_d, [], [cstb], cstb)
        idbf, idbfb = sbt(es, "idbf", [128, 128], BF16)
        cp("dve", idbf[:], cst[:, C_ID:C_ID + 128], [cstb], [idbfb])
        ID32 = cst[:, C_ID:C_ID + 128]
        UT32 = cst[:, C_UT:C_UT + 128]
        SM32 = cst[:, C_SM:C_SM + 128]
        ONES32 = cst[:, C_ONES:C_ONES + 128]
        onebf, onebfb = sbt(es, "onebf", [128, 128], BF16)
        cp("dve", onebf[:], ONES32, [cstb], [onebfb])
        negmbf, negmbfb = sbt(es, "negmbf", [128, 256], BF16)
        cp("dve", negmbf[:], cst[:, C_NEGM:C_NEGM + 256], [cstb], [negmbfb])

        dummy = Buf("dummy")
        wobD = Buf("wobD")
        for r0 in range(0, T, 512):
            c.dma("pool", xb_d[r0:r0 + 512, :], x_d[r0:r0 + 512, :], [], [], dummy)
        c.dma("pool", memb_d, mem_d, [], [], dummy)
        c.barrier(["sp", "pool"])
        memT, memTb = sbt(es, "memT", [128, KC, NMEM], BF16)
        for kc in range(KC):
            c.dma("sp", memT[:, kc, :], memb_d[:, kc * 128:(kc + 1) * 128], [], [memTb], memTb, transpose=True)
        mkT, mkTb = sbt(es, "mkT", [128, 8, NMEM], BF16)
        mv, mvb = sbt(es, "mv", [128, 2, 1024], BF16)

        has_ret = "ret" in kinds
        rot_d = dt("rot_d", [3, 128, NBT, 64], F32).ap()
        if has_ret:
            c.begin_phase()
            with contextlib.ExitStack() as ps:
                cosT, cosb = sbt(ps, "cosT0", [128, NBT, 64], F32)
                sinT, sinb = sbt(ps, "sinT0", [128, NBT, 64], F32)
                nsinT, nsinb = sbt(ps, "nsinT0", [128, NBT, 64], F32)
                pi_, pib = sbt(ps, "posi", [128, NBT], I32)
                pf_, pfb = sbt(ps, "posf", [128, NBT], F32)
                ang, angb = sbt(ps, "ang", [128, NBT, 64], F32)
                nf, nfb = sbt(ps, "nf", [128, NBT, 64], F32)
                ni, nib = sbt(ps, "ni", [128, NBT, 64], I32)
                c.dma("sp", pi_[:], pos_d, [], [pib], pib)
                cp("dve", pf_[:], pi_[:], [pib], [pfb])
                invf = cst[:, C_INVF:C_INVF + 64]
                for b in range(NBT):
                    ts("dve", ang[:, b, :], invf, pf_[:, b:b + 1], None, ALU.mult, None, [cstb, pfb], [angb])

                def reduce_and_sin(dst, dstb, shift):
                    ts("dve", nf[:], ang[:], shift, 1.0 / TWO_PI, ALU.add, ALU.mult, [angb], [nfb])
                    cp("dve", ni[:], nf[:], [nfb], [nib])
                    cp("dve", nf[:], ni[:], [nib], [nfb])
                    stt("dve", nf[:], nf[:], -TWO_PI, ang[:], ALU.mult, ALU.add, [nfb, angb], [nfb])
                    if shift != 0.0:
                        ts("dve", nf[:], nf[:], shift, None, ALU.add, None, [nfb], [nfb])
                    ni_f = ni[:].bitcast(F32)
                    ts("dve", ni_f, nf[:], float(np.pi), -TWO_PI, ALU.is_gt, ALU.mult, [nfb], [nib])
                    tt("dve", nf[:], nf[:], ni_f, ALU.add, [nfb, nib], [nfb])
                    ts("dve", ni_f, nf[:], -float(np.pi), TWO_PI, ALU.is_lt, ALU.mult, [nfb], [nib])
                    tt("dve", nf[:], nf[:], ni_f, ALU.add, [nfb, nib], [nfb])
                    act(dst[:], nf[:], AF.Sin, [nfb], [dstb])

                reduce_and_sin(sinT, sinb, 0.0)
                reduce_and_sin(cosT, cosb, float(np.pi / 2))
                ts("dve", nsinT[:], sinT[:], -1.0, None, ALU.mult, None, [sinb], [nsinb])
                c.dma("sp", rot_d[0], cosT[:], [cosb], [], cosb)
                c.dma("sp", rot_d[1], sinT[:], [sinb], [], sinb)
                c.dma("sp", rot_d[2], nsinT[:], [nsinb], [], nsinb)
                c.end_phase()
        c.barrier()

        def wtiles(stack, name, shape, dtype, n=2):
            return [Reg(*_mk(stack, "%s%d" % (name, i), shape, dtype)) for i in range(n)]

        def _mk(stack, name, shape, dtype):
            t, b = sbt(stack, name, shape, dtype)
            return t[:], b

        def load_bcast(stack, name, src, n, q="sp"):
            t, b = sbt(stack, name, [128, n], F32)
            c.dma(q, t[:], src.partition_broadcast(128), [], [b], b)
            return t, b

        def mem_kv(l):
            c.begin_phase()
            with contextlib.ExitStack() as ps:
                wk = wtiles(ps, "wkv", [128, KC, 512], BF16, 2)
                pk = [Reg(ps.enter_context(nc.psum_tensor(uname("pkv"), [128, 512], F32))[:], Buf("pkv%d" % i, excl=True)) for i in range(2)]
                it = 0
                for ch in range(4):
                    w = wk[ch % 2]
                    for q4 in range(4):
                        c.dma("pool", w.ap[:, q4 * 4:(q4 + 1) * 4, :], wkv_d[l][ch][:, q4 * 4:(q4 + 1) * 4, :], [], [w.buf], w.buf)
                    if ch < 2:
                        for ct in range(4):
                            p = pk[it % 2]; it += 1
                            mm(p.ap[:, 0:NMEM], [(w.ap[:, kc, ct * 128:(ct + 1) * 128], memT[:, kc, :]) for kc in range(KC)],
                               [w.buf, memTb], [p.buf])
                            act(mkT[:, ch * 4 + ct, :], p.ap[:, 0:NMEM], AF.Copy, [p.buf], [mkTb])
                    else:
                        for mc in range(2):
                            p = pk[it % 2]; it += 1
                            mm(p.ap[:, :], [(memT[:, kc, mc * 128:(mc + 1) * 128], w.ap[:, kc, :]) for kc in range(KC)],
                               [w.buf, memTb], [p.buf])
                            act(mv[:, mc, (ch - 2) * 512:(ch - 1) * 512], p.ap[:, :], AF.Copy, [p.buf], [mvb])
                c.end_phase()

        def phase_a(l, s):
            kind = kinds[l]
            tok0 = s * SEGT
            c.begin_phase()
            with contextlib.ExitStack() as ps:
                xT, _xTb0 = sbt(ps, "xT", [128, KC, SEGT], BF16)
                xTbs = [Buf("xT%d" % i) for i in range((SEGT + 511) // 512)]
                for r0 in range(0, SEGT, 512):
                    r1 = min(SEGT, r0 + 512)
                    for kc in range(KC):
                        c.dma("sp", xT[:, kc, r0:r1], xb_d[tok0 + r0:tok0 + r1, kc * 128:(kc + 1) * 128], [], [xTbs[r0 // 512]], xTbs[r0 // 512], transpose=True)
                Wt = wtiles(ps, "W", [128, KC, GW], BF16, 2)
                PS = [ps.enter_context(nc.psum_tensor(uname("ps"), [128, 512], F32)) for i in range(7)]
                PSB = ps.enter_context(nc.psum_tensor(uname("psb"), [128, 1024], BF16))
                _regs = {}
                _bankbuf = {}

                def R(bank, c0, c1):
                    k = (bank, c0, c1)
                    if k not in _regs:
                        if bank not in _bankbuf:
                            _bankbuf[bank] = Buf("psbank_%s" % bank, excl=True)
                        if bank == "b":
                            _regs[k] = Reg(PSB[:, c0:c1], _bankbuf[bank])
                        else:
                            _regs[k] = Reg(PS[bank][:, c0:c1], _bankbuf[bank])
                    return _regs[k]

                def load_w(g, ncols):
                    w = Wt[g % 2]
                    for q4 in range(4):
                        c.dma("pool", w.ap[:, q4 * 4:(q4 + 1) * 4, 0:ncols], wg_d[l][g][:, q4 * 4:(q4 + 1) * 4, 0:ncols], [], [w.buf], w.buf)
                    return w

                def store_branch(t, blk, col0, ncol):
                    r0 = tok0 + blk * 128
                    c.dma("sp", br_d[r0:r0 + 128, col0:col0 + ncol], t.ap, [t.buf], [], t.buf)

                gcols = [768] * 12 + [512] * 4 if kind == "ret" else [GW] * 12 + [512] * 4
                load_w(0, gcols[0])
                if s == 0:
                    for n8 in range(8):
                        c.dma("pool", wob_d[l][n8], wo_d[l][n8], [], [], wobD)

                st4 = wtiles(ps, "st4", [128, 16], F32, 2)
                e_t = wtiles(ps, "e_t", [128, 256], F32, 2)
                zs_t = wtiles(ps, "zs_t", [128, 256], F32, 2)
                osb = wtiles(ps, "osb", [128, 256], F32, 2)
                junk = wtiles(ps, "junk", [128, 256], BF16, 2)
                brt = wtiles(ps, "brt", [128, 256], BF16, 3)

                def gate_from_z(zreg, it, width=256):
                    e = e_t[it % 2]; zs = zs_t[it % 2]
                    act(e.ap[:, 0:width], zreg.ap, AF.Exp, [zreg.buf], [e.buf], scale=-1.0)
                    ts("dve", e.ap[:, 0:width], e.ap[:, 0:width], 1.0, None, ALU.add, None, [e.buf], [e.buf])
                    c.op("dve", lambda: nc.vector.reciprocal(out=e.ap[:, 0:width], in_=e.ap[:, 0:width]), [e.buf], [e.buf])
                    tt("dve", zs.ap[:, 0:width], zreg.ap, e.ap[:, 0:width], ALU.mult, [zreg.buf, e.buf], [zs.buf])
                    return zs

                def rstd_from(stt_, col_in, col_out, scale, eps):
                    act(stt_.ap[:, col_out:col_out + 1], stt_.ap[:, col_in:col_in + 1], AF.Ln, [stt_.buf, epsTb], [stt_.buf], scale=scale, bias=epsT[:, eps:eps + 1])
                    act(stt_.ap[:, col_out:col_out + 1], stt_.ap[:, col_out:col_out + 1], AF.Exp, [stt_.buf], [stt_.buf], scale=-0.5)

                epsT, epsTb = sbt(ps, "epsT", [128, 2], F32)
                c.op("dve", lambda: nc.vector.memset(epsT[:, 0:1], NORM_EPS), [], [epsTb])
                c.op("dve", lambda: nc.vector.memset(epsT[:, 1:2], LN_EPS), [], [epsTb])

                git = 0
                if kind == "ret":
                    gam, gamb = load_bcast(ps, "rng", p1_d[l], 3072)
                    b0 = tok0 // 128
                    cosT, cosb = sbt(ps, "cosT", [128, NB, 64], F32)
                    sinT, sinb = sbt(ps, "sinT", [128, NB, 64], F32)
                    nsinT, nsinb = sbt(ps, "nsinT", [128, NB, 64], F32)
                    c.dma("sp", cosT[:], rot_d[0][:, b0:b0 + NB, :], [], [cosb], cosb)
                    c.dma("sp", sinT[:], rot_d[1][:, b0:b0 + NB, :], [], [sinb], sinb)
                    c.dma("sp", nsinT[:], rot_d[2][:, b0:b0 + NB, :], [], [nsinb], nsinb)
                    qks = wtiles(ps, "qks", [128, 256], F32, 2)
                    vbf = wtiles(ps, "vbf", [128, 256], BF16, 2)
                    rA = wtiles(ps, "rA", [128, 256], F32, 2)
                    rB = wtiles(ps, "rB", [128, 256], F32, 2)
                    rot = wtiles(ps, "rot", [128, 256], BF16, 2)
                    qkT = wtiles(ps, "qkT", [128, 256], BF16, 2)
                    PT = wtiles(ps, "PT", [128, 128], BF16, 2)
                    Y, Yb = sbt(ps, "Y", [128, 256], F32)
                    Sbf = wtiles(ps, "Sbf", [128, 256], BF16, 2)
                    for h in range(12):
                        w = Wt[h % 2]
                        load_w(h + 1, gcols[h + 1])
                        g128 = float((1.0 - 2.0 ** (-5 - h)) ** 128)
                        have_state = s > 0
                        if have_state:
                            c.dma("sp", Y[:], st_d[l][h], [], [Yb], Yb)
                            sb0 = Sbf[0]
                            act(sb0.ap, Y[:], AF.Copy, [Yb], [sb0.buf], scale=g128)
                        git0 = git; git += NB
                        hs = [have_state]

                        def a_proj(blk):
                            it = git0 + blk
                            tok = slice(blk * 128, (blk + 1) * 128)
                            gb = blk
                            PA = R(it % 2, 0, 512)
                            PZ = R(2 + it % 2, 0, 256)
                            mm(PA.ap, [(xT[:, kc, tok], w.ap[:, kc, 0:512]) for kc in range(KC)], [xTbs[blk // 4], w.buf], [PA.buf])
                            mm(PZ.ap, [(xT[:, kc, tok], w.ap[:, kc, 512:768]) for kc in range(KC)], [xTbs[blk // 4], w.buf], [PZ.buf])

                        def a_rest(blk):
                            it = git0 + blk
                            tok = slice(blk * 128, (blk + 1) * 128)
                            gb = blk
                            PA = R(it % 2, 0, 512)
                            PZ = R(2 + it % 2, 0, 256)
                            qk = qks[it % 2]; v = vbf[it % 2]
                            act(qk.ap[:, 0:128], PA.ap[:, 0:128], AF.Copy, [PA.buf, cstb], [qk.buf], scale=cst[:, C_QS + h:C_QS + h + 1])
                            act(qk.ap[:, 128:256], PA.ap[:, 128:256], AF.Copy, [PA.buf, cstb], [qk.buf], scale=cst[:, C_KS + h:C_KS + h + 1])
                            act(v.ap, PA.ap[:, 256:512], AF.Copy, [PA.buf], [v.buf])
                            gate_from_z(PZ, it)
                            yield
                            a_ = rA[it % 2]; b_ = rB[it % 2]; ro = rot[it % 2]
                            qk4 = qk.ap.rearrange("p (a b d) -> p a b d", a=2, b=2)
                            a4 = a_.ap.rearrange("p (a b d) -> p a b d", a=2, b=2)
                            b4 = b_.ap.rearrange("p (a b d) -> p a b d", a=2, b=2)
                            cosb4 = cosT[:, gb, :].unsqueeze(1).unsqueeze(1).to_broadcast([128, 2, 2, 64])
                            sinb3 = sinT[:, gb, :].unsqueeze(1).to_broadcast([128, 2, 64])
                            nsinb3 = nsinT[:, gb, :].unsqueeze(1).to_broadcast([128, 2, 64])
                            tt("dve", a4, qk4, cosb4, ALU.mult, [qk.buf, cosb], [a_.buf])
                            tt("pool", b4[:, :, 0, :], qk4[:, :, 1, :], nsinb3, ALU.mult, [qk.buf, nsinb], [b_.buf])
                            tt("pool", b4[:, :, 1, :], qk4[:, :, 0, :], sinb3, ALU.mult, [qk.buf, sinb], [b_.buf])
                            yield
                            tt("dve", ro.ap, a_.ap, b_.ap, ALU.add, [a_.buf, b_.buf], [ro.buf])
                            yield
                            TR = R("b", 0, 256)
                            trp(TR.ap[:, 0:128], ro.ap[:, 0:128], idbf[:], [ro.buf, idbfb], [TR.buf])
                            trp(TR.ap[:, 128:256], ro.ap[:, 128:256], idbf[:], [ro.buf, idbfb], [TR.buf])
                            yield
                            qt = qkT[it % 2]
                            act(qt.ap, TR.ap, AF.Copy, [TR.buf], [qt.buf])
                            yield
                            SC = R(4, 0, 128)
                            mm(SC.ap, [(qt.ap[:, 128:256], qt.ap[:, 0:128])], [qt.buf], [SC.buf])
                            yield
                            pt = PT[it % 2]
                            tt("dve", pt.ap, SC.ap, UT32, ALU.mult, [SC.buf, cstb], [pt.buf])
                            yield

                        def b_part(blk):
                            it = git0 + blk
                            tok = slice(blk * 128, (blk + 1) * 128)
                            gb = blk
                            PA = R(it % 2, 0, 512)
                            PZ = R(2 + it % 2, 0, 256)
                            qk = qks[it % 2]; v = vbf[it % 2]
                            a_ = rA[it % 2]; b_ = rB[it % 2]; ro = rot[it % 2]
                            qt = qkT[it % 2]; pt = PT[it % 2]
                            O = R(5, 0, 256)
                            sprev = Sbf[blk % 2]
                            if hs[0]:
                                mm(O.ap, [(pt.ap, v.ap), (qt.ap[:, 0:128], sprev.ap)], [pt.buf, v.buf, qt.buf, sprev.buf], [O.buf])
                            else:
                                mm(O.ap, [(pt.ap, v.ap)], [pt.buf, v.buf], [O.buf])
                            KV = R(6, 0, 256)
                            mm(KV.ap, [(ro.ap[:, 128:256], v.ap)], [ro.buf, v.buf], [KV.buf])
                            yield
                            if hs[0]:
                                stt("dve", Y[:], Y[:], g128, KV.ap, ALU.mult, ALU.add, [Yb, KV.buf], [Yb])
                            else:
                                cp("dve", Y[:], KV.ap, [KV.buf], [Yb])
                            hs[0] = True
                            snext = Sbf[(blk + 1) % 2]
                            act(snext.ap, Y[:], AF.Copy, [Yb], [snext.buf], scale=g128)
                            st = st4[it % 2]; o_ = osb[it % 2]; jk = junk[it % 2]
                            act(o_.ap, O.ap, AF.Copy, [O.buf], [o_.buf, st.buf], accum_out=st.ap[:, 0:1])
                            act(jk.ap, O.ap, AF.Square, [O.buf], [jk.buf, st.buf], accum_out=st.ap[:, 1:2])
                            yield
                            ts("dve", st.ap[:, 2:3], st.ap[:, 0:1], 1.0 / 256, None, ALU.mult, None, [st.buf], [st.buf])
                            tt("dve", st.ap[:, 3:4], st.ap[:, 2:3], st.ap[:, 2:3], ALU.mult, [st.buf], [st.buf])
                            stt("dve", st.ap[:, 4:5], st.ap[:, 1:2], 1.0 / 256, st.ap[:, 3:4], ALU.mult, ALU.subtract, [st.buf], [st.buf])
                            yield
                            rstd_from(st, 4, 5, 1.0, 0)
                            yield
                            ts("dve", o_.ap, o_.ap, st.ap[:, 2:3], st.ap[:, 5:6], ALU.subtract, ALU.mult, [o_.buf, st.buf], [o_.buf])
                            zs = zs_t[it % 2]
                            yield
                            tt("pool", o_.ap, o_.ap, gam[:, h * 256:(h + 1) * 256], ALU.mult, [o_.buf, gamb], [o_.buf])
                            yield
                            bt = brt[it % 3]
                            tt("dve", bt.ap, o_.ap, zs.ap, ALU.mult, [o_.buf, zs.buf], [bt.buf])
                            store_branch(bt, blk, h * 256, 256)

                        def rr_run(gl):
                            alive = list(gl)
                            while alive:
                                nxt = []
                                for gen_ in alive:
                                    try:
                                        next(gen_)
                                        nxt.append(gen_)
                                    except StopIteration:
                                        pass
                                alive = nxt

                        a_proj(0)
                        rr_run([a_rest(0)])
                        for blk in range(NB):
                            gb_ = b_part(blk)
                            next(gb_)
                            if blk + 1 < NB:
                                a_proj(blk + 1)
                                rr_run([gb_, a_rest(blk + 1)])
                            else:
                                rr_run([gb_])
                        c.dma("sp", st_d[l][h], Y[:], [Yb], [], Yb)
                else:
                    gdn_heads(ps, l, s, tok0, xT, xTbs, Wt, load_w, gcols, R, store_branch, gate_from_z, rstd_from,
                              st4, e_t, zs_t, osb, junk, brt)
                    git = 1000
                mqT = wtiles(ps, "mqT", [128, 2, 128], BF16, 2)
                pbf = wtiles(ps, "pbf", [128, 256], BF16, 2)
                pTt = wtiles(ps, "pTt", [128, 256], BF16, 2)
                for m in range(4):
                    g = 12 + m
                    w = Wt[g % 2]
                    if g + 1 < 16:
                        load_w(g + 1, gcols[g + 1])
                    for blk in range(NB):
                        it = git; git += 1
                        tok = slice(blk * 128, (blk + 1) * 128)
                        PQ = R(it % 2, 0, 256)
                        PZ = R(2 + it % 2, 0, 256)
                        for hf in range(2):
                            mm(PQ.ap[:, hf * 128:(hf + 1) * 128], [(w.ap[:, kc, hf * 128:(hf + 1) * 128], xT[:, kc, tok]) for kc in range(KC)],
                               [xTbs[blk // 4], w.buf], [PQ.buf])
                        mm(PZ.ap, [(xT[:, kc, tok], w.ap[:, kc, 256:512]) for kc in range(KC)], [xTbs[blk // 4], w.buf], [PZ.buf])
                        mq = mqT[it % 2]
                        act(mq.ap.rearrange("p a b -> p (a b)"), PQ.ap, AF.Copy, [PQ.buf], [mq.buf])
                        SC = R(4, 0, 256)
                        mm(SC.ap, [(mq.ap[:, hf, :], mkT[:, m * 2 + hf, :]) for hf in range(2)], [mq.buf, mkTb], [SC.buf])
                        st = st4[it % 2]
                        c.op("dve", lambda: nc.vector.reduce_max(out=st.ap[:, 0:1], in_=SC.ap, axis=AX.X), [SC.buf], [st.buf])
                        ts("dve", st.ap[:, 1:2], st.ap[:, 0:1], -1.0 / 16, None, ALU.mult, None, [st.buf], [st.buf])
                        pb_ = pbf[it % 2]
                        act(pb_.ap, SC.ap, AF.Exp, [SC.buf, st.buf], [pb_.buf, st.buf], scale=1.0 / 16, bias=st.ap[:, 1:2], accum_out=st.ap[:, 2:3])
                        TR = R("b", 0, 256)
                        trp(TR.ap[:, 0:128], pb_.ap[:, 0:128], idbf[:], [pb_.buf, idbfb], [TR.buf])
                        trp(TR.ap[:, 128:256], pb_.ap[:, 128:256], idbf[:], [pb_.buf, idbfb], [TR.buf])
                        pT_ = pTt[it % 2]
                        cp("dve", pT_.ap, TR.ap, [TR.buf], [pT_.buf])
                        O = R(5, 0, 256)
                        mm(O.ap, [(pT_.ap[:, mc * 128:(mc + 1) * 128], mv[:, mc, m * 256:(m + 1) * 256]) for mc in range(2)],
                           [pT_.buf, mvb], [O.buf])
                        c.op("dve", lambda: nc.vector.reciprocal(out=st.ap[:, 3:4], in_=st.ap[:, 2:3]), [st.buf], [st.buf])
                        zs = gate_from_z(PZ, it)
                        bt = brt[it % 3]
                        stt("dve", bt.ap, O.ap, st.ap[:, 3:4], zs.ap, ALU.mult, ALU.mult, [O.buf, st.buf, zs.buf], [bt.buf])
                        store_branch(bt, blk, 3072 + m * 256, 256)
                c.end_phase()

        def gdn_heads(ps, l, s, tok0, xT, xTbs, Wt, load_w, gcols, R, store_branch, gate_from_z, rstd_from,
                      st4, e_t, zs_t, osb, junk, brt):
            TT = min(512, SEGT)
            NBK = TT // 128
            cwt, cwtb = sbt(ps, "cwt", [128, 192], F32)
            c.dma("sp", cwt[:], p2_d[l], [], [cwtb], cwtb)
            negA, negAb = load_bcast(ps, "negA", p3_d[l], 24)
            act(negA[:], negA[:], AF.Exp, [negAb], [negAb])
            ts("dve", negA[:], negA[:], -1.0, None, ALU.mult, None, [negAb], [negAb])
            dtb, dtbb = load_bcast(ps, "dtb", p4_d[l], 24)
            gg, ggb = sbt(ps, "gg", [128, 256], F32)
            c.dma("sp", gg[:, 0:128], p1_d[l].partition_broadcast(128), [], [ggb], ggb)
            c.dma("sp", gg[:, 128:256], p1_d[l].partition_broadcast(128), [], [ggb], ggb)
            epsG, epsGb = sbt(ps, "epsG", [128, 1], F32)
            c.op("dve", lambda: nc.vector.memset(epsG[:], NORM_EPS), [], [epsGb])
            hraw, hrawb = sbt(ps, "hraw", [128, 4, 3 + TT], F32)
            acc, accb = sbt(ps, "acc", [128, 2, TT], F32)
            etmp, etmpb = sbt(ps, "etmp", [128, TT], F32)
            cvo, cvob = sbt(ps, "cvo", [128, 4, TT], BF16)
            qsq, qsqb = sbt(ps, "qsq", [128, 2, TT], BF16)
            S32, S32b = sbt(ps, "S32", [128, 2, 128], F32)
            Sbf = wtiles(ps, "gSbf", [128, 2, 128], BF16, 2)
            scal = wtiles(ps, "scal", [128, 32], F32, 4)
            gt_ = wtiles(ps, "gt", [128, 2], F32, 4)
            ez_ = wtiles(ps, "ez", [128, 256], F32, 2)
            zsb_ = wtiles(ps, "zsb", [128, 256], F32, 4)
            Gbt = wtiles(ps, "Gbt", [128, 128], F32, 2)
            ngbt = wtiles(ps, "ngbt", [128, 128], F32, 2)
            gbtt = wtiles(ps, "gbtt", [128, 128], F32, 2)
            Dm = wtiles(ps, "Dm", [128, 2, 128], F32, 2)
            DTm = wtiles(ps, "DTm", [128, 2, 128], F32, 2)
            DSm = wtiles(ps, "DSm", [128, 2, 128], F32, 2)
            khat_ = wtiles(ps, "khat", [128, 128], BF16, 2)
            khT_ = wtiles(ps, "khT", [128, 128], BF16, 4)
            khT2_ = wtiles(ps, "khT2", [128, 128], BF16, 2)
            vb_ = wtiles(ps, "vb", [128, 2, 128], F32, 4)
            ktl_ = wtiles(ps, "ktl", [128, 2, 128], BF16, 4)
            XD = wtiles(ps, "XD", [128, 2, 384], F32, 2)
            QKD = wtiles(ps, "QKD", [128, 2, 128], BF16, 4)
            TTt = wtiles(ps, "TTt", [128, 2, 128], BF16, 4)
            r_ = wtiles(ps, "r_", [128, 2, 128], BF16, 2)
            qSs = wtiles(ps, "qSs", [128, 2, 128], F32, 2)
            vn_ = wtiles(ps, "vn", [128, 2, 128], BF16, 2)
            ones_row = cst[0:1, C_ONES:C_ONES + 128]
            git = 0
            for g in range(12):
                w = Wt[g % 2]
                load_w(g + 1, gcols[g + 1])
                if s > 0:
                    for h in range(2):
                        c.dma("sp", S32[:, h, :], st_d[l][2 * g + h][:, 0:128], [], [S32b], S32b)
                    c.dma("sp", hraw[:, :, 0:3], cst8_d[l][g].rearrange("p (a b) -> p a b", a=4), [], [hrawb], hrawb)
                else:
                    c.op("dve", lambda: nc.vector.memset(S32[:], 0.0), [], [S32b])
                    c.op("dve", lambda: nc.vector.memset(hraw[:, :, 0:3], 0.0), [], [hrawb])
                cp("dve", Sbf[0].ap, S32[:], [S32b], [Sbf[0].buf])
                sidx = 0
                for tt_i in range(SEGT // TT):
                    t0 = tt_i * TT
                    for ct in range(4):
                        PJ = R(1, 0, TT)
                        mm(PJ.ap, [(w.ap[:, kc, ct * 128:(ct + 1) * 128], xT[:, kc, t0:t0 + TT]) for kc in range(KC)], [xTbs[tt_i], w.buf], [PJ.buf])
                        act(hraw[:, ct, 3:3 + TT], PJ.ap, AF.Copy, [PJ.buf], [hrawb])
                        cb = g * 16 + ct * 4
                        ts("dve", acc[:, ct % 2, :], hraw[:, ct, 0:TT], cwt[:, cb:cb + 1], None, ALU.mult, None, [hrawb, cwtb], [accb])
                        for jj in range(1, 4):
                            stt("dve", acc[:, ct % 2, :], hraw[:, ct, jj:jj + TT], cwt[:, cb + jj:cb + jj + 1], acc[:, ct % 2, :], ALU.mult, ALU.add,
                                [hrawb, cwtb, accb], [accb])
                        act(etmp[:], acc[:, ct % 2, :], AF.Exp, [accb], [etmpb], scale=-1.0)
                        ts("dve", etmp[:], etmp[:], 1.0, None, ALU.add, None, [etmpb], [etmpb])
                        c.op("dve", lambda: nc.vector.reciprocal(out=etmp[:], in_=etmp[:]), [etmpb], [etmpb])
                        tt("dve", cvo[:, ct, :], acc[:, ct % 2, :], etmp[:], ALU.mult, [accb, etmpb], [cvob])
                        if ct < 2:
                            act(qsq[:, ct, :], cvo[:, ct, :], AF.Square, [cvob], [qsqb])
                    cp("dve", hraw[:, :, 0:3], hraw[:, :, TT:TT + 3], [hrawb], [hrawb])
                    def pre(bk, tt_i=tt_i, t0=t0):
                        blk = tt_i * NBK + bk
                        tb = slice(bk * 128, (bk + 1) * 128)
                        tokb = slice(t0 + bk * 128, t0 + (bk + 1) * 128)
                        par = bk % 2
                        PZ = R(par, 0, 260)
                        mm(PZ.ap, [(xT[:, kc, tokb], w.ap[:, kc, 512:772]) for kc in range(KC)], [xTbs[tt_i], w.buf], [PZ.buf])
                        TRK = R("b", par * 512, par * 512 + 384)
                        for i3 in range(3):
                            trp(TRK.ap[:, i3 * 128:(i3 + 1) * 128], cvo[:, 1 + i3, tb], idbf[:], [cvob, idbfb], [TRK.buf])
                        SM = R(par, 264, 272)
                        mm(SM.ap[:, 0:1], [(qsq[:, 0, tb], onebf[:, 0:1])], [qsqb, onebfb], [SM.buf])
                        mm(SM.ap[:, 1:2], [(qsq[:, 1, tb], onebf[:, 0:1])], [qsqb, onebfb], [SM.buf])
                        yield
                        sc = scal[bk]; gt = gt_[bk]
                        act(sc.ap[:, 0:2], SM.ap[:, 0:2], AF.Ln, [SM.buf, epsGb], [sc.buf], bias=epsG[:, 0:1])
                        act(sc.ap[:, 0:2], sc.ap[:, 0:2], AF.Exp, [sc.buf], [sc.buf], scale=-0.5)
                        tt("dve", sc.ap[:, 2:4], PZ.ap[:, 256:258], dtb[:, 2 * g:2 * g + 2], ALU.add, [PZ.buf, dtbb], [sc.buf])
                        act(sc.ap[:, 2:4], sc.ap[:, 2:4], AF.Exp, [sc.buf], [sc.buf])
                        act(sc.ap[:, 2:4], sc.ap[:, 2:4], AF.Ln, [sc.buf], [sc.buf], bias=1.0)
                        tt("dve", gt.ap, sc.ap[:, 2:4], negA[:, 2 * g:2 * g + 2], ALU.mult, [sc.buf, negAb], [gt.buf])
                        act(sc.ap[:, 4:6], PZ.ap[:, 258:260], AF.Exp, [PZ.buf], [sc.buf], scale=-1.0)
                        ts("dve", sc.ap[:, 4:6], sc.ap[:, 4:6], 1.0, None, ALU.add, None, [sc.buf], [sc.buf])
                        c.op("dve", lambda: nc.vector.reciprocal(out=sc.ap[:, 4:6], in_=sc.ap[:, 4:6]), [sc.buf], [sc.buf])
                        e = ez_[bk % 2]; zs = zsb_[bk]
                        act(e.ap, PZ.ap[:, 0:256], AF.Exp, [PZ.buf], [e.buf], scale=-1.0)
                        ts("dve", e.ap, e.ap, 1.0, None, ALU.add, None, [e.buf], [e.buf])
                        c.op("dve", lambda: nc.vector.reciprocal(out=e.ap, in_=e.ap), [e.buf], [e.buf])
                        tt("dve", zs.ap, PZ.ap[:, 0:256], e.ap, ALU.mult, [PZ.buf, e.buf], [zs.buf])
                        tt("pool", zs.ap, zs.ap, gg[:], ALU.mult, [zs.buf, ggb], [zs.buf])
                        yield
                        mm(SM.ap[:, 2:4], [(UT32, gt.ap)], [cstb, gt.buf], [SM.buf])
                        mm(SM.ap[:, 4:6], [(ONES32, gt.ap)], [cstb, gt.buf], [SM.buf])
                        yield
                        act(sc.ap[:, 20:24], SM.ap[:, 2:6], AF.Copy, [SM.buf], [sc.buf])
                        ts("dve", sc.ap[:, 24:26], sc.ap[:, 20:22], -1.0, None, ALU.mult, None, [sc.buf], [sc.buf])
                        D_ = Dm[bk % 2]; DT_ = DTm[bk % 2]; DS_ = DSm[bk % 2]
                        Gbs = [Gbt[h] for h in range(2)]
                        for h in range(2):
                            ts("dve", Gbs[h].ap, ONES32, gt.ap[:, h:h + 1], None, ALU.mult, None, [cstb, gt.buf], [Gbs[h].buf])
                        GCBs = [R(2, (2 * par + h) * 128, (2 * par + h + 1) * 128) for h in range(2)]
                        for h in range(2):
                            mm(GCBs[h].ap, [(Gbs[h].ap, UT32)], [Gbs[h].buf, cstb], [GCBs[h].buf])
                        yield
                        for h in range(2):
                            ngb = ngbt[h]; gbt = gbtt[h]
                            stt("dve", ngb.ap, GCBs[h].ap, -1.0, cst[:, C_NEGM:C_NEGM + 128], ALU.mult, ALU.add, [GCBs[h].buf, cstb], [ngb.buf])
                            tt("dve", gbt.ap, GCBs[h].ap, cst[:, C_NEGMT:C_NEGMT + 128], ALU.add, [GCBs[h].buf, cstb], [gbt.buf])
                            act(D_.ap[:, h, :], ngb.ap, AF.Exp, [ngb.buf, sc.buf], [D_.buf], bias=sc.ap[:, 20 + h:21 + h])
                            act(DT_.ap[:, h, :], gbt.ap, AF.Exp, [gbt.buf, sc.buf], [DT_.buf], bias=sc.ap[:, 24 + h:25 + h])
                            tt("pool", DS_.ap[:, h, :], D_.ap[:, h, :], SM32, ALU.mult, [D_.buf, cstb], [DS_.buf])
                        kh = khat_[bk % 2]; khT = khT_[bk]; khT2 = khT2_[bk % 2]
                        act(kh.ap, TRK.ap[:, 0:128], AF.Copy, [TRK.buf, sc.buf], [kh.buf], scale=sc.ap[:, 1:2])
                        yield
                        TK2 = R("b", par * 512 + 384, par * 512 + 512)
                        trp(TK2.ap, kh.ap, idbf[:], [kh.buf, idbfb], [TK2.buf])
                        yield
                        act(khT.ap, TK2.ap, AF.Copy, [TK2.buf], [khT.buf])
                        cp("dve", khT2.ap, TK2.ap, [TK2.buf], [khT2.buf])
                        vb = vb_[bk]; ktl = ktl_[bk]
                        for h in range(2):
                            act(vb.ap[:, h, :], TRK.ap[:, 128 + h * 128:256 + h * 128], AF.Copy, [TRK.buf, sc.buf], [vb.buf], scale=sc.ap[:, 4 + h:5 + h])
                        tt("dve", sc.ap[:, 6:8], sc.ap[:, 22:24], sc.ap[:, 20:22], ALU.subtract, [sc.buf], [sc.buf])
                        act(sc.ap[:, 6:8], sc.ap[:, 6:8], AF.Exp, [sc.buf], [sc.buf])
                        act(sc.ap[:, 8:12], sc.ap[:, 20:24], AF.Exp, [sc.buf], [sc.buf])
                        stt("dve", sc.ap[:, 12:14], sc.ap[:, 4:6], -1.0, sc.ap[:, 8:10], ALU.mult, ALU.mult, [sc.buf], [sc.buf])
                        ts("dve", sc.ap[:, 14:15], sc.ap[:, 0:1], 128.0 ** -0.5, None, ALU.mult, None, [sc.buf], [sc.buf])
                        ts("dve", sc.ap[:, 16:18], sc.ap[:, 8:10], sc.ap[:, 14:15], None, ALU.mult, None, [sc.buf], [sc.buf])
                        ts("dve", sc.ap[:, 18:20], sc.ap[:, 4:6], -1.0, None, ALU.mult, None, [sc.buf], [sc.buf])
                        for h in range(2):
                            ts("dve", ktl.ap[:, h, :], kh.ap, sc.ap[:, 6 + h:7 + h], None, ALU.mult, None, [kh.buf, sc.buf], [ktl.buf])
                        yield
                        KK = R(2, par * 256, par * 256 + 128); QKT = R(2, par * 256 + 128, par * 256 + 256)
                        mm(KK.ap, [(khT.ap, khT2.ap)], [khT.buf, khT2.buf], [KK.buf])
                        mm(QKT.ap, [(khT.ap, cvo[:, 0, tb])], [khT.buf, cvob], [QKT.buf])
                        yield
                        X = XD[bk % 2]; qkd = QKD[bk]; TTm = TTt[bk]
                        for h in range(2):
                            stt("dve", X.ap[:, h, 0:128], KK.ap, sc.ap[:, 18 + h:19 + h], DS_.ap[:, h, :], ALU.mult, ALU.mult, [KK.buf, sc.buf, DS_.buf], [X.buf])
                            tt("dve", qkd.ap[:, h, :], QKT.ap, DT_.ap[:, h, :], ALU.mult, [QKT.buf, DT_.buf], [qkd.buf])
                        yield
                        DBs = [R(3 + 2 * par + h, 0, 384) for h in range(2)]
                        for h in range(2):
                            trp(DBs[h].ap[:, 128:256], X.ap[:, h, 0:128], ID32, [X.buf, cstb], [DBs[h].buf])
                        yield
                        for h in range(2):
                            act(X.ap[:, h, 128:256], DBs[h].ap[:, 128:256], AF.Copy, [DBs[h].buf], [X.buf])
                            tt("dve", X.ap[:, h, 256:384], DBs[h].ap[:, 128:256], ID32, ALU.add, [DBs[h].buf, cstb], [X.buf])
                        yield
                        for h in range(2):
                            mm(DBs[h].ap[:, 0:128], [(X.ap[:, h, 128:256], X.ap[:, h, 0:128])], [X.buf], [DBs[h].buf])
                            mm(DBs[h].ap[:, 128:256], [(X.ap[:, h, 0:128], X.ap[:, h, 128:256])], [X.buf], [DBs[h].buf])
                        yield
                        for h in range(2):
                            act(X.ap[:, h, 0:256], DBs[h].ap[:, 0:256], AF.Copy, [DBs[h].buf], [X.buf])
                        yield
                        for lev in range(5):
                            for h in range(2):
                                mm(DBs[h].ap[:, 0:128], [(X.ap[:, h, 128:256], X.ap[:, h, 0:128])], [X.buf], [DBs[h].buf])
                                mm(DBs[h].ap[:, 128:384], [(X.ap[:, h, 0:128], X.ap[:, h, 128:384])], [X.buf], [DBs[h].buf])
                            yield
                            for h in range(2):
                                act(X.ap[:, h, 0:256], DBs[h].ap[:, 0:256], AF.Copy, [DBs[h].buf], [X.buf])
                                tt("dve", X.ap[:, h, 256:384], X.ap[:, h, 256:384], DBs[h].ap[:, 256:384], ALU.add, [DBs[h].buf, X.buf], [X.buf])
                            yield
                        for h in range(2):
                            mm(DBs[h].ap[:, 0:128], [(X.ap[:, h, 0:128], X.ap[:, h, 256:384])], [X.buf], [DBs[h].buf])
                        yield
                        for h in range(2):
                            tt("dve", TTm.ap[:, h, :], X.ap[:, h, 256:384], DBs[h].ap[:, 0:128], ALU.add, [DBs[h].buf, X.buf], [TTm.buf])

                    for bk0 in range(0, NBK, 2):
                        alive = [pre(bk) for bk in range(bk0, min(NBK, bk0 + 2))]
                        while alive:
                            nxt = []
                            for gen_ in alive:
                                try:
                                    next(gen_)
                                    nxt.append(gen_)
                                except StopIteration:
                                    pass
                            alive = nxt
                    for bk in range(NBK):
                        it = git; git += 1
                        blk = tt_i * NBK + bk
                        tb = slice(bk * 128, (bk + 1) * 128)
                        sc = scal[bk]; khT = khT_[bk]; vb = vb_[bk]; ktl = ktl_[bk]; qkd = QKD[bk]; TTm = TTt[bk]; zs = zsb_[bk]
                        sprev = Sbf[sidx % 2]; snext = Sbf[(sidx + 1) % 2]; sidx += 1
                        rr = r_[it % 2]; qs_ = qSs[it % 2]; vn = vn_[it % 2]; o_ = osb[it % 2]; st = st4[it % 2]; jk = junk[it % 2]
                        for h in range(2):
                            kS = R(3 + h, 0, 128)
                            qS = R(3 + h, 128, 256)
                            mm(kS.ap, [(khT.ap, sprev.ap[:, h, :])], [khT.buf, sprev.buf], [kS.buf])
                            mm(qS.ap, [(cvo[:, 0, tb], sprev.ap[:, h, :])], [cvob, sprev.buf], [qS.buf])
                        for h in range(2):
                            kS = R(3 + h, 0, 128)
                            qS = R(3 + h, 128, 256)
                            stt("dve", rr.ap[:, h, :], kS.ap, sc.ap[:, 12 + h:13 + h], vb.ap[:, h, :], ALU.mult, ALU.add, [kS.buf, sc.buf, vb.buf], [rr.buf])
                            act(qs_.ap[:, h, :], qS.ap, AF.Copy, [qS.buf, sc.buf], [qs_.buf], scale=sc.ap[:, 16 + h:17 + h])
                        for h in range(2):
                            VN = R(5 + h, 0, 128)
                            mm(VN.ap, [(TTm.ap[:, h, :], rr.ap[:, h, :])], [TTm.buf, rr.buf], [VN.buf])
                        for h in range(2):
                            VN = R(5 + h, 0, 128)
                            act(vn.ap[:, h, :], VN.ap, AF.Copy, [VN.buf], [vn.buf])
                        for h in range(2):
                            O2 = R(5 + h, 128, 256)
                            KVr = R(5 + h, 256, 384)
                            mm(O2.ap, [(qkd.ap[:, h, :], vn.ap[:, h, :])], [qkd.buf, vn.buf], [O2.buf])
                            mm(KVr.ap, [(ktl.ap[:, h, :], vn.ap[:, h, :])], [ktl.buf, vn.buf], [KVr.buf])
                        for h in range(2):
                            O2 = R(5 + h, 128, 256)
                            KVr = R(5 + h, 256, 384)
                            stt("dve", S32[:, h, :], S32[:, h, :], sc.ap[:, 10 + h:11 + h], KVr.ap, ALU.mult, ALU.add, [S32b, sc.buf, KVr.buf], [S32b])
                            act(snext.ap[:, h, :], S32[:, h, :], AF.Copy, [S32b], [snext.buf])
                            stt("dve", o_.ap[:, h * 128:(h + 1) * 128], O2.ap, sc.ap[:, 14:15], qs_.ap[:, h, :], ALU.mult, ALU.add,
                                [O2.buf, sc.buf, qs_.buf], [o_.buf])
                            act(jk.ap[:, h * 128:(h + 1) * 128], o_.ap[:, h * 128:(h + 1) * 128], AF.Square, [o_.buf], [jk.buf, st.buf], accum_out=st.ap[:, h:h + 1])
                        act(st.ap[:, 2:4], st.ap[:, 0:2], AF.Ln, [st.buf, epsGb], [st.buf], scale=1.0 / 128, bias=epsG[:, 0:1])
                        act(st.ap[:, 2:4], st.ap[:, 2:4], AF.Exp, [st.buf], [st.buf], scale=-0.5)
                        bt = brt[it % 3]
                        for h in range(2):
                            stt("dve", bt.ap[:, h * 128:(h + 1) * 128], o_.ap[:, h * 128:(h + 1) * 128], st.ap[:, 2 + h:3 + h], zs.ap[:, h * 128:(h + 1) * 128],
                                ALU.mult, ALU.mult, [o_.buf, st.buf, zs.buf], [bt.buf])
                        store_branch(bt, blk, g * 256, 256)
                for h in range(2):
                    c.dma("sp", st_d[l][2 * g + h][:, 0:128], S32[:, h, :], [S32b], [], S32b)
                c.dma("sp", cst8_d[l][g].rearrange("p (a b) -> p a b", a=4), hraw[:, :, 0:3], [hrawb], [], hrawb)

        def phase_b(l, s, last):
            tok0 = s * SEGT
            TT = min(512, SEGT)
            NBK = TT // 128
            c.begin_phase()
            with contextlib.ExitStack() as ps:
                brT = wtiles(ps, "brT", [128, 32, TT], BF16, 2)
                wo = wtiles(ps, "wo", [128, 32, 256], BF16, 2)
                xr2 = [wtiles(ps, "xr%d" % i, [128, D], F32, NBK) for i in range(2)]
                jk, jkb = sbt(ps, "jkB", [128, D], F32)
                lng, lngb = load_bcast(ps, "lng", lng_d[l], D)
                lnb, lnbb = load_bcast(ps, "lnb", lnb_d[l], D)
                st = wtiles(ps, "stB", [128, 8], F32, 2)
                epsT, epsTb = sbt(ps, "epsB", [128, 1], F32)
                c.op("dve", lambda: nc.vector.memset(epsT[:], LN_EPS), [], [epsTb])
                PS = [Reg(ps.enter_context(nc.psum_tensor(uname("pB"), [128, 512], F32))[:], Buf("pB%d" % i, excl=True)) for i in range(8)]
                src = x_d if l == 0 else xres_d
                dst = out_d if last else xres_d
                pit = 0
                wit = 0

                def tile_loads(ti):
                    tl0 = tok0 + ti * TT
                    bT_ = brT[ti % 2]
                    for fc in range(32):
                        c.dma("sp", bT_.ap[:, fc, :], br_d[tl0:tl0 + TT, fc * 128:(fc + 1) * 128], [], [bT_.buf], bT_.buf, transpose=True)
                    for bk in range(NBK):
                        xx = xr2[ti % 2][bk]
                        c.dma("sp", xx.ap, src[tl0 + bk * 128:tl0 + (bk + 1) * 128, :], [], [xx.buf], xx.buf)

                tile_loads(0)
                for tt_i in range(SEGT // TT):
                    t0 = tok0 + tt_i * TT
                    bT = brT[tt_i % 2]
                    xr = xr2[tt_i % 2]
                    y = xr
                    if tt_i + 1 < SEGT // TT:
                        tile_loads(tt_i + 1)
                    for n in range(8):
                        w = wo[wit % 2]; wit += 1
                        for q4 in range(2):
                            c.dma("act", w.ap[:, q4 * 16:(q4 + 1) * 16, :], wob_d[l][n][:, q4 * 16:(q4 + 1) * 16, :], [], [w.buf], w.buf)
                        for bk in range(NBK):
                            p = PS[pit % 8]; pit += 1
                            mm(p.ap[:, 0:256], [(bT.ap[:, fc, bk * 128:(bk + 1) * 128], w.ap[:, fc, :]) for fc in range(32)],
                               [bT.buf, w.buf], [p.buf])
                            stt("dve", y[bk].ap[:, n * 256:(n + 1) * 256], xr[bk].ap[:, n * 256:(n + 1) * 256], ALPHA, p.ap[:, 0:256],
                                ALU.mult, ALU.add, [xr[bk].buf, p.buf], [y[bk].buf])
                    for bk in range(NBK):
                        s_ = st[bk % 2]; yy = y[bk]
                        act(jk[:], yy.ap, AF.Copy, [yy.buf], [jkb, s_.buf], accum_out=s_.ap[:, 0:1])
                        act(jk[:], yy.ap, AF.Square, [yy.buf], [jkb, s_.buf], accum_out=s_.ap[:, 1:2])
                        ts("dve", s_.ap[:, 2:3], s_.ap[:, 0:1], 1.0 / D, None, ALU.mult, None, [s_.buf], [s_.buf])
                        tt("dve", s_.ap[:, 3:4], s_.ap[:, 2:3], s_.ap[:, 2:3], ALU.mult, [s_.buf], [s_.buf])
                        stt("dve", s_.ap[:, 4:5], s_.ap[:, 1:2], 1.0 / D, s_.ap[:, 3:4], ALU.mult, ALU.subtract, [s_.buf], [s_.buf])
                        act(s_.ap[:, 5:6], s_.ap[:, 4:5], AF.Ln, [s_.buf, epsTb], [s_.buf], bias=epsT[:, 0:1])
                        act(s_.ap[:, 5:6], s_.ap[:, 5:6], AF.Exp, [s_.buf], [s_.buf], scale=-0.5)
                        ts("dve", yy.ap, yy.ap, s_.ap[:, 2:3], s_.ap[:, 5:6], ALU.subtract, ALU.mult, [yy.buf, s_.buf], [yy.buf])
                        tt("pool", yy.ap, yy.ap, lng[:], ALU.mult, [yy.buf, lngb], [yy.buf])
                        tt("pool", yy.ap, yy.ap, lnb[:], ALU.add, [yy.buf, lnbb], [yy.buf])
                        r0 = t0 + bk * 128
                        c.dma("sp", dst[r0:r0 + 128, :], yy.ap, [yy.buf], [], yy.buf)
                        if not last:
                            c.dma("pool", xb_d[r0:r0 + 128, :], yy.ap, [yy.buf], [], yy.buf)
                c.end_phase()

        for l in range(NL):
            mem_kv(l)
            for s in range(NSEG):
                phase_a(l, s)
                phase_b(l, s, l == NL - 1)
        if dbg:
            pass
        c.barrier()
    return nc


def _consts():
    cst = np.zeros((128, C_END), np.float32)
    i = np.arange(128)
    cst[:, C_ID:C_ID + 128] = np.eye(128, dtype=np.float32)
    cst[:, C_UT:C_UT + 128] = (i[None, :] >= i[:, None]).astype(np.float32)
    cst[:, C_SM:C_SM + 128] = (i[:, None] > i[None, :]).astype(np.float32)
    cst[:, C_NEGM:C_NEGM + 128] = np.where(i[None, :] > i[:, None], -30000.0, 0.0)
    cst[:, C_NEGMT:C_NEGMT + 128] = np.where(i[:, None] > i[None, :], -30000.0, 0.0)
    cst[:, C_ONES:C_ONES + 128] = 1.0
    for h in range(12):
        gam = 1.0 - 2.0 ** (-5.0 - h)
        cst[:, C_QS + h] = gam ** (i + 1.0)
        cst[:, C_KS + h] = gam ** (-(i + 1.0)) * 128.0 ** -0.5
    half = 64
    invf = (10000.0 ** (-np.arange(half, dtype=np.float32) / half)).astype(np.float32)
    cst[:, C_INVF:C_INVF + 64] = invf[None, :]
    return cst


def _kc_layout(w, ncols_pad=None):
    n = w.shape[1]
    a = np.ascontiguousarray(w.reshape(KC, 128, n).transpose(1, 0, 2))
    if ncols_pad is not None and ncols_pad != n:
        o = np.zeros((128, KC, ncols_pad), np.float32)
        o[:, :, :n] = a
        return o
    return a


def prep_weights(inp, kinds):
    ws = {"consts": _consts()}
    for l, kind in enumerate(kinds):
        j = l // 2
        wg = np.zeros((16, 128, KC, GW), np.float32)
        if kind == "ret":
            w = np.asarray(inp["w_in_ret"][j])
            for h in range(12):
                cols = np.concatenate([np.arange(h * 128, (h + 1) * 128), 1536 + np.arange(h * 128, (h + 1) * 128),
                                       3072 + np.arange(h * 256, (h + 1) * 256), 7168 + np.arange(h * 256, (h + 1) * 256)])
                wg[h] = _kc_layout(w[:, cols], GW)
            ws["rng%d" % l] = np.ascontiguousarray(inp["ret_norm_g"][j], dtype=np.float32)
        else:
            w = np.asarray(inp["w_in_gdn"][j])
            cwl = np.zeros((128, 192), np.float32)
            cw = np.asarray(inp["conv_w"][j])
            for g in range(12):
                chans = [g * 128, 1536 + g * 128, 3072 + (2 * g) * 128, 3072 + (2 * g + 1) * 128]
                cols = np.concatenate([np.arange(ch, ch + 128) for ch in chans] + [7168 + np.arange(g * 256, (g + 1) * 256),
                                      np.array([11264 + 2 * g, 11264 + 2 * g + 1, 11288 + 2 * g, 11288 + 2 * g + 1])])
                wg[g] = _kc_layout(w[:, cols], GW)
                for ct, ch in enumerate(chans):
                    for jj in range(4):
                        cwl[:, g * 16 + ct * 4 + jj] = cw[jj, ch:ch + 128]
            ws["cw%d" % l] = cwl
            ws["gng%d" % l] = np.ascontiguousarray(inp["gdn_norm_g"][j], dtype=np.float32)
            ws["alog%d" % l] = np.ascontiguousarray(inp["a_log"][j], dtype=np.float32)
            ws["dtb%d" % l] = np.ascontiguousarray(inp["dt_bias"][j], dtype=np.float32)
        for m in range(4):
            cols = np.concatenate([6144 + np.arange(m * 256, (m + 1) * 256), 7168 + 3072 + np.arange(m * 256, (m + 1) * 256)])
            wg[12 + m] = _kc_layout(w[:, cols], GW)
        ws["wg%d" % l] = wg
        wo = np.asarray(inp["w_out"][l])
        ws["wo%d" % l] = np.ascontiguousarray(wo.reshape(32, 128, 8, 256).transpose(2, 1, 0, 3))
        wkv = np.asarray(inp["w_mem_kv"][l])
        ws["wkv%d" % l] = np.ascontiguousarray(wkv.reshape(KC, 128, 4, 512).transpose(2, 1, 0, 3))
        ws["lng%d" % l] = np.ascontiguousarray(inp["ln_g"][l], dtype=np.float32)
        ws["lnb%d" % l] = np.ascontiguousarray(inp["ln_b"][l], dtype=np.float32)
    return ws


def core_map(ws, x_b, mem_b, pos_b):
    T = x_b.shape[0]
    m = dict(ws)
    m["x"] = np.ascontiguousarray(x_b, dtype=np.float32)
    m["mem"] = np.ascontiguousarray(mem_b, dtype=np.float32)
    m["pos"] = np.ascontiguousarray(np.asarray(pos_b, dtype=np.int32).reshape(T // 128, 128).T)
    return m


KINDS = ["ret", "gdn", "ret", "gdn"]


def kernel(x, mem, positions, w_in_ret, ret_norm_g, w_in_gdn, conv_w, a_log, dt_bias, gdn_norm_g,
           w_mem_kv, w_out, ln_g, ln_b):
    inp = dict(w_in_ret=w_in_ret, ret_norm_g=ret_norm_g, w_in_gdn=w_in_gdn, conv_w=conv_w, a_log=a_log, dt_bias=dt_bias,
               gdn_norm_g=gdn_norm_g, w_mem_kv=w_mem_kv, w_out=w_out, ln_g=ln_g, ln_b=ln_b)
    x = np.asarray(x); mem = np.asarray(mem); positions = np.asarray(positions)
    B, S, _ = x.shape
    ws = prep_weights(inp, KINDS)
    nc = build(2048, S // 2048, KINDS)
    in_maps = [core_map(ws, x[b % B], mem[b % B], positions[b % B]) for b in range(8)]
    res = run_bass_kernel_spmd(nc, in_maps, core_ids=list(range(8)))
    return np.stack([res.results[b]["out"] for b in range(B)], axis=0).astype(np.float32)
```
